# Optimizing a Trainium2 kernel written in Bass

```python
import math
import jax, jax.numpy as jnp
from jax import lax
import numpy as np

D_MODEL = 1024
BATCH = 4
SEQ = 8192
DEPTH = 4

N_MIXERS = 2
EPS = 1e-6

ML_HEADS = 4
ML_QK_DIM = D_MODEL // 2 // ML_HEADS
ML_V_DIM = D_MODEL // ML_HEADS
ML_QK = ML_HEADS * ML_QK_DIM
ML_V = ML_HEADS * ML_V_DIM
ML_IN = 2 * ML_QK + 2 * ML_V + 2 * ML_HEADS
ML_CHUNK = 64
GATE_CAP = 15.0

SW_Q_HEADS = 16
SW_KV_HEADS = 4
SW_GROUP = SW_Q_HEADS // SW_KV_HEADS
SW_HEAD_DIM = 64
WINDOW = 128
SW_IN = (SW_Q_HEADS + 2 * SW_KV_HEADS) * SW_HEAD_DIM
SW_OUT_IN = SW_Q_HEADS * SW_HEAD_DIM

N_GROUPS = 8
EXPERTS_PER_GROUP = 8
N_EXPERTS = N_GROUPS * EXPERTS_PER_GROUP
TOP_K = 2
D_EXPERT = 384
MOE_BLOCK = 256

kernel_name = "hybrid_mlstm_swa_hmoe_adaln"


def rmsnorm(x, g):
    xf = x.astype(jnp.float32)
    y = xf * lax.rsqrt(jnp.mean(xf * xf, axis=-1, keepdims=True) + EPS)
    return y.astype(x.dtype) * g


def softcap(z):
    return GATE_CAP * jnp.tanh(z / GATE_CAP)


def mlstm_mixer(h, w_in, b_gate, g_out, w_out):
    B, S, _ = h.shape
    H, dk, dv, L = ML_HEADS, ML_QK_DIM, ML_V_DIM, ML_CHUNK
    NC = S // L
    proj = h @ w_in
    q, k, v, o, gates = jnp.split(proj, [ML_QK, 2 * ML_QK, 2 * ML_QK + ML_V, 2 * ML_QK + 2 * ML_V], axis=-1)
    gates = softcap(gates.astype(jnp.float32) + b_gate)
    i_pre = gates[..., :H]
    log_f = jax.nn.log_sigmoid(gates[..., H:])

    def to_chunks(t, d):
        return t.astype(jnp.float32).reshape(B, NC, L, H, d).transpose(1, 0, 3, 2, 4)

    def gate_chunks(t):
        return t.reshape(B, NC, L, H).transpose(1, 0, 3, 2)

    qc = to_chunks(q, dk)
    kc = to_chunks(k, dk) * (dk ** -0.5)
    vc = to_chunks(v, dv)
    ic = gate_chunks(i_pre)
    fc = gate_chunks(log_f)
    causal = jnp.tril(jnp.ones((L, L), dtype=bool))

    def step(carry, inp):
        C, n, m = carry
        qb, kb, vb, ib, fb = inp
        b = jnp.cumsum(fb, axis=-1)
        d_log = jnp.where(causal, b[..., :, None] - b[..., None, :] + ib[..., None, :], -jnp.inf)
        inter = b + m[..., None]
        m_t = jnp.maximum(inter, d_log.max(-1))
        w_intra = jnp.exp(d_log - m_t[..., None])
        w_inter = jnp.exp(inter - m_t)
        s = jnp.einsum('bhtd,bhsd->bhts', qb, kb) * w_intra
        num = w_inter[..., None] * jnp.einsum('bhtd,bhdv->bhtv', qb, C) + jnp.einsum('bhts,bhsv->bhtv', s, vb)
        den = w_inter * jnp.einsum('bhtd,bhd->bht', qb, n) + s.sum(-1)
        hb = num / jnp.maximum(jnp.abs(den), jnp.exp(-m_t))[..., None]
        g = b[..., -1]
        a = g[..., None] - b + ib
        m_new = jnp.maximum(g + m, a.max(-1))
        w_state = jnp.exp(a - m_new[..., None])
        decay = jnp.exp(g + m - m_new)
        C_new = decay[..., None, None] * C + jnp.einsum('bhs,bhsd,bhsv->bhdv', w_state, kb, vb)
        n_new = decay[..., None] * n + jnp.einsum('bhs,bhsd->bhd', w_state, kb)
        return (C_new, n_new, m_new), hb

    init = (jnp.zeros((B, H, dk, dv), jnp.float32), jnp.zeros((B, H, dk), jnp.float32), jnp.zeros((B, H), jnp.float32))
    _, hs = lax.scan(step, init, (qc, kc, vc, ic, fc))
    hs = hs.transpose(1, 0, 3, 2, 4).reshape(B, S, H, dv)
    hs = rmsnorm(hs, g_out.reshape(H, dv)).astype(h.dtype)
    hs = hs * jax.nn.sigmoid(o).reshape(B, S, H, dv)
    return hs.reshape(B, S, ML_V) @ w_out


def swa_mixer(h, w_in, g_q, g_k, sinks, w_out):
    B, S, _ = h.shape
    W, dh = WINDOW, SW_HEAD_DIM
    NB = S // W
    proj = h @ w_in
    q, k, v = jnp.split(proj, [SW_Q_HEADS * dh, (SW_Q_HEADS + SW_KV_HEADS) * dh], axis=-1)
    q = rmsnorm(q.reshape(B, S, SW_Q_HEADS, dh), g_q)
    k = rmsnorm(k.reshape(B, S, SW_KV_HEADS, dh), g_k)
    v = v.reshape(B, S, SW_KV_HEADS, dh)
    qb = q.reshape(B, NB, W, SW_KV_HEADS, SW_GROUP, dh)

    def band(t):
        tb = t.reshape(B, NB, W, SW_KV_HEADS, dh)
        prev = jnp.concatenate([jnp.zeros_like(tb[:, :1]), tb[:, :-1]], axis=1)
        return jnp.concatenate([prev, tb], axis=2)

    kb, vb = band(k), band(v)
    scores = jnp.einsum('bnqhgd,bnkhd->bnhgqk', qb, kb).astype(jnp.float32) * (dh ** -0.5)
    qi = jnp.arange(W)[:, None]
    ki = jnp.arange(2 * W)[None, :]
    rel = qi + W - ki
    in_win = (rel >= 0) & (rel < W)
    first = (jnp.arange(NB)[:, None, None] == 0) & (ki[None] < W)
    mask = in_win[None] & ~first
    scores = jnp.where(mask[None, :, None, None], scores, -jnp.inf)
    sink = sinks.astype(jnp.float32).reshape(SW_KV_HEADS, SW_GROUP)[None, None, :, :, None]
    m = jnp.maximum(scores.max(-1), sink)
    p = jnp.exp(scores - m[..., None])
    denom = p.sum(-1) + jnp.exp(sink - m)
    p = (p / denom[..., None]).astype(v.dtype)
    o = jnp.einsum('bnhgqk,bnkhd->bnqhgd', p, vb)
    return o.reshape(B, S, SW_OUT_IN) @ w_out


def hier_moe(h, w_group, b_group, w_router, b_router, w1, w3, w2):
    B, S, D = h.shape
    T = B * S
    A = T * TOP_K
    xt = h.reshape(T, D)
    grp_logits = (xt @ w_group + b_group).astype(jnp.float32)
    g_sel = jnp.argmax(grp_logits, axis=-1)
    p_grp = jnp.take_along_axis(jax.nn.softmax(grp_logits, axis=-1), g_sel[:, None], axis=-1)
    exp_logits = (xt @ w_router + b_router).astype(jnp.float32).reshape(T, N_GROUPS, EXPERTS_PER_GROUP)
    in_grp = jnp.take_along_axis(exp_logits, g_sel[:, None, None], axis=1)[:, 0]
    top_v, top_i = lax.top_k(in_grp, TOP_K)
    gate = jax.nn.softmax(top_v, axis=-1) * p_grp
    e_idx = (g_sel[:, None] * EXPERTS_PER_GROUP + top_i).reshape(A).astype(jnp.int32)
    tok = jnp.repeat(jnp.arange(T, dtype=jnp.int32), TOP_K)

    order = jnp.argsort(e_idx, stable=True)
    se = e_idx[order]
    counts = jnp.bincount(e_idx, length=N_EXPERTS)
    starts = jnp.cumsum(counts) - counts
    padded = (counts + MOE_BLOCK - 1) // MOE_BLOCK * MOE_BLOCK
    pad_ends = jnp.cumsum(padded)
    pad_starts = pad_ends - padded
    dest = pad_starts[se] + jnp.arange(A, dtype=jnp.int32) - starts[se]
    NB = -(-A // MOE_BLOCK) + N_EXPERTS
    P = NB * MOE_BLOCK
    slot_tok = jnp.full((P,), T, dtype=jnp.int32).at[dest].set(tok[order])
    slot_w = jnp.zeros((P,), jnp.float32).at[dest].set(gate.reshape(A)[order])
    blk_exp = jnp.minimum(jnp.searchsorted(pad_ends, jnp.arange(NB, dtype=jnp.int32) * MOE_BLOCK, side='right'), N_EXPERTS - 1)
    x_slots = jnp.concatenate([xt, jnp.zeros((1, D), xt.dtype)], axis=0)[slot_tok].reshape(NB, MOE_BLOCK, D)

    def expert_block(args):
        xb, e = args
        return (jax.nn.silu(xb @ w1[e]) * (xb @ w3[e])) @ w2[e]

    y_slots = lax.map(expert_block, (x_slots, blk_exp)).reshape(P, D)
    y = jnp.zeros((T + 1, D), h.dtype).at[slot_tok].add(y_slots * slot_w[:, None].astype(h.dtype))
    return y[:T].reshape(B, S, D)


def setup_inputs(seed: int = 0) -> dict:
    key = jax.random.key(seed)
    ks = jax.random.split(key, 24)
    n_ml = (DEPTH + 1) // N_MIXERS
    n_sw = DEPTH // N_MIXERS
    D = D_MODEL
    nrm = lambda k, shape, s: jax.random.normal(k, shape, jnp.float32) * s
    b_i = -1.0 + nrm(ks[6], (n_ml, ML_HEADS), 0.1)
    b_f = 3.0 + nrm(ks[7], (n_ml, ML_HEADS), 0.5)
    return {
        "x": nrm(ks[0], (BATCH, SEQ, D), 1.0),
        "c": nrm(ks[1], (BATCH, D), 1.0),
        "w_ada": nrm(ks[2], (DEPTH, D, 6 * D), 0.5 * D ** -0.5),
        "b_ada": nrm(ks[3], (DEPTH, 6 * D), 0.02),
        "norm1_g": 1.0 + nrm(ks[4], (DEPTH, D), 0.05),
        "norm2_g": 1.0 + nrm(ks[5], (DEPTH, D), 0.05),
        "ml_w_in": nrm(ks[8], (n_ml, D, ML_IN), D ** -0.5),
        "ml_b_gate": jnp.concatenate([b_i, b_f], axis=-1),
        "ml_g_out": 1.0 + nrm(ks[9], (n_ml, ML_V), 0.05),
        "ml_w_out": nrm(ks[10], (n_ml, ML_V, D), ML_V ** -0.5),
        "sw_w_in": nrm(ks[11], (n_sw, D, SW_IN), D ** -0.5),
        "sw_g_q": 1.0 + nrm(ks[12], (n_sw, SW_HEAD_DIM), 0.05),
        "sw_g_k": 1.0 + nrm(ks[13], (n_sw, SW_HEAD_DIM), 0.05),
        "sw_sinks": nrm(ks[14], (n_sw, SW_Q_HEADS), 0.5),
        "sw_w_out": nrm(ks[15], (n_sw, SW_OUT_IN, D), SW_OUT_IN ** -0.5),
        "moe_w_group": nrm(ks[16], (DEPTH, D, N_GROUPS), D ** -0.5),
        "moe_b_group": nrm(ks[17], (DEPTH, N_GROUPS), 0.01),
        "moe_w_router": nrm(ks[18], (DEPTH, D, N_EXPERTS), D ** -0.5),
        "moe_b_router": nrm(ks[19], (DEPTH, N_EXPERTS), 0.01),
        "moe_w1": nrm(ks[20], (DEPTH, N_EXPERTS, D, D_EXPERT), D ** -0.5),
        "moe_w3": nrm(ks[21], (DEPTH, N_EXPERTS, D, D_EXPERT), D ** -0.5),
        "moe_w2": nrm(ks[22], (DEPTH, N_EXPERTS, D_EXPERT, D), D_EXPERT ** -0.5),
    }


def reference(x, c, w_ada, b_ada, norm1_g, norm2_g, ml_w_in, ml_b_gate, ml_g_out, ml_w_out,
              sw_w_in, sw_g_q, sw_g_k, sw_sinks, sw_w_out, moe_w_group, moe_b_group,
              moe_w_router, moe_b_router, moe_w1, moe_w3, moe_w2):
    cond = jax.nn.silu(c)
    for layer in range(DEPTH):
        mod = (cond @ w_ada[layer] + b_ada[layer])[:, None, :]
        sh1, sc1, gt1, sh2, sc2, gt2 = jnp.split(mod, 6, axis=-1)
        hn = rmsnorm(x, norm1_g[layer]) * (1.0 + sc1) + sh1
        j = layer // N_MIXERS
        if layer % N_MIXERS == 0:
            y = mlstm_mixer(hn, ml_w_in[j], ml_b_gate[j], ml_g_out[j], ml_w_out[j])
        else:
            y = swa_mixer(hn, sw_w_in[j], sw_g_q[j], sw_g_k[j], sw_sinks[j], sw_w_out[j])
        x = x + gt1 * y
        hn = rmsnorm(x, norm2_g[layer]) * (1.0 + sc2) + sh2
        x = x + gt2 * hier_moe(hn, moe_w_group[layer], moe_b_group[layer], moe_w_router[layer],
                               moe_b_router[layer], moe_w1[layer], moe_w3[layer], moe_w2[layer])
    return x
```

```python
import math
import threading
from contextlib import ExitStack

import numpy as np
import concourse.bass as bass
import concourse.mybir as mybir
from concourse.bass_utils import run_bass_kernel_spmd

F32 = mybir.dt.float32
BF16 = mybir.dt.bfloat16
I32 = mybir.dt.int32
AF = mybir.ActivationFunctionType
ALU = mybir.AluOpType
AX = mybir.AxisListType

D = 1024
KC = 8
DEPTH = 4
EPS = 1e-6
ML_IN = 3080
SW_IN = 1536
NE = 64
DE = 384
BS = 256
NEG = -30000.0


class Buf:
    def __init__(self, K, name, t, dram=False):
        self.K = K
        self.name = name
        self.t = t
        self.dram = dram
        self.w = {}
        self.r = {}
        self.dsem = None
        self.dcnt = 0
        self.wgroup = None
        K.all_bufs.append(self)

    def __getitem__(self, k):
        return self.t[k]

    def ap(self):
        return self.t.ap() if self.dram else self.t[:]


class Eng:
    def __init__(self, name, h, sem):
        self.name = name
        self.h = h
        self.sem = sem
        self.cnt = 0
        self.waited = {}


class Kern:
    def __init__(self, nc, stack):
        self.nc = nc
        self.stack = stack
        self.sems = {}
        self.engs = {}
        self.all_bufs = []
        for nm, h in (("pe", nc.tensor), ("act", nc.scalar), ("dve", nc.vector),
                      ("pool", nc.gpsimd), ("sp", nc.sync)):
            s = stack.enter_context(nc.semaphore("sem_" + nm))
            self.sems[id(s)] = s
            self.engs[nm] = Eng(nm, h, s)
        self.dsem_pool = []
        self.ninst = 0
        self.outst = {"sp": [], "act": [], "pool": []}
        self.hold = 0
        self.il = None
        self.maxq = {"sp": 12, "act": 12, "pool": 8}

    def sbuf(self, name, shape, dt):
        t = self.stack.enter_context(self.nc.sbuf_tensor(name, list(shape), dt))
        return Buf(self, name, t)

    def psum(self, name, shape, dt=F32):
        t = self.stack.enter_context(self.nc.psum_tensor(name, list(shape), dt))
        return Buf(self, name, t)

    def dram(self, name, shape, dt, kind="Internal"):
        t = self.nc.dram_tensor(name, list(shape), dt, kind=kind)
        return Buf(self, name, t, dram=True)

    def view(self, name, ap):
        return Buf(self, name, ap)

    def _dsem(self, b):
        if b.dsem is None:
            s = self.stack.enter_context(self.nc.semaphore("ds_" + b.name))
            self.sems[id(s)] = s
            b.dsem = s
        return b.dsem

    def _deps(self, reads, writes, group=None):
        deps = {}
        for b in reads:
            for k, v in b.w.items():
                if deps.get(k, 0) < v:
                    deps[k] = v
        for b in writes:
            same = (group is not None and b.wgroup == group)
            for d in ((b.r,) if same else (b.w, b.r)):
                for k, v in d.items():
                    if deps.get(k, 0) < v:
                        deps[k] = v
        return deps

    def _wait(self, e, deps, skip_self=False):
        for k, v in deps.items():
            if skip_self and k == id(e.sem):
                continue
            if e.waited.get(k, 0) < v:
                e.h.wait_ge(self.sems[k], v)
                e.waited[k] = v
                self.ninst += 1

    def _commit(self, reads, writes, k, v, group=None):
        for b in reads:
            if b.r.get(k, 0) < v:
                b.r[k] = v
        for b in writes:
            if group is not None and b.wgroup == group:
                if b.w.get(k, 0) < v:
                    b.w[k] = v
            else:
                b.w = {k: v}
            b.wgroup = group
            b.r = {}

    def interleave(self, fns, credit=3):
        if len(fns) == 1:
            fns[0]()
            return
        il = {"turn": 0, "alive": [True] * len(fns), "ids": {}, "err": None, "credit": credit, "left": credit,
              "cv": threading.Condition(), "sig": set()}
        self.il = il

        def nxt(i):
            n = len(fns)
            for d in range(1, n):
                j = (i + d) % n
                if il["alive"][j]:
                    il["turn"] = j
                    il["left"] = il["credit"]
                    return
            il["left"] = il["credit"]

        il["nxt"] = nxt

        def worker(i, fn):
            il["ids"][threading.get_ident()] = i
            with il["cv"]:
                while il["turn"] != i:
                    il["cv"].wait()
            try:
                fn()
            except BaseException as ex:
                il["err"] = ex
            finally:
                with il["cv"]:
                    il["alive"][i] = False
                    nxt(i)
                    il["cv"].notify_all()

        ths = [threading.Thread(target=worker, args=(i, f)) for i, f in enumerate(fns)]
        for th in ths:
            th.start()
        for th in ths:
            th.join()
        self.il = None
        if il["err"] is not None:
            raise il["err"]

    def signal(self, key):
        il = self.il
        if il is None:
            return
        with il["cv"]:
            il["sig"].add(key)

    def wait_for(self, key):
        il = self.il
        if il is None:
            return
        i = il["ids"].get(threading.get_ident())
        assert self.hold == 0
        with il["cv"]:
            while key not in il["sig"]:
                assert any(a for j, a in enumerate(il["alive"]) if j != i), ("deadlock waiting for", key)
                il["nxt"](i)
                il["cv"].notify_all()
                while il["turn"] != i:
                    il["cv"].wait()

    def point(self):
        il = self.il
        if il is None or self.hold > 0:
            return
        i = il["ids"].get(threading.get_ident())
        if i is None:
            return
        with il["cv"]:
            il["left"] -= 1
            if il["left"] <= 0:
                il["nxt"](i)
                il["cv"].notify_all()
                while il["turn"] != i:
                    il["cv"].wait()

    def op(self, eng, fn, reads=(), writes=(), inc=True):
        self.point()
        e = self.engs[eng]
        self._wait(e, self._deps(reads, writes), skip_self=(eng == "pe"))
        ins = fn(e.h)
        self.ninst += 1
        if inc:
            e.cnt += 1
            ins.then_inc(e.sem, 1)
            v = e.cnt
        else:
            v = e.cnt + 1
        self._commit(reads, writes, id(e.sem), v)
        return ins

    def dma(self, q, fn, reads=(), writes=(), sem_buf=None, group=None):
        self.point()
        e = self.engs[q]
        self._wait(e, self._deps(reads, writes, group))
        if sem_buf is None:
            cand = [b for b in list(writes) + list(reads) if not b.dram]
            sem_buf = cand[0] if cand else (list(writes) + list(reads))[0]
        s = self._dsem(sem_buf)
        ins = fn(e.h)
        self.ninst += 1
        sem_buf.dcnt += 16
        ins.then_inc(s, 16)
        self._commit(reads, writes, id(s), sem_buf.dcnt, group)
        q_ = self.outst[q]
        q_.append((id(s), sem_buf.dcnt))
        if len(q_) > self.maxq[q]:
            k, v = q_.pop(0)
            self._wait(e, {k: v})
        return ins

    def barrier(self):
        deps = {}
        for e in self.engs.values():
            if e.cnt:
                deps[id(e.sem)] = e.cnt
        for b in self.all_bufs:
            for d in (b.w, b.r):
                for k, v in d.items():
                    if deps.get(k, 0) < v:
                        deps[k] = v
        for e in self.engs.values():
            self._wait(e, deps)

    def finish(self, bufs):
        e = self.engs["sp"]
        deps = {}
        for b in bufs:
            for d in (b.w, b.r):
                for k, v in d.items():
                    if deps.get(k, 0) < v:
                        deps[k] = v
        self._wait(e, deps)


def build_program(S, depth=DEPTH, debug=False):
    NT = S // 128
    NBLK = (2 * S) // BS + NE
    NSLOT = NBLK * BS
    assert NSLOT % 128 == 0
    nc = bass.Bass("TRN2", target_bir_lowering=False)

    def din(name, shape, dt=F32):
        return nc.dram_tensor(name, list(shape), dt, kind="ExternalInput")

    x_in = din("x", [S, D])
    c_in = din("c", [128, KC])
    coff_in = din("coff", [128, 1])
    w_ada = din("w_ada", [DEPTH, D, 6 * D])
    b_ada = din("b_ada", [DEPTH, 6 * D])
    n1g = din("norm1_g", [DEPTH, D])
    n2g = din("norm2_g", [DEPTH, D])
    ml_w_in = din("ml_w_in", [2, D, ML_IN])
    ml_b_gate = din("ml_b_gate", [2, 8])
    ml_g_out = din("ml_g_out", [2, D])
    ml_w_out = din("ml_w_out", [2, D, D])
    sw_w_in = din("sw_w_in", [2, D, SW_IN])
    sw_g_q = din("sw_g_q", [2, 64])
    sw_g_k = din("sw_g_k", [2, 64])
    sw_sinks = din("sw_sinks", [2, 16])
    sw_w_out = din("sw_w_out", [2, D, D])
    w_rt = din("w_rt", [DEPTH, D, 72])
    b_rt = din("b_rt", [DEPTH, 72])
    w1r = din("w1r", [DEPTH * NE * 128, KC * DE])
    w3r = din("w3r", [DEPTH * NE * 128, KC * DE])
    w2r = din("w2r", [DEPTH * NE * 128, 3 * D])
    out = nc.dram_tensor("out", [S, D], F32, kind="ExternalOutput")
    dbg = nc.dram_tensor("dbg", [depth * S, D], F32, kind="ExternalOutput") if debug else None

    with ExitStack() as st:
        K = Kern(nc, st)
        op, dma = K.op, K.dma
        x_in_b = Buf(K, "x_in", x_in, dram=True)
        out_b = Buf(K, "out", out, dram=True)
        dbg_b = Buf(K, "dbg", dbg, dram=True) if debug else None
        wsrc = Buf(K, "wsrc", None, dram=True)
        xs = K.dram("xs", [S, D], F32)
        hn2d = K.dram("hn2d", [S + 128, D], BF16)
        slot_tw = K.dram("slot_tw", [NSLOT, 2], F32)
        y_slots = K.dram("y_slots", [NSLOT, D], F32)

        identf = K.sbuf("identf", [128, 128], F32)
        identb = K.sbuf("identb", [128, 128], BF16)
        onesf = K.sbuf("onesf", [128, 128], F32)
        onesb = K.sbuf("onesb", [128, 128], BF16)
        triinc = K.sbuf("triinc", [128, 128], F32)
        tristrb = K.sbuf("tristrb", [128, 128], BF16)
        negm = K.sbuf("negm", [128, 128], F32)
        negcur = K.sbuf("negcur", [128, 4, 128], BF16)
        negprev = K.sbuf("negprev", [128, 4, 128], BF16)
        tmpc = K.sbuf("tmpc", [128, 512], F32)
        sel4 = [K.sbuf("sel4_%d" % h, [4, 128], F32) for h in range(4)]
        iota_p = K.sbuf("iota_p", [128, 1], F32)
        tokid = K.sbuf("tokid", [128, NT], F32)
        jb = K.sbuf("jb", [128, 16], F32)
        padinit = K.sbuf("padinit", [128, 16, 2], F32)
        zrow = K.sbuf("zrow", [128, 256], BF16)

        op("pool", lambda e: e.memset(onesf[:], 1.0), writes=[onesf])
        op("pool", lambda e: e.memset(onesb[:], 1.0), writes=[onesb])
        op("pool", lambda e: e.affine_select(out=identf[:], in_=onesf[:], pattern=[[-1, 128]], compare_op=ALU.is_equal,
                                             fill=0.0, base=0, channel_multiplier=1), reads=[onesf], writes=[identf])
        op("pool", lambda e: e.tensor_copy(out=identb[:], in_=identf[:]), reads=[identf], writes=[identb])
        op("pool", lambda e: e.affine_select(out=triinc[:], in_=onesf[:], pattern=[[1, 128]], compare_op=ALU.is_ge,
                                             fill=0.0, base=0, channel_multiplier=-1), reads=[onesf], writes=[triinc])
        op("pool", lambda e: e.affine_select(out=tmpc[:, 0:128], in_=onesf[:], pattern=[[1, 128]], compare_op=ALU.is_gt,
                                             fill=0.0, base=0, channel_multiplier=-1), reads=[onesf], writes=[tmpc])
        op("pool", lambda e: e.tensor_copy(out=tristrb[:], in_=tmpc[:, 0:128]), reads=[tmpc], writes=[tristrb])
        op("pool", lambda e: e.memset(tmpc[:], 0.0), reads=[tmpc], writes=[tmpc])
        op("pool", lambda e: e.affine_select(out=negm[:], in_=tmpc[:, 0:128], pattern=[[1, 128]], compare_op=ALU.is_ge,
                                             fill=NEG, base=0, channel_multiplier=-1), reads=[tmpc], writes=[negm])
        op("pool", lambda e: e.affine_select(out=negcur[:].rearrange("p h q -> p (h q)"), in_=tmpc[:], pattern=[[0, 4], [1, 128]],
                                             compare_op=ALU.is_ge, fill=NEG, base=0, channel_multiplier=-1),
           reads=[tmpc], writes=[negcur])
        op("pool", lambda e: e.affine_select(out=negprev[:].rearrange("p h q -> p (h q)"), in_=tmpc[:], pattern=[[0, 4], [-1, 128]],
                                             compare_op=ALU.is_gt, fill=NEG, base=0, channel_multiplier=1),
           reads=[tmpc], writes=[negprev])
        for h in range(4):
            op("pool", lambda e, h=h: e.affine_select(out=sel4[h][:], in_=onesf[0:4, :], pattern=[[0, 128]], compare_op=ALU.is_equal,
                                                      fill=0.0, base=-h, channel_multiplier=1), reads=[onesf], writes=[sel4[h]])
        op("pool", lambda e: e.iota(iota_p[:], pattern=[[0, 1]], base=0, channel_multiplier=1, allow_small_or_imprecise_dtypes=True),
           writes=[iota_p])
        op("pool", lambda e: e.iota(tokid[:], pattern=[[128, NT]], base=0, channel_multiplier=1, allow_small_or_imprecise_dtypes=True),
           writes=[tokid])
        op("pool", lambda e: e.iota(jb[:], pattern=[[BS, 16]], base=0, channel_multiplier=0, allow_small_or_imprecise_dtypes=True),
           writes=[jb])
        op("pool", lambda e: e.memset(padinit[:, :, 0:1], float(S)), writes=[padinit])
        op("pool", lambda e: e.memset(padinit[:, :, 1:2], 0.0), reads=[padinit], writes=[padinit])
        op("pool", lambda e: e.memset(zrow[:], 0.0), writes=[zrow])
        for q4 in range(4):
            dma("sp", lambda e, q4=q4: e.dma_start(out=hn2d[S:S + 128, q4 * 256:(q4 + 1) * 256], in_=zrow[:]), reads=[zrow], writes=[hn2d])

        mod6 = K.sbuf("mod6", [128, 6, D], F32)
        cond = K.sbuf("cond", [128, KC], F32)
        rowv = tmpc
        wrt = K.sbuf("wrt", [128, KC, 72], BF16)
        brb = K.sbuf("brb", [128, 72], F32)
        bgb = K.sbuf("bgb", [128, 8], F32)
        goutb = K.sbuf("goutb", [128, D], F32)
        gqb = K.sbuf("gqb", [128, 64], F32)
        gkb = K.sbuf("gkb", [128, 64], F32)
        esink = K.sbuf("esink", [128, 16], F32)
        ARENA = KC * (ML_IN + D)
        arena = st.enter_context(nc.sbuf_tensor("arena", [128, ARENA], BF16))
        wmix_in_ml = K.view("wmix_in_ml", arena[:, 0:KC * ML_IN].rearrange("p (k n) -> p k n", k=KC))
        wmix_out = K.view("wmix_out", arena[:, KC * ML_IN:ARENA].rearrange("p (k n) -> p k n", k=KC))
        wmix_in_sw = K.view("wmix_in_sw", arena[:, 0:KC * SW_IN].rearrange("p (k n) -> p k n", k=KC))
        stage = K.view("stage", arena[:, 0:2 * KC * 512].bitcast(F32).rearrange("p (k n) -> p k n", k=KC))
        condb = K.view("condb", arena[:, 2 * KC * 512:2 * KC * 512 + 2 * KC * 128].bitcast(F32).rearrange("p (k n) -> p k n", k=KC))
        o = 0
        def carve(name, n, shape_str=None, **kw):
            nonlocal o
            ap = arena[:, o:o + n]
            o += n
            if shape_str:
                ap = ap.rearrange(shape_str, **kw)
            return K.view(name, ap)
        w1b = [carve("w1b%d" % i, KC * DE, "p (k n) -> p k n", k=KC) for i in range(2)]
        w3b = [carve("w3b%d" % i, KC * DE, "p (k n) -> p k n", k=KC) for i in range(2)]
        w2b = [carve("w2b%d" % i, 3 * D, "p (k n) -> p k n", k=3) for i in range(2)]
        xgc = [[carve("xg%d_%d" % (c_, i), D) for i in range(2)] for c_ in range(2)]
        xTmc = [carve("xTm%d" % c_, KC * BS, "p (k n) -> p k n", k=KC) for c_ in range(2)]
        hTmc = [carve("hTm%d" % c_, 3 * BS, "p (k n) -> p k n", k=3) for c_ in range(2)]
        ybc = [K.view("ybc%d" % i, arena[:, i * 2 * D:(i + 1) * 2 * D].bitcast(F32)) for i in range(2)]
        assert o <= ARENA

        ps = [K.psum("ps%d" % i, [128, 512], F32) for i in range(6)]
        pt = [K.psum("pt%d" % i, [128, 1024], BF16) for i in range(2)]

        class WSet:
            pass
        WS = []
        for p_ in range(2):
            W = WSet()
            sfx = "_%d" % p_
            W.p = p_
            W.xt = K.sbuf("xt" + sfx, [128, D], F32)
            W.junk = K.sbuf("junk" + sfx, [128, D], BF16)
            W.tmpf = K.sbuf("tmpf" + sfx, [128, D], F32)
            W.hnb = K.sbuf("hnb" + sfx, [128, D], BF16)
            W.hnT = K.sbuf("hnT" + sfx, [128, KC, 128], BF16)
            W.sm = K.sbuf("sm" + sfx, [128, 64], F32)
            W.hg = K.sbuf("hg" + sfx, [128, D], BF16)
            W.hgT = K.sbuf("hgT" + sfx, [128, KC, 128], BF16)
            W.xt2 = K.sbuf("xt2" + sfx, [128, D], F32)
            W.gates = K.sbuf("gates" + sfx, [128, 40], F32)
            W.bT = K.sbuf("bT" + sfx, [4, 128], F32)
            W.ssq = K.sbuf("ssq" + sfx, [128, 32], F32)
            W.lg = K.sbuf("lg" + sfx, [128, 72], F32)
            W.rsm = K.sbuf("rsm" + sfx, [128, 64], F32)
            W.t64 = K.sbuf("t64" + sfx, [128, 64], F32)
            W.ohs = K.sbuf("ohs" + sfx, [128, 64], BF16)
            W.rtot = K.sbuf("rtot" + sfx, [128, 64], F32)
            W.go = K.sbuf("go" + sfx, [128, D], BF16)
            W.res = K.sbuf("res" + sfx, [128, 257], F32)
            W.tmpA = K.sbuf("tmpA" + sfx, [128, 257], F32)
            W.dT = K.sbuf("dT" + sfx, [128, 128], F32)
            MXN = 4 * 128 * 3 + 4 * 257 + 2 * 128
            mx = st.enter_context(nc.sbuf_tensor("mx" + sfx, [128, max(MXN, 16 * 128 + 256 + 1024)], BF16))
            o_ = 0
            def cv(name, n, rs=None, **kw):
                nonlocal o_
                ap = mx[:, o_:o_ + n]
                o_ += n
                if rs:
                    ap = ap.rearrange(rs, **kw)
                return K.view(name + sfx, ap)
            W.qT = cv("qT", 512, "p (h n) -> p h n", h=4)
            W.kT = cv("kT", 512, "p (h n) -> p h n", h=4)
            W.ktok = cv("ktok", 512)
            W.vp = cv("vp", 4 * 257, "p (h n) -> p h n", h=4)
            W.pT_ = cv("pT_", 128)
            W.kw_ = cv("kw_", 128)
            o_ = 0
            W.qTs = K.view("qTs" + sfx, mx[0:64, 0:2048].rearrange("p (h n) -> p h n", h=16))
            o_ = 2048
            W.kn = cv("kn", 256)
            W.pprev = cv("pprev", 512)
            W.pcur = cv("pcur", 512)
            W.qn = W.junk
            WS.append(W)
        c32 = K.sbuf("c32", [128, 4, 257], F32)
        cb = K.sbuf("cb", [128, 4, 257], BF16)
        kTs = [K.sbuf("kTs%d" % i, [64, 4, 128], BF16) for i in range(3)]
        vps = [K.sbuf("vps%d" % i, [128, 4, 65], BF16) for i in range(3)]
        OH1 = K.sbuf("OH1", [128, NT, 64], BF16)
        OH2 = K.sbuf("OH2", [128, NT, 64], BF16)
        R12 = K.sbuf("R12", [128, 2, NT], F32)
        GT = K.sbuf("GT", [128, 2, NT], F32)
        base = K.sbuf("base", [128, 64], F32)
        pp = K.sbuf("pp", [128, 4, 64], F32)
        ppi = K.sbuf("ppi", [128, 64], I32)
        DEST = K.sbuf("DEST", [128, 2, NT], F32)
        DESTi = K.sbuf("DESTi", [128, 2, NT], I32)
        SRC = K.sbuf("SRC", [128, 2, NT, 2], F32)
        BE = K.sbuf("BE", [128, NBLK], F32)
        BEi = K.sbuf("BEi", [128, NBLK], I32)
        stwc = [[K.sbuf("stw%d_%d" % (c_, i), [128, 2], F32) for i in range(2)] for c_ in range(2)]
        stokc = [[K.sbuf("stok%d_%d" % (c_, i), [128, 1], I32) for i in range(2)] for c_ in range(2)]
        sil = tmpc

        ya = WS[1].tmpf
        yb = WS[0].tmpf
        sm = WS[0].sm
        bigb = WS[1].xt
        big = bigb[:, 0:1024].rearrange("p (a b) -> p a b", b=64)
        xt = WS[0].xt
        xt2 = WS[0].xt2

        def rmsnorm_mod(W, src, a_idx, b_idx, dst_bf):
            op("act", lambda e: e.activation(out=W.junk[:], in_=src[:], func=AF.Square, accum_out=W.sm[:, 0:1]),
               reads=[src], writes=[W.junk, W.sm])
            op("dve", lambda e: e.tensor_scalar(out=W.sm[:, 1:2], in0=W.sm[:, 0:1], scalar1=1.0 / D, scalar2=EPS, op0=ALU.mult, op1=ALU.add),
               reads=[W.sm], writes=[W.sm])
            op("act", lambda e: e.activation(out=W.sm[:, 2:3], in_=W.sm[:, 1:2], func=AF.Sqrt), reads=[W.sm], writes=[W.sm])
            op("dve", lambda e: e.reciprocal(out=W.sm[:, 3:4], in_=W.sm[:, 2:3]), reads=[W.sm], writes=[W.sm])
            op("dve", lambda e: e.scalar_tensor_tensor(out=W.tmpf[:], in0=src[:], scalar=W.sm[:, 3:4], in1=mod6[:, a_idx, :],
                                                       op0=ALU.mult, op1=ALU.mult), reads=[src, W.sm, mod6], writes=[W.tmpf])
            op("pool", lambda e: e.tensor_tensor(out=dst_bf[:], in0=W.tmpf[:], in1=mod6[:, b_idx, :], op=ALU.add),
               reads=[W.tmpf, mod6], writes=[dst_bf])

        def transpose8(src_bf, dstT, pbank):
            K.hold += 1
            for kc in range(KC):
                op("pe", lambda e, kc=kc: e.transpose(out=pbank[:, kc * 128:(kc + 1) * 128], in_=src_bf[:, kc * 128:(kc + 1) * 128],
                                                      identity=identb[:]), reads=[src_bf, identb], writes=[pbank], inc=(kc == KC - 1))
            K.hold -= 1
            op("act", lambda e: e.copy(out=dstT[:].rearrange("p k n -> p (k n)"), in_=pbank[:]), reads=[pbank], writes=[dstT])

        def mm_group(pbuf, out_ap, pairs, reads):
            n = len(pairs)
            K.hold += 1
            for i, (l, r) in enumerate(pairs):
                op("pe", lambda e, l=l, r=r, i=i: e.matmul(out_ap, lhsT=l, rhs=r, start=(i == 0), stop=(i == n - 1)),
                   reads=reads, writes=[pbuf], inc=(i == n - 1))
            K.hold -= 1

        def bcast_row(dst_ap, dst_buf, src_dram_ap, n):
            dma("sp", lambda e: e.dma_start(out=dst_ap, in_=src_dram_ap.partition_broadcast(128)), reads=[wsrc], writes=[dst_buf])

        dma("sp", lambda e: e.dma_start(out=cond[:], in_=c_in.ap()), reads=[wsrc], writes=[cond])
        op("act", lambda e: e.activation(out=cond[:], in_=cond[:], func=AF.Silu), reads=[cond], writes=[cond])

        x_src = x_in_b
        x_src_t = x_in
        for layer in range(depth):
            j = layer // 2
            is_ml = (layer % 2 == 0)
            last = (layer == depth - 1)
            for kc in range(KC):
                op("dve", lambda e, kc=kc: e.tensor_scalar(out=condb[:, kc, :], in0=onesf[:], scalar1=cond[:, kc:kc + 1], scalar2=None,
                                                           op0=ALU.mult), reads=[onesf, cond], writes=[condb])
            for ncol in range(12):
                dma("sp", lambda e, ncol=ncol: e.dma_start(out=rowv[0:1, :], in_=b_ada[layer:layer + 1, ncol * 512:(ncol + 1) * 512]), reads=[wsrc], writes=[rowv])
                dma("sp", lambda e, ncol=ncol: e.dma_start(
                    out=stage[:], in_=w_ada[layer].rearrange("(kc p) n -> p kc n", p=128)[:, :, ncol * 512:(ncol + 1) * 512]),
                    reads=[wsrc], writes=[stage])
                pb = ps[ncol % 2]
                pairs = [(condb[:, kc, :], stage[:, kc, :]) for kc in range(KC)]
                pairs.append((onesf[0:1, :], rowv[0:1, :]))
                mm_group(pb, pb[:], pairs, [condb, stage, onesf, rowv])
                op("dve", lambda e, ncol=ncol, pb=pb: e.tensor_copy(out=mod6[:, ncol // 2, (ncol % 2) * 512:(ncol % 2 + 1) * 512], in_=pb[:]),
                   reads=[pb], writes=[mod6])
            for (gsrc, idx) in ((n1g, 1), (n2g, 4)):
                bcast_row(WS[0].tmpf[:], WS[0].tmpf, gsrc[layer, :], D)
                op("dve", lambda e, idx=idx: e.scalar_tensor_tensor(out=mod6[:, idx, :], in0=mod6[:, idx, :], scalar=1.0, in1=WS[0].tmpf[:],
                                                                    op0=ALU.add, op1=ALU.mult), reads=[mod6, WS[0].tmpf], writes=[mod6])
            dma("pool", lambda e: e.dma_start(out=wrt[:], in_=w_rt[layer].rearrange("(kc p) n -> p kc n", p=128)), reads=[wsrc], writes=[wrt])
            bcast_row(brb[:], brb, b_rt[layer, :], 72)
            K.barrier()
            if is_ml:
                for kc in range(KC):
                    dma("pool", lambda e, kc=kc: e.dma_start(out=wmix_in_ml[:, kc, :], in_=ml_w_in[j, kc * 128:(kc + 1) * 128, :]),
                        reads=[wsrc], writes=[wmix_in_ml])
                    dma("pool", lambda e, kc=kc: e.dma_start(out=wmix_out[:, kc, :], in_=ml_w_out[j, kc * 128:(kc + 1) * 128, :]),
                        reads=[wsrc], writes=[wmix_out])
                bcast_row(bgb[:], bgb, ml_b_gate[j, :], 8)
                bcast_row(goutb[:], goutb, ml_g_out[j, :], D)
                op("dve", lambda e: e.memset(c32[:], 0.0), writes=[c32])
                op("dve", lambda e: e.memset(cb[:], 0.0), writes=[cb])
                for W_ in WS:
                    op("dve", lambda e, W_=W_: e.memset(W_.vp[:], 1.0), writes=[W_.vp])
                win = wmix_in_ml
            else:
                for kc in range(KC):
                    dma("pool", lambda e, kc=kc: e.dma_start(out=wmix_in_sw[:, kc, :], in_=sw_w_in[j, kc * 128:(kc + 1) * 128, :]),
                        reads=[wsrc], writes=[wmix_in_sw])
                    dma("pool", lambda e, kc=kc: e.dma_start(out=wmix_out[:, kc, :], in_=sw_w_out[j, kc * 128:(kc + 1) * 128, :]),
                        reads=[wsrc], writes=[wmix_out])
                bcast_row(gqb[:], gqb, sw_g_q[j, :], 64)
                bcast_row(gkb[:], gkb, sw_g_k[j, :], 64)
                bcast_row(esink[:], esink, sw_sinks[j, :], 16)
                op("act", lambda e: e.activation(out=esink[:], in_=esink[:], func=AF.Exp), reads=[esink], writes=[esink])
                for i in range(3):
                    op("dve", lambda e, i=i: e.memset(kTs[i][:], 0.0), writes=[kTs[i]])
                    op("dve", lambda e, i=i: e.memset(vps[i][:], 0.0), writes=[vps[i]])
                win = wmix_in_sw
            op("dve", lambda e: e.memset(base[:], 0.0), writes=[base])

            real_ps, real_pt = ps, pt

            class _PS:
                def __init__(self, p_):
                    self.p_ = p_
                def __getitem__(self, i):
                    return real_ps[3 * self.p_ + (i % 3)]

            class _PT:
                def __init__(self, p_):
                    self.p_ = p_
                def __getitem__(self, i):
                    return real_pt[self.p_]

            def tile_body(t, ps, pt, W):
                rows = slice(t * 128, (t + 1) * 128)
                dma("sp", lambda e: e.dma_start(out=W.xt[:], in_=x_src_t[rows, :]), reads=[x_src], writes=[W.xt])
                rmsnorm_mod(W, W.xt, 1, 0, W.hnb)
                transpose8(W.hnb, W.hnT, pt[0])
                if is_ml:
                    for (dst, coff, scl) in ((W.qT, 0, 1.0), (W.kT, 512, 128 ** -0.5)):
                        pb = ps[0] if coff == 0 else ps[1]
                        for h in range(4):
                            mm_group(pb, pb[:, h * 128:(h + 1) * 128],
                                     [(win[:, kc, coff + h * 128:coff + (h + 1) * 128], W.hnT[:, kc, :]) for kc in range(KC)], [win, W.hnT])
                        op("act", lambda e, dst=dst, pb=pb, scl=scl: e.mul(out=dst[:].rearrange("p h n -> p (h n)"), in_=pb[:], mul=scl),
                           reads=[pb], writes=[dst])
                    mm_group(ps[2], ps[2][:], [(W.hnT[:, kc, :], win[:, kc, 512:1024]) for kc in range(KC)], [win, W.hnT])
                    op("act", lambda e: e.mul(out=W.ktok[:], in_=ps[2][:], mul=128 ** -0.5), reads=[ps[2]], writes=[W.ktok])
                    for half in range(2):
                        pb = ps[3 + half]
                        mm_group(pb, pb[:], [(W.hnT[:, kc, :], win[:, kc, 1024 + half * 512:1024 + (half + 1) * 512]) for kc in range(KC)], [win, W.hnT])
                        op("dve", lambda e, pb=pb, half=half: e.tensor_copy(out=W.vp[:, 2 * half:2 * half + 2, 0:256],
                                                                            in_=pb[:].rearrange("p (h n) -> p h n", h=2)),
                           reads=[pb], writes=[W.vp])
                    for half in range(2):
                        pb = ps[(5 + half) % 6]
                        mm_group(pb, pb[:], [(W.hnT[:, kc, :], win[:, kc, 2048 + half * 512:2048 + (half + 1) * 512]) for kc in range(KC)], [win, W.hnT])
                        op("act", lambda e, pb=pb, half=half: e.activation(out=W.go[:, half * 512:(half + 1) * 512], in_=pb[:], func=AF.Sigmoid),
                           reads=[pb], writes=[W.go])
                    op("pool", lambda e: e.tensor_tensor(out=W.go[:], in0=W.go[:], in1=goutb[:], op=ALU.mult), reads=[W.go, goutb], writes=[W.go])
                    mm_group(ps[1], ps[1][:, 0:8], [(W.hnT[:, kc, :], win[:, kc, 3072:3080]) for kc in range(KC)], [win, W.hnT])
                    G = W.gates
                    op("dve", lambda e: e.tensor_tensor(out=G[:, 0:8], in0=ps[1][:, 0:8], in1=bgb[:], op=ALU.add), reads=[ps[1], bgb], writes=[G])
                    op("act", lambda e: e.activation(out=G[:, 0:8], in_=G[:, 0:8], func=AF.Tanh, scale=1.0 / 15.0), reads=[G], writes=[G])
                    op("dve", lambda e: e.tensor_scalar(out=G[:, 0:8], in0=G[:, 0:8], scalar1=15.0, scalar2=None, op0=ALU.mult), reads=[G], writes=[G])
                    op("act", lambda e: e.activation(out=G[:, 8:12], in_=G[:, 4:8], func=AF.Exp, scale=-1.0), reads=[G], writes=[G])
                    op("act", lambda e: e.activation(out=G[:, 8:12], in_=G[:, 8:12], func=AF.Ln, bias=1.0), reads=[G], writes=[G])
                    op("dve", lambda e: e.tensor_scalar(out=G[:, 8:12], in0=G[:, 8:12], scalar1=-1.0, scalar2=None, op0=ALU.mult), reads=[G], writes=[G])
                    mm_group(ps[0], ps[0][:, 0:4], [(triinc[:], G[:, 8:12])], [triinc, G])
                    mm_group(ps[0], ps[0][:, 4:8], [(onesf[:], G[:, 8:12])], [onesf, G])
                    op("dve", lambda e: e.tensor_copy(out=G[:, 12:16], in_=ps[0][:, 0:4]), reads=[ps[0]], writes=[G])
                    op("dve", lambda e: e.tensor_copy(out=G[:, 20:24], in_=ps[0][:, 4:8]), reads=[ps[0]], writes=[G])
                    op("dve", lambda e: e.tensor_tensor(out=G[:, 16:20], in0=G[:, 0:4], in1=G[:, 12:16], op=ALU.subtract), reads=[G], writes=[G])
                    op("dve", lambda e: e.tensor_tensor(out=G[:, 24:28], in0=G[:, 16:20], in1=G[:, 20:24], op=ALU.add), reads=[G], writes=[G])
                    op("act", lambda e: e.activation(out=G[:, 24:28], in_=G[:, 24:28], func=AF.Exp), reads=[G], writes=[G])
                    op("act", lambda e: e.activation(out=G[:, 28:32], in_=G[:, 20:24], func=AF.Exp), reads=[G], writes=[G])
                    op("act", lambda e: e.activation(out=G[:, 32:36], in_=G[:, 12:16], func=AF.Exp), reads=[G], writes=[G])
                    op("pe", lambda e: e.transpose(out=ps[0][0:4, 128:256], in_=G[:, 12:16], identity=identf[:]), reads=[G, identf], writes=[ps[0]])
                    op("dve", lambda e: e.tensor_copy(out=W.bT[:], in_=ps[0][0:4, 128:256]), reads=[ps[0]], writes=[W.bT])
                    for h in range(4):
                        mm_group(ps[1], ps[1][:, 0:128], [(sel4[h][:], W.bT[:]), (identf[:], negm[:])], [sel4[h], W.bT, identf, negm])
                        op("act", lambda e, h=h: e.activation(out=W.dT[:], in_=ps[1][:, 0:128], func=AF.Exp, bias=G[:, 16 + h:17 + h]),
                           reads=[ps[1], G], writes=[W.dT])
                        mm_group(ps[2], ps[2][:, 0:128], [(W.kT[:, h, :], W.qT[:, h, :])], [W.kT, W.qT])
                        op("dve", lambda e: e.tensor_tensor(out=W.pT_[:], in0=ps[2][:, 0:128], in1=W.dT[:], op=ALU.mult), reads=[ps[2], W.dT], writes=[W.pT_])
                        mm_group(ps[3], ps[3][:, 0:257], [(W.pT_[:], W.vp[:, h, :])], [W.pT_, W.vp])
                        if W.p == 1:
                            K.wait_for(("st", h))
                        mm_group(ps[4], ps[4][:, 0:257], [(W.qT[:, h, :], cb[:, h, :])], [W.qT, cb])
                        op("act", lambda e, h=h: e.activation(out=W.tmpA[:], in_=ps[4][:, 0:257], func=AF.Copy, scale=G[:, 32 + h:33 + h]),
                           reads=[ps[4], G], writes=[W.tmpA])
                        op("dve", lambda e: e.tensor_tensor(out=W.res[:], in0=W.tmpA[:], in1=ps[3][:, 0:257], op=ALU.add), reads=[W.tmpA, ps[3]], writes=[W.res])
                        op("pool", lambda e, h=h: e.tensor_scalar(out=W.kw_[:], in0=W.ktok[:, h * 128:(h + 1) * 128], scalar1=G[:, 24 + h:25 + h], scalar2=None,
                                                                  op0=ALU.mult), reads=[W.ktok, G], writes=[W.kw_])
                        mm_group(ps[5], ps[5][:, 0:257], [(W.kw_[:], W.vp[:, h, :])], [W.kw_, W.vp])
                        op("dve", lambda e, h=h: e.scalar_tensor_tensor(out=c32[:, h, :], in0=c32[:, h, :], scalar=G[:, 28 + h:29 + h], in1=ps[5][:, 0:257],
                                                                        op0=ALU.mult, op1=ALU.add), reads=[c32, G, ps[5]], writes=[c32])
                        op("pool", lambda e, h=h: e.tensor_copy(out=cb[:, h, :], in_=c32[:, h, :]), reads=[c32], writes=[cb])
                        if W.p == 0:
                            K.signal(("st", h))
                        S_ = W.sm
                        op("dve", lambda e: e.tensor_scalar(out=S_[:, 7:8], in0=W.res[:, 256:257], scalar1=-1.0, scalar2=None, op0=ALU.mult),
                           reads=[W.res], writes=[S_])
                        op("dve", lambda e: e.tensor_tensor(out=S_[:, 8:9], in0=S_[:, 7:8], in1=W.res[:, 256:257], op=ALU.max), reads=[W.res, S_], writes=[S_])
                        op("dve", lambda e: e.tensor_scalar(out=S_[:, 8:9], in0=S_[:, 8:9], scalar1=1.0, scalar2=None, op0=ALU.max), reads=[S_], writes=[S_])
                        op("dve", lambda e: e.reciprocal(out=S_[:, 9:10], in_=S_[:, 8:9]), reads=[S_], writes=[S_])
                        op("act", lambda e: e.activation(out=W.junk[:, 0:256], in_=W.res[:, 0:256], func=AF.Square, accum_out=S_[:, 10:11]),
                           reads=[W.res], writes=[W.junk, S_])
                        op("dve", lambda e: e.tensor_tensor(out=S_[:, 11:12], in0=S_[:, 9:10], in1=S_[:, 9:10], op=ALU.mult), reads=[S_], writes=[S_])
                        op("dve", lambda e: e.scalar_tensor_tensor(out=S_[:, 12:13], in0=S_[:, 10:11], scalar=1.0 / 256.0, in1=S_[:, 11:12],
                                                                   op0=ALU.mult, op1=ALU.mult), reads=[S_], writes=[S_])
                        op("dve", lambda e: e.tensor_scalar(out=S_[:, 12:13], in0=S_[:, 12:13], scalar1=EPS, scalar2=None, op0=ALU.add), reads=[S_], writes=[S_])
                        op("act", lambda e: e.activation(out=S_[:, 13:14], in_=S_[:, 12:13], func=AF.Sqrt), reads=[S_], writes=[S_])
                        op("dve", lambda e: e.reciprocal(out=S_[:, 14:15], in_=S_[:, 13:14]), reads=[S_], writes=[S_])
                        op("dve", lambda e: e.tensor_tensor(out=S_[:, 15:16], in0=S_[:, 14:15], in1=S_[:, 9:10], op=ALU.mult), reads=[S_], writes=[S_])
                        op("dve", lambda e, h=h: e.scalar_tensor_tensor(out=W.hg[:, h * 256:(h + 1) * 256], in0=W.res[:, 0:256], scalar=S_[:, 15:16],
                                                                        in1=W.go[:, h * 256:(h + 1) * 256], op0=ALU.mult, op1=ALU.mult),
                           reads=[W.res, S_, W.go], writes=[W.hg])
                else:
                    cur, prv = t % 3, (t + 2) % 3
                    for half in range(2):
                        mm_group(ps[half], ps[half][:], [(W.hnT[:, kc, :], win[:, kc, half * 512:(half + 1) * 512]) for kc in range(KC)], [win, W.hnT])
                    mm_group(ps[2], ps[2][:], [(W.hnT[:, kc, :], win[:, kc, 1024:1536]) for kc in range(KC)], [win, W.hnT])
                    for half in range(2):
                        op("act", lambda e, half=half: e.activation(out=W.tmpf[:, half * 512:(half + 1) * 512], in_=ps[half][:], func=AF.Square),
                           reads=[ps[half]], writes=[W.tmpf])
                    op("dve", lambda e: e.tensor_reduce(out=W.ssq[:, 0:16], in_=W.tmpf[:].rearrange("p (h d) -> p h d", d=64), axis=AX.X, op=ALU.add),
                       reads=[W.tmpf], writes=[W.ssq])
                    op("act", lambda e: e.activation(out=W.xt2[:, 0:256], in_=ps[2][:, 0:256], func=AF.Square), reads=[ps[2]], writes=[W.xt2])
                    op("dve", lambda e: e.tensor_reduce(out=W.ssq[:, 16:20], in_=W.xt2[:, 0:256].rearrange("p (h d) -> p h d", d=64), axis=AX.X, op=ALU.add),
                       reads=[W.xt2], writes=[W.ssq])
                    op("dve", lambda e: e.tensor_scalar(out=W.ssq[:, 0:20], in0=W.ssq[:, 0:20], scalar1=1.0 / 64.0, scalar2=EPS, op0=ALU.mult, op1=ALU.add),
                       reads=[W.ssq], writes=[W.ssq])
                    op("act", lambda e: e.activation(out=W.ssq[:, 0:20], in_=W.ssq[:, 0:20], func=AF.Sqrt), reads=[W.ssq], writes=[W.ssq])
                    op("dve", lambda e: e.reciprocal(out=W.ssq[:, 0:20], in_=W.ssq[:, 0:20]), reads=[W.ssq], writes=[W.ssq])
                    for half in range(2):
                        op("dve", lambda e, half=half: e.tensor_tensor(
                            out=W.tmpf[:, half * 512:(half + 1) * 512].rearrange("p (h d) -> p h d", d=64),
                            in0=ps[half][:].rearrange("p (h d) -> p h d", d=64),
                            in1=W.ssq[:, half * 8:(half + 1) * 8].unsqueeze(2).broadcast_to([128, 8, 64]), op=ALU.mult),
                            reads=[ps[half], W.ssq], writes=[W.tmpf])
                    op("pool", lambda e: e.tensor_tensor(out=W.qn[:].rearrange("p (h d) -> p h d", d=64), in0=W.tmpf[:].rearrange("p (h d) -> p h d", d=64),
                                                         in1=gqb[:].unsqueeze(1).broadcast_to([128, 16, 64]), op=ALU.mult),
                       reads=[W.tmpf, gqb], writes=[W.qn])
                    op("dve", lambda e: e.tensor_tensor(out=W.xt2[:, 0:256].rearrange("p (h d) -> p h d", d=64),
                                                        in0=ps[2][:, 0:256].rearrange("p (h d) -> p h d", d=64),
                                                        in1=W.ssq[:, 16:20].unsqueeze(2).broadcast_to([128, 4, 64]), op=ALU.mult),
                       reads=[ps[2], W.ssq], writes=[W.xt2])
                    op("pool", lambda e: e.tensor_tensor(out=W.kn[:].rearrange("p (h d) -> p h d", d=64), in0=W.xt2[:, 0:256].rearrange("p (h d) -> p h d", d=64),
                                                         in1=gkb[:].unsqueeze(1).broadcast_to([128, 4, 64]), op=ALU.mult),
                       reads=[W.xt2, gkb], writes=[W.kn])
                    op("act", lambda e: e.copy(out=vps[cur][:, :, 0:64], in_=ps[2][:, 256:512].rearrange("p (h d) -> p h d", d=64)),
                       reads=[ps[2]], writes=[vps[cur]])
                    op("pool", lambda e: e.memset(vps[cur][:, :, 64:65], 1.0), reads=[vps[cur]], writes=[vps[cur]])
                    for half in range(2):
                        pb = pt[0]
                        K.hold += 1
                        for h8 in range(8):
                            hh = half * 8 + h8
                            op("pe", lambda e, hh=hh, h8=h8, pb=pb: e.transpose(out=pb[0:64, h8 * 128:(h8 + 1) * 128], in_=W.qn[:, hh * 64:(hh + 1) * 64],
                                                                                identity=identb[:]), reads=[W.qn, identb], writes=[pb], inc=(h8 == 7))
                        K.hold -= 1
                        op("act", lambda e, half=half, pb=pb: e.copy(out=W.qTs[:, half * 8:(half + 1) * 8, :].rearrange("p h n -> p (h n)"), in_=pb[0:64, :]),
                           reads=[pb], writes=[W.qTs])
                    K.hold += 1
                    for g in range(4):
                        op("pe", lambda e, g=g: e.transpose(out=pt[0][0:64, g * 128:(g + 1) * 128], in_=W.kn[:, g * 64:(g + 1) * 64], identity=identb[:]),
                           reads=[W.kn, identb], writes=[pt[0]], inc=(g == 3))
                    K.hold -= 1
                    op("act", lambda e: e.copy(out=kTs[cur][:].rearrange("p h n -> p (h n)"), in_=pt[0][0:64, 0:512]), reads=[pt[0]], writes=[kTs[cur]])
                    if W.p == 0:
                        K.signal("kv")
                    else:
                        K.wait_for("kv")
                    for g in range(4):
                        qv = W.qTs[:, 4 * g:4 * g + 4, :].rearrange("p h n -> p (h n)")
                        mm_group(ps[3], ps[3][:], [(kTs[prv][:, g, :], qv), (identb[:], negprev[:].rearrange("p h q -> p (h q)"))],
                                 [kTs[prv], W.qTs, identb, negprev])
                        mm_group(ps[4], ps[4][:], [(kTs[cur][:, g, :], qv), (identb[:], negcur[:].rearrange("p h q -> p (h q)"))],
                                 [kTs[cur], W.qTs, identb, negcur])
                        op("act", lambda e: e.activation(out=W.pprev[:], in_=ps[3][:], func=AF.Exp, scale=0.125), reads=[ps[3]], writes=[W.pprev])
                        op("act", lambda e: e.activation(out=W.pcur[:], in_=ps[4][:], func=AF.Exp, scale=0.125), reads=[ps[4]], writes=[W.pcur])
                        for hh in range(4):
                            mm_group(ps[5], ps[5][:, hh * 65:(hh + 1) * 65],
                                     [(W.pprev[:, hh * 128:(hh + 1) * 128], vps[prv][:, g, :]), (W.pcur[:, hh * 128:(hh + 1) * 128], vps[cur][:, g, :])],
                                     [W.pprev, W.pcur, vps[prv], vps[cur]])
                        pv = ps[5][:, 0:260].rearrange("p (h d) -> p h d", d=65)
                        op("dve", lambda e, g=g, pv=pv: e.tensor_tensor(out=W.ssq[:, 20:24], in0=pv[:, :, 64], in1=esink[:, 4 * g:4 * g + 4], op=ALU.add),
                           reads=[ps[5], esink], writes=[W.ssq])
                        op("dve", lambda e: e.reciprocal(out=W.ssq[:, 24:28], in_=W.ssq[:, 20:24]), reads=[W.ssq], writes=[W.ssq])
                        op("dve", lambda e, g=g, pv=pv: e.tensor_tensor(out=W.hg[:, g * 256:(g + 1) * 256].rearrange("p (h d) -> p h d", d=64), in0=pv[:, :, 0:64],
                                                                        in1=W.ssq[:, 24:28].unsqueeze(2).broadcast_to([128, 4, 64]), op=ALU.mult),
                           reads=[ps[5], W.ssq], writes=[W.hg])
                transpose8(W.hg, W.hgT, pt[1])
                for half in range(2):
                    pb = ps[half]
                    mm_group(pb, pb[:], [(W.hgT[:, kc, :], wmix_out[:, kc, half * 512:(half + 1) * 512]) for kc in range(KC)], [W.hgT, wmix_out])
                    op("dve", lambda e, pb=pb, half=half: e.tensor_tensor(out=W.tmpf[:, half * 512:(half + 1) * 512], in0=pb[:],
                                                                          in1=mod6[:, 2, half * 512:(half + 1) * 512], op=ALU.mult),
                       reads=[pb, mod6], writes=[W.tmpf])
                op("pool", lambda e: e.tensor_tensor(out=W.xt2[:], in0=W.tmpf[:], in1=W.xt[:], op=ALU.add), reads=[W.tmpf, W.xt], writes=[W.xt2])
                dma("sp", lambda e: e.dma_start(out=xs[rows, :], in_=W.xt2[:]), reads=[W.xt2], writes=[xs])
                rmsnorm_mod(W, W.xt2, 4, 3, W.hnb)
                dma("act", lambda e: e.dma_start(out=hn2d[rows, :], in_=W.hnb[:]), reads=[W.hnb], writes=[hn2d], group=("hn2", layer))
                transpose8(W.hnb, W.hnT, pt[0])
                mm_group(ps[2], ps[2][:, 0:72], [(W.hnT[:, kc, :], wrt[:, kc, :]) for kc in range(KC)], [W.hnT, wrt])
                op("dve", lambda e: e.tensor_tensor(out=W.lg[:], in0=ps[2][:, 0:72], in1=brb[:], op=ALU.add), reads=[ps[2], brb], writes=[W.lg])
                R = W.rsm
                op("dve", lambda e: e.tensor_reduce(out=R[:, 0:1], in_=W.lg[:, 0:8], axis=AX.X, op=ALU.max), reads=[W.lg], writes=[R])
                op("dve", lambda e: e.tensor_scalar(out=R[:, 1:2], in0=R[:, 0:1], scalar1=-1.0, scalar2=None, op0=ALU.mult), reads=[R], writes=[R])
                op("dve", lambda e: e.tensor_scalar(out=R[:, 4:12], in0=W.lg[:, 0:8], scalar1=R[:, 0:1], scalar2=None, op0=ALU.is_equal), reads=[W.lg, R], writes=[R])
                op("act", lambda e: e.activation(out=R[:, 48:56], in_=W.lg[:, 0:8], func=AF.Exp, bias=R[:, 1:2], accum_out=R[:, 2:3]), reads=[W.lg, R], writes=[R])
                op("dve", lambda e: e.reciprocal(out=R[:, 3:4], in_=R[:, 2:3]), reads=[R], writes=[R])
                op("dve", lambda e: e.tensor_tensor(out=W.t64[:].rearrange("p (g x) -> p g x", g=8), in0=W.lg[:, 8:72].rearrange("p (g x) -> p g x", g=8),
                                                    in1=R[:, 4:12].unsqueeze(2).broadcast_to([128, 8, 8]), op=ALU.mult), reads=[W.lg, R], writes=[W.t64])
                op("dve", lambda e: e.tensor_reduce(out=R[:, 12:20], in_=W.t64[:].rearrange("p (g x) -> p x g", g=8), axis=AX.X, op=ALU.add),
                   reads=[W.t64], writes=[R])
                op("dve", lambda e: e.max(out=R[:, 20:28], in_=R[:, 12:20]), reads=[R], writes=[R])
                op("dve", lambda e: e.tensor_scalar(out=R[:, 28:36], in0=R[:, 12:20], scalar1=R[:, 20:21], scalar2=None, op0=ALU.is_equal), reads=[R], writes=[R])
                op("dve", lambda e: e.tensor_scalar(out=R[:, 36:44], in0=R[:, 12:20], scalar1=R[:, 21:22], scalar2=None, op0=ALU.is_equal), reads=[R], writes=[R])
                op("dve", lambda e: e.tensor_tensor(out=R[:, 44:45], in0=R[:, 20:21], in1=R[:, 21:22], op=ALU.subtract), reads=[R], writes=[R])
                op("act", lambda e: e.activation(out=R[:, 45:46], in_=R[:, 44:45], func=AF.Sigmoid), reads=[R], writes=[R])
                op("dve", lambda e: e.tensor_tensor(out=GT[:, 0, t:t + 1], in0=R[:, 45:46], in1=R[:, 3:4], op=ALU.mult), reads=[R], writes=[GT])
                op("dve", lambda e: e.tensor_tensor(out=GT[:, 1, t:t + 1], in0=R[:, 3:4], in1=GT[:, 0, t:t + 1], op=ALU.subtract), reads=[R, GT], writes=[GT])
                for (OH, c0) in ((OH1, 28), (OH2, 36)):
                    op("dve", lambda e, OH=OH, c0=c0: e.tensor_tensor(out=OH[:, t, :].rearrange("p (g x) -> p g x", g=8),
                                                                      in0=R[:, 4:12].unsqueeze(2).broadcast_to([128, 8, 8]),
                                                                      in1=R[:, c0:c0 + 8].unsqueeze(1).broadcast_to([128, 8, 8]), op=ALU.mult),
                       reads=[R], writes=[OH])
                op("dve", lambda e: e.tensor_tensor(out=W.ohs[:], in0=OH1[:, t, :], in1=OH2[:, t, :], op=ALU.add), reads=[OH1, OH2], writes=[W.ohs])
                mm_group(ps[3], ps[3][:, 0:64], [(tristrb[:], W.ohs[:])], [tristrb, W.ohs])
                mm_group(ps[3], ps[3][:, 64:128], [(onesb[:], W.ohs[:])], [onesb, W.ohs])
                if W.p == 1:
                    K.wait_for("base")
                op("dve", lambda e: e.tensor_tensor(out=W.rtot[:], in0=ps[3][:, 0:64], in1=base[:], op=ALU.add), reads=[ps[3], base], writes=[W.rtot])
                op("dve", lambda e: e.tensor_tensor(out=base[:], in0=ps[3][:, 64:128], in1=base[:], op=ALU.add), reads=[ps[3], base], writes=[base])
                if W.p == 0:
                    K.signal("base")
                for k_, OH in ((0, OH1), (1, OH2)):
                    op("dve", lambda e, OH=OH: e.tensor_tensor(out=W.t64[:], in0=OH[:, t, :], in1=W.rtot[:], op=ALU.mult), reads=[OH, W.rtot], writes=[W.t64])
                    op("dve", lambda e, k_=k_: e.tensor_reduce(out=R12[:, k_, t:t + 1], in_=W.t64[:], axis=AX.X, op=ALU.add), reads=[W.t64], writes=[R12])

            for t0_ in range(0, NT, 2):
                K.interleave([lambda t=t0_ + q_: tile_body(t, _PS(t % 2), _PT(t % 2), WS[t % 2]) for q_ in range(min(2, NT - t0_))])

            op("dve", lambda e: e.tensor_scalar(out=pp[:, 0, :], in0=base[:], scalar1=1.0 / BS, scalar2=(BS / 2 - 0.5) / BS, op0=ALU.mult, op1=ALU.add),
               reads=[base], writes=[pp])
            op("dve", lambda e: e.tensor_copy(out=ppi[:], in_=pp[:, 0, :]), reads=[pp], writes=[ppi])
            op("dve", lambda e: e.tensor_copy(out=pp[:, 0, :], in_=ppi[:]), reads=[ppi], writes=[pp])
            op("dve", lambda e: e.tensor_scalar(out=pp[:, 0, :], in0=pp[:, 0, :], scalar1=float(BS), scalar2=None, op0=ALU.mult), reads=[pp], writes=[pp])
            op("dve", lambda e: e.tensor_tensor_scan(out=pp[:, 1, :], data0=onesf[:, 0:64], data1=pp[:, 0, :], initial=0.0, op0=ALU.mult, op1=ALU.add),
               reads=[pp, onesf], writes=[pp])
            op("dve", lambda e: e.tensor_tensor(out=pp[:, 2, :], in0=pp[:, 1, :], in1=pp[:, 0, :], op=ALU.subtract), reads=[pp], writes=[pp])
            for k_, OH in ((0, OH1), (1, OH2)):
                for c0 in range(0, NT, 16):
                    n = min(16, NT - c0)
                    op("dve", lambda e, OH=OH, c0=c0, n=n: e.tensor_tensor(out=big[:, 0:n, :], in0=OH[:, c0:c0 + n, :],
                                                                           in1=pp[:, 2, :].unsqueeze(1).broadcast_to([128, n, 64]), op=ALU.mult),
                       reads=[OH, pp], writes=[bigb])
                    op("dve", lambda e, k_=k_, c0=c0, n=n: e.tensor_reduce(out=DEST[:, k_, c0:c0 + n], in_=big[:, 0:n, :], axis=AX.X, op=ALU.add),
                       reads=[bigb], writes=[DEST])
            op("dve", lambda e: e.tensor_tensor(out=DEST[:], in0=DEST[:], in1=R12[:], op=ALU.add), reads=[DEST, R12], writes=[DEST])
            op("dve", lambda e: e.tensor_copy(out=DESTi[:], in_=DEST[:]), reads=[DEST], writes=[DESTi])
            for k_ in range(2):
                op("dve", lambda e, k_=k_: e.tensor_copy(out=SRC[:, k_, :, 0], in_=tokid[:]), reads=[tokid], writes=[SRC])
                op("dve", lambda e, k_=k_: e.tensor_copy(out=SRC[:, k_, :, 1], in_=GT[:, k_, :]), reads=[GT], writes=[SRC])
            for c0 in range(0, NBLK, 16):
                n = min(16, NBLK - c0)
                op("dve", lambda e, c0=c0, n=n: e.tensor_scalar(out=sm[:, 16:16 + n], in0=jb[:, 0:n], scalar1=float(c0 * BS), scalar2=None, op0=ALU.add),
                   reads=[jb], writes=[sm])
                op("dve", lambda e, n=n: e.tensor_tensor(out=big[:, 0:n, :], in0=pp[:, 1, :].unsqueeze(1).broadcast_to([128, n, 64]),
                                                         in1=sm[:, 16:16 + n].unsqueeze(2).broadcast_to([128, n, 64]), op=ALU.is_le),
                   reads=[pp, sm], writes=[bigb])
                op("dve", lambda e, c0=c0, n=n: e.tensor_reduce(out=BE[:, c0:c0 + n], in_=big[:, 0:n, :], axis=AX.X, op=ALU.add), reads=[bigb], writes=[BE])
            op("dve", lambda e: e.tensor_scalar(out=BE[:], in0=BE[:], scalar1=float(NE - 1), scalar2=128.0, op0=ALU.min, op1=ALU.mult), reads=[BE], writes=[BE])
            op("dve", lambda e: e.tensor_scalar(out=BE[:], in0=BE[:], scalar1=iota_p[:, 0:1], scalar2=float(layer * NE * 128), op0=ALU.add, op1=ALU.add),
               reads=[BE, iota_p], writes=[BE])
            op("dve", lambda e: e.tensor_copy(out=BEi[:], in_=BE[:]), reads=[BE], writes=[BEi])
            NR_ = NSLOT // 128
            for r0_ in range(0, NR_, 16):
                dma("sp", lambda e, r0_=r0_: e.dma_start(out=slot_tw.ap().rearrange("(p r) c -> p r c", p=128)[:, r0_:r0_ + 16, :], in_=padinit[:]),
                    reads=[padinit], writes=[slot_tw])
            for k_ in range(2):
                for t in range(NT):
                    dma("pool", lambda e, k_=k_, t=t: e.indirect_dma_start(
                        out=slot_tw[:, :], out_offset=bass.IndirectOffsetOnAxis(ap=DESTi[:, k_, t:t + 1], axis=0),
                        in_=SRC[:, k_, t, :], in_offset=None), reads=[SRC, DESTi], writes=[slot_tw], sem_buf=SRC)

            K.barrier()

            def expert_block(jblk, ps, pt, c_):
                pb_ = jblk % 2
                xg, xTm, hTm, stw, stok = xgc[c_], xTmc[c_], hTmc[c_], stwc[c_], stokc[c_]
                yo = [WS[c_].xt, WS[c_].xt2]
                sil = WS[c_].tmpA
                for (wb, wr_) in ((w1b[pb_], w1r), (w3b[pb_], w3r), (w2b[pb_], w2r)):
                    dma("pool", lambda e, wb=wb, wr_=wr_: e.indirect_dma_start(
                        out=wb[:].rearrange("p k n -> p (k n)"), out_offset=None, in_=wr_[:, :],
                        in_offset=bass.IndirectOffsetOnAxis(ap=BEi[:, jblk:jblk + 1], axis=0)), reads=[wsrc, BEi], writes=[wb])
                for sub in range(BS // 128):
                    r0 = jblk * BS + sub * 128
                    dma("sp", lambda e, sub=sub, r0=r0: e.dma_start(out=stw[sub][:], in_=slot_tw[r0:r0 + 128, :]), reads=[slot_tw], writes=[stw[sub]])
                    op("dve", lambda e, sub=sub: e.tensor_copy(out=stok[sub][:], in_=stw[sub][:, 0:1]), reads=[stw[sub]], writes=[stok[sub]])
                    dma("pool", lambda e, sub=sub: e.indirect_dma_start(
                        out=xg[sub][:], out_offset=None, in_=hn2d[:, :],
                        in_offset=bass.IndirectOffsetOnAxis(ap=stok[sub][:, 0:1], axis=0)), reads=[hn2d, stok[sub]], writes=[xg[sub]])
                    K.hold += 1
                    for kc in range(KC):
                        op("pe", lambda e, kc=kc, sub=sub: e.transpose(out=pt[0][:, kc * 128:(kc + 1) * 128], in_=xg[sub][:, kc * 128:(kc + 1) * 128],
                                                                       identity=identb[:]), reads=[xg[sub], identb], writes=[pt[0]], inc=(kc == KC - 1))
                    K.hold -= 1
                    op("act", lambda e, sub=sub: e.copy(out=xTm[:, :, sub * 128:(sub + 1) * 128], in_=pt[0][:].rearrange("p (k n) -> p k n", k=KC)),
                       reads=[pt[0]], writes=[xTm])
                for m in range(3):
                    p1, p3 = ps[(2 * m) % 3], ps[(2 * m + 1) % 3]
                    mm_group(p1, p1[:, 0:BS], [(w1b[pb_][:, kc, m * 128:(m + 1) * 128], xTm[:, kc, :]) for kc in range(KC)], [w1b[pb_], xTm])
                    mm_group(p3, p3[:, 0:BS], [(w3b[pb_][:, kc, m * 128:(m + 1) * 128], xTm[:, kc, :]) for kc in range(KC)], [w3b[pb_], xTm])
                    op("act", lambda e, p1=p1: e.activation(out=sil[:, 0:BS], in_=p1[:, 0:BS], func=AF.Silu), reads=[p1], writes=[sil])
                    op("dve", lambda e, p3=p3, m=m: e.tensor_tensor(out=hTm[:, m, :], in0=p3[:, 0:BS], in1=sil[:, 0:BS], op=ALU.mult), reads=[p3, sil], writes=[hTm])
                for sub in range(BS // 128):
                    r0 = jblk * BS + sub * 128
                    for half in range(2):
                        pb = ps[(2 * sub + half) % 3]
                        mm_group(pb, pb[:], [(hTm[:, c, sub * 128:(sub + 1) * 128], w2b[pb_][:, c, half * 512:(half + 1) * 512]) for c in range(3)],
                                 [hTm, w2b[pb_]])
                        op("act", lambda e, pb=pb, sub=sub, half=half: e.activation(out=yo[sub][:, half * 512:(half + 1) * 512], in_=pb[:], func=AF.Copy,
                                                                                    scale=stw[sub][:, 1:2]), reads=[pb, stw[sub]], writes=[yo[sub]])
                    dma("sp", lambda e, sub=sub, r0=r0: e.dma_start(out=y_slots[r0:r0 + 128, :], in_=yo[sub][:]), reads=[yo[sub]], writes=[y_slots],
                        group=("ys", layer))

            for j0_ in range(0, NBLK, 2):
                K.interleave([lambda j=j0_ + q_: expert_block(j, _PS(j % 2), _PT(j % 2), j % 2) for q_ in range(min(2, NBLK - j0_))])

            K.barrier()
            dst_t = out if last else xs.t
            dst_b = out_b if last else xs

            def combine_tile(t, c_):
                rows = slice(t * 128, (t + 1) * 128)
                xt, xt2, ya, yb = WS[c_].xt, WS[c_].xt2, WS[c_].tmpf, ybc[c_]
                dma("sp", lambda e: e.dma_start(out=xt[:], in_=xs[rows, :]), reads=[xs], writes=[xt])
                dma("pool", lambda e, t=t: e.indirect_dma_start(out=ya[:], out_offset=None, in_=y_slots[:, :],
                                                               in_offset=bass.IndirectOffsetOnAxis(ap=DESTi[:, 0, t:t + 1], axis=0)),
                    reads=[y_slots, DESTi], writes=[ya])
                dma("pool", lambda e, t=t: e.indirect_dma_start(out=yb[:], out_offset=None, in_=y_slots[:, :],
                                                               in_offset=bass.IndirectOffsetOnAxis(ap=DESTi[:, 1, t:t + 1], axis=0)),
                    reads=[y_slots, DESTi], writes=[yb])
                op("dve", lambda e: e.tensor_tensor(out=ya[:], in0=ya[:], in1=yb[:], op=ALU.add), reads=[ya, yb], writes=[ya])
                op("pool", lambda e: e.tensor_tensor(out=ya[:], in0=ya[:], in1=mod6[:, 5, :], op=ALU.mult), reads=[ya, mod6], writes=[ya])
                op("dve", lambda e: e.tensor_tensor(out=xt2[:], in0=ya[:], in1=xt[:], op=ALU.add), reads=[ya, xt], writes=[xt2])
                if last:
                    dma("sp", lambda e: e.dma_start(out=dst_t[rows, :], in_=xt2[:]), reads=[xt2], writes=[dst_b], group=("out", layer))
                else:
                    dma("sp", lambda e: e.dma_start(out=dst_t[rows, :], in_=xt2[:]), reads=[xt2], writes=[dst_b])
                if debug:
                    dma("sp", lambda e: e.dma_start(out=dbg[layer * S + t * 128:layer * S + (t + 1) * 128, :], in_=xt2[:]), reads=[xt2], writes=[dbg_b])

            for t0_ in range(0, NT, 2):
                K.interleave([lambda t=t0_ + q_: combine_tile(t, t % 2) for q_ in range(min(2, NT - t0_))])
            x_src = xs
            x_src_t = xs.t
            K.barrier()
        K.finish([out_b] + ([dbg_b] if debug else []))
        K.barrier()
        ninst = K.ninst
    return nc, ninst


def prep_weights(inp):
    f = lambda a: np.ascontiguousarray(np.asarray(a, dtype=np.float32))
    w = {}
    for k in ("w_ada", "b_ada", "norm1_g", "norm2_g", "ml_w_in", "ml_b_gate", "ml_g_out", "ml_w_out",
              "sw_w_in", "sw_g_q", "sw_g_k", "sw_sinks", "sw_w_out"):
        w[k] = f(inp[k])
    w["w_rt"] = f(np.concatenate([np.asarray(inp["moe_w_group"]), np.asarray(inp["moe_w_router"])], axis=-1))
    w["b_rt"] = f(np.concatenate([np.asarray(inp["moe_b_group"]), np.asarray(inp["moe_b_router"])], axis=-1))
    w1 = np.asarray(inp["moe_w1"]).reshape(DEPTH, NE, KC, 128, DE)
    w["w1r"] = f(w1.transpose(0, 1, 3, 2, 4).reshape(DEPTH * NE * 128, KC * DE))
    w3 = np.asarray(inp["moe_w3"]).reshape(DEPTH, NE, KC, 128, DE)
    w["w3r"] = f(w3.transpose(0, 1, 3, 2, 4).reshape(DEPTH * NE * 128, KC * DE))
    w2 = np.asarray(inp["moe_w2"]).reshape(DEPTH, NE, 3, 128, D)
    w["w2r"] = f(w2.transpose(0, 1, 3, 2, 4).reshape(DEPTH * NE * 128, 3 * D))
    return w


def run(inp, S, depth=DEPTH, trace=False, debug=False):
    x = np.asarray(inp["x"], dtype=np.float32)
    c = np.asarray(inp["c"], dtype=np.float32)
    B = x.shape[0]
    w = prep_weights(inp)
    nc, ninst = build_program(S, depth, debug)
    in_maps = []
    for core in range(8):
        b = (core // 2) % B
        m = dict(w)
        m["x"] = np.ascontiguousarray(x[b, :S])
        m["c"] = np.ascontiguousarray(c[b].reshape(KC, 128).T)
        m["coff"] = np.full((128, 1), 0.0 if core % 2 == 0 else 1.0e7, dtype=np.float32)
        in_maps.append(m)
    res = run_bass_kernel_spmd(nc, in_maps, core_ids=list(range(8)), **({"trace": True} if trace else {}))
    outs = np.stack([res.results[2 * b]["out"] for b in range(B)], axis=0)
    return outs, res


def kernel(**inputs):
    S = np.asarray(inputs["x"]).shape[1]
    outs, _ = run(inputs, S, DEPTH)
    return outs.astype(np.float32)
```

```python
import math
import threading
from contextlib import ExitStack

import numpy as np
import concourse.bass as bass
import concourse.mybir as mybir
from concourse.bass_utils import run_bass_kernel_spmd

F32 = mybir.dt.float32
BF16 = mybir.dt.bfloat16
I32 = mybir.dt.int32
AF = mybir.ActivationFunctionType
ALU = mybir.AluOpType
AX = mybir.AxisListType

D = 1024
KC = 8
DEPTH = 4
EPS = 1e-6
ML_IN = 3080
SW_IN = 1536
NE = 64
DE = 384
BS = 256
NEG = -30000.0


class Buf:
    def __init__(self, K, name, t, dram=False):
        self.K = K
        self.name = name
        self.t = t
        self.dram = dram
        self.w = {}
        self.r = {}
        self.dsem = None
        self.dcnt = 0
        self.wgroup = None
        K.all_bufs.append(self)

    def __getitem__(self, k):
        return self.t[k]

    def ap(self):
        return self.t.ap() if self.dram else self.t[:]


class Eng:
    def __init__(self, name, h, sem):
        self.name = name
        self.h = h
        self.sem = sem
        self.cnt = 0
        self.waited = {}


class Kern:
    def __init__(self, nc, stack):
        self.nc = nc
        self.stack = stack
        self.sems = {}
        self.engs = {}
        self.all_bufs = []
        for nm, h in (("pe", nc.tensor), ("act", nc.scalar), ("dve", nc.vector),
                      ("pool", nc.gpsimd), ("sp", nc.sync)):
            s = stack.enter_context(nc.semaphore("sem_" + nm))
            self.sems[id(s)] = s
            self.engs[nm] = Eng(nm, h, s)
        self.dsem_pool = []
        self.ninst = 0
        self.outst = {"sp": [], "act": [], "pool": []}
        self.hold = 0
        self.il = None
        self.maxq = {"sp": 12, "act": 12, "pool": 8}

    def sbuf(self, name, shape, dt):
        t = self.stack.enter_context(self.nc.sbuf_tensor(name, list(shape), dt))
        return Buf(self, name, t)

    def psum(self, name, shape, dt=F32):
        t = self.stack.enter_context(self.nc.psum_tensor(name, list(shape), dt))
        return Buf(self, name, t)

    def dram(self, name, shape, dt, kind="Internal"):
        t = self.nc.dram_tensor(name, list(shape), dt, kind=kind)
        return Buf(self, name, t, dram=True)

    def view(self, name, ap):
        return Buf(self, name, ap)

    def _dsem(self, b):
        if b.dsem is None:
            s = self.stack.enter_context(self.nc.semaphore("ds_" + b.name))
            self.sems[id(s)] = s
            b.dsem = s
        return b.dsem

    def _deps(self, reads, writes, group=None):
        deps = {}
        for b in reads:
            for k, v in b.w.items():
                if deps.get(k, 0) < v:
                    deps[k] = v
        for b in writes:
            same = (group is not None and b.wgroup == group)
            for d in ((b.r,) if same else (b.w, b.r)):
                for k, v in d.items():
                    if deps.get(k, 0) < v:
                        deps[k] = v
        return deps

    def _wait(self, e, deps, skip_self=False):
        for k, v in deps.items():
            if skip_self and k == id(e.sem):
                continue
            if e.waited.get(k, 0) < v:
                e.h.wait_ge(self.sems[k], v)
                e.waited[k] = v
                self.ninst += 1

    def _commit(self, reads, writes, k, v, group=None):
        for b in reads:
            if b.r.get(k, 0) < v:
                b.r[k] = v
        for b in writes:
            if group is not None and b.wgroup == group:
                if b.w.get(k, 0) < v:
                    b.w[k] = v
            else:
                b.w = {k: v}
            b.wgroup = group
            b.r = {}

    def interleave(self, fns, credit=3):
        if len(fns) == 1:
            fns[0]()
            return
        il = {"turn": 0, "alive": [True] * len(fns), "ids": {}, "err": None, "credit": credit, "left": credit,
              "cv": threading.Condition(), "sig": set()}
        self.il = il

        def nxt(i):
            n = len(fns)
            for d in range(1, n):
                j = (i + d) % n
                if il["alive"][j]:
                    il["turn"] = j
                    il["left"] = il["credit"]
                    return
            il["left"] = il["credit"]

        il["nxt"] = nxt

        def worker(i, fn):
            il["ids"][threading.get_ident()] = i
            with il["cv"]:
                while il["turn"] != i:
                    il["cv"].wait()
            try:
                fn()
            except BaseException as ex:
                il["err"] = ex
            finally:
                with il["cv"]:
                    il["alive"][i] = False
                    nxt(i)
                    il["cv"].notify_all()

        ths = [threading.Thread(target=worker, args=(i, f)) for i, f in enumerate(fns)]
        for th in ths:
            th.start()
        for th in ths:
            th.join()
        self.il = None
        if il["err"] is not None:
            raise il["err"]

    def signal(self, key):
        il = self.il
        if il is None:
            return
        with il["cv"]:
            il["sig"].add(key)

    def wait_for(self, key):
        il = self.il
        if il is None:
            return
        i = il["ids"].get(threading.get_ident())
        assert self.hold == 0
        with il["cv"]:
            while key not in il["sig"]:
                assert any(a for j, a in enumerate(il["alive"]) if j != i), ("deadlock waiting for", key)
                il["nxt"](i)
                il["cv"].notify_all()
                while il["turn"] != i:
                    il["cv"].wait()

    def point(self):
        il = self.il
        if il is None or self.hold > 0:
            return
        i = il["ids"].get(threading.get_ident())
        if i is None:
            return
        with il["cv"]:
            il["left"] -= 1
            if il["left"] <= 0:
                il["nxt"](i)
                il["cv"].notify_all()
                while il["turn"] != i:
                    il["cv"].wait()

    def op(self, eng, fn, reads=(), writes=(), inc=True):
        self.point()
        e = self.engs[eng]
        self._wait(e, self._deps(reads, writes), skip_self=(eng == "pe"))
        ins = fn(e.h)
        self.ninst += 1
        if inc:
            e.cnt += 1
            ins.then_inc(e.sem, 1)
            v = e.cnt
        else:
            v = e.cnt + 1
        self._commit(reads, writes, id(e.sem), v)
        return ins

    def dma(self, q, fn, reads=(), writes=(), sem_buf=None, group=None):
        self.point()
        e = self.engs[q]
        self._wait(e, self._deps(reads, writes, group))
        if sem_buf is None:
            cand = [b for b in list(writes) + list(reads) if not b.dram]
            sem_buf = cand[0] if cand else (list(writes) + list(reads))[0]
        s = self._dsem(sem_buf)
        ins = fn(e.h)
        self.ninst += 1
        sem_buf.dcnt += 16
        ins.then_inc(s, 16)
        self._commit(reads, writes, id(s), sem_buf.dcnt, group)
        q_ = self.outst[q]
        q_.append((id(s), sem_buf.dcnt))
        if len(q_) > self.maxq[q]:
            k, v = q_.pop(0)
            self._wait(e, {k: v})
        return ins

    def barrier(self):
        deps = {}
        for e in self.engs.values():
            if e.cnt:
                deps[id(e.sem)] = e.cnt
        for b in self.all_bufs:
            for d in (b.w, b.r):
                for k, v in d.items():
                    if deps.get(k, 0) < v:
                        deps[k] = v
        for e in self.engs.values():
            self._wait(e, deps)

    def finish(self, bufs):
        e = self.engs["sp"]
        deps = {}
        for b in bufs:
            for d in (b.w, b.r):
                for k, v in d.items():
                    if deps.get(k, 0) < v:
                        deps[k] = v
        self._wait(e, deps)


def build_program(S, depth=DEPTH, debug=False):
    NT = S // 128
    NBLK = (2 * S) // BS + NE
    NSLOT = NBLK * BS
    assert NSLOT % 128 == 0
    nc = bass.Bass("TRN2", target_bir_lowering=False)

    def din(name, shape, dt=F32):
        return nc.dram_tensor(name, list(shape), dt, kind="ExternalInput")

    x_in = din("x", [S, D])
    c_in = din("c", [128, KC])
    coff_in = din("coff", [128, 1])
    w_ada = din("w_ada", [DEPTH, D, 6 * D])
    b_ada = din("b_ada", [DEPTH, 6 * D])
    n1g = din("norm1_g", [DEPTH, D])
    n2g = din("norm2_g", [DEPTH, D])
    ml_w_in = din("ml_w_in", [2, D, ML_IN])
    ml_b_gate = din("ml_b_gate", [2, 8])
    ml_g_out = din("ml_g_out", [2, D])
    ml_w_out = din("ml_w_out", [2, D, D])
    sw_w_in = din("sw_w_in", [2, D, SW_IN])
    sw_g_q = din("sw_g_q", [2, 64])
    sw_g_k = din("sw_g_k", [2, 64])
    sw_sinks = din("sw_sinks", [2, 16])
    sw_w_out = din("sw_w_out", [2, D, D])
    w_rt = din("w_rt", [DEPTH, D, 72])
    b_rt = din("b_rt", [DEPTH, 72])
    w1r = din("w1r", [DEPTH * NE * 128, KC * DE])
    w3r = din("w3r", [DEPTH * NE * 128, KC * DE])
    w2r = din("w2r", [DEPTH * NE * 128, 3 * D])
    out = nc.dram_tensor("out", [S, D], F32, kind="ExternalOutput")
    dbg = nc.dram_tensor("dbg", [depth * S, D], F32, kind="ExternalOutput") if debug else None

    with ExitStack() as st:
        K = Kern(nc, st)
        op, dma = K.op, K.dma
        x_in_b = Buf(K, "x_in", x_in, dram=True)
        out_b = Buf(K, "out", out, dram=True)
        dbg_b = Buf(K, "dbg", dbg, dram=True) if debug else None
        wsrc = Buf(K, "wsrc", None, dram=True)
        xs = K.dram("xs", [S, D], F32)
        hn2d = K.dram("hn2d", [S + 128, D], BF16)
        slot_tw = K.dram("slot_tw", [NSLOT, 2], F32)
        y_slots = K.dram("y_slots", [NSLOT, D], F32)

        identf = K.sbuf("identf", [128, 128], F32)
        identb = K.sbuf("identb", [128, 128], BF16)
        onesf = K.sbuf("onesf", [128, 128], F32)
        onesb = K.sbuf("onesb", [128, 128], BF16)
        triinc = K.sbuf("triinc", [128, 128], F32)
        tristrb = K.sbuf("tristrb", [128, 128], BF16)
        negm = K.sbuf("negm", [128, 128], F32)
        negcur = K.sbuf("negcur", [128, 4, 128], BF16)
        negprev = K.sbuf("negprev", [128, 4, 128], BF16)
        tmpc = K.sbuf("tmpc", [128, 512], F32)
        sel4 = [K.sbuf("sel4_%d" % h, [4, 128], F32) for h in range(4)]
        iota_p = K.sbuf("iota_p", [128, 1], F32)
        tokid = K.sbuf("tokid", [128, NT], F32)
        jb = K.sbuf("jb", [128, 16], F32)
        padinit = K.sbuf("padinit", [128, 16, 2], F32)
        zrow = K.sbuf("zrow", [128, 256], BF16)

        op("pool", lambda e: e.memset(onesf[:], 1.0), writes=[onesf])
        op("pool", lambda e: e.memset(onesb[:], 1.0), writes=[onesb])
        op("pool", lambda e: e.affine_select(out=identf[:], in_=onesf[:], pattern=[[-1, 128]], compare_op=ALU.is_equal,
                                             fill=0.0, base=0, channel_multiplier=1), reads=[onesf], writes=[identf])
        op("pool", lambda e: e.tensor_copy(out=identb[:], in_=identf[:]), reads=[identf], writes=[identb])
        op("pool", lambda e: e.affine_select(out=triinc[:], in_=onesf[:], pattern=[[1, 128]], compare_op=ALU.is_ge,
                                             fill=0.0, base=0, channel_multiplier=-1), reads=[onesf], writes=[triinc])
        op("pool", lambda e: e.affine_select(out=tmpc[:, 0:128], in_=onesf[:], pattern=[[1, 128]], compare_op=ALU.is_gt,
                                             fill=0.0, base=0, channel_multiplier=-1), reads=[onesf], writes=[tmpc])
        op("pool", lambda e: e.tensor_copy(out=tristrb[:], in_=tmpc[:, 0:128]), reads=[tmpc], writes=[tristrb])
        op("pool", lambda e: e.memset(tmpc[:], 0.0), reads=[tmpc], writes=[tmpc])
        op("pool", lambda e: e.affine_select(out=negm[:], in_=tmpc[:, 0:128], pattern=[[1, 128]], compare_op=ALU.is_ge,
                                             fill=NEG, base=0, channel_multiplier=-1), reads=[tmpc], writes=[negm])
        op("pool", lambda e: e.affine_select(out=negcur[:].rearrange("p h q -> p (h q)"), in_=tmpc[:], pattern=[[0, 4], [1, 128]],
                                             compare_op=ALU.is_ge, fill=NEG, base=0, channel_multiplier=-1),
           reads=[tmpc], writes=[negcur])
        op("pool", lambda e: e.affine_select(out=negprev[:].rearrange("p h q -> p (h q)"), in_=tmpc[:], pattern=[[0, 4], [-1, 128]],
                                             compare_op=ALU.is_gt, fill=NEG, base=0, channel_multiplier=1),
           reads=[tmpc], writes=[negprev])
        for h in range(4):
            op("pool", lambda e, h=h: e.affine_select(out=sel4[h][:], in_=onesf[0:4, :], pattern=[[0, 128]], compare_op=ALU.is_equal,
                                                      fill=0.0, base=-h, channel_multiplier=1), reads=[onesf], writes=[sel4[h]])
        op("pool", lambda e: e.iota(iota_p[:], pattern=[[0, 1]], base=0, channel_multiplier=1, allow_small_or_imprecise_dtypes=True),
           writes=[iota_p])
        op("pool", lambda e: e.iota(tokid[:], pattern=[[128, NT]], base=0, channel_multiplier=1, allow_small_or_imprecise_dtypes=True),
           writes=[tokid])
        op("pool", lambda e: e.iota(jb[:], pattern=[[BS, 16]], base=0, channel_multiplier=0, allow_small_or_imprecise_dtypes=True),
           writes=[jb])
        op("pool", lambda e: e.memset(padinit[:, :, 0:1], float(S)), writes=[padinit])
        op("pool", lambda e: e.memset(padinit[:, :, 1:2], 0.0), reads=[padinit], writes=[padinit])
        op("pool", lambda e: e.memset(zrow[:], 0.0), writes=[zrow])
        for q4 in range(4):
            dma("sp", lambda e, q4=q4: e.dma_start(out=hn2d[S:S + 128, q4 * 256:(q4 + 1) * 256], in_=zrow[:]), reads=[zrow], writes=[hn2d])

        mod6 = K.sbuf("mod6", [128, 6, D], F32)
        cond = K.sbuf("cond", [128, KC], F32)
        rowv = tmpc
        wrt = K.sbuf("wrt", [128, KC, 72], BF16)
        brb = K.sbuf("brb", [128, 72], F32)
        bgb = K.sbuf("bgb", [128, 8], F32)
        goutb = K.sbuf("goutb", [128, D], F32)
        gqb = K.sbuf("gqb", [128, 64], F32)
        gkb = K.sbuf("gkb", [128, 64], F32)
        esink = K.sbuf("esink", [128, 16], F32)
        ARENA = KC * (ML_IN + D)
        arena = st.enter_context(nc.sbuf_tensor("arena", [128, ARENA], BF16))
        wmix_in_ml = K.view("wmix_in_ml", arena[:, 0:KC * ML_IN].rearrange("p (k n) -> p k n", k=KC))
        wmix_out = K.view("wmix_out", arena[:, KC * ML_IN:ARENA].rearrange("p (k n) -> p k n", k=KC))
        wmix_in_sw = K.view("wmix_in_sw", arena[:, 0:KC * SW_IN].rearrange("p (k n) -> p k n", k=KC))
        stage = K.view("stage", arena[:, 0:2 * KC * 512].bitcast(F32).rearrange("p (k n) -> p k n", k=KC))
        condb = K.view("condb", arena[:, 2 * KC * 512:2 * KC * 512 + 2 * KC * 128].bitcast(F32).rearrange("p (k n) -> p k n", k=KC))
        o = 0
        def carve(name, n, shape_str=None, **kw):
            nonlocal o
            ap = arena[:, o:o + n]
            o += n
            if shape_str:
                ap = ap.rearrange(shape_str, **kw)
            return K.view(name, ap)
        w1b = [carve("w1b%d" % i, KC * DE, "p (k n) -> p k n", k=KC) for i in range(2)]
        w3b = [carve("w3b%d" % i, KC * DE, "p (k n) -> p k n", k=KC) for i in range(2)]
        w2b = [carve("w2b%d" % i, 3 * D, "p (k n) -> p k n", k=3) for i in range(3)]
        xgc = [[carve("xg%d_%d" % (c_, i), D) for i in range(2)] for c_ in range(2)]
        xTmc = [carve("xTm%d" % c_, KC * BS, "p (k n) -> p k n", k=KC) for c_ in range(2)]
        hTmc = [carve("hTm%d" % c_, 3 * BS, "p (k n) -> p k n", k=3) for c_ in range(2)]
        ybc = [K.view("ybc%d" % i, arena[:, i * 2 * D:(i + 1) * 2 * D].bitcast(F32)) for i in range(2)]
        assert o <= ARENA

        ps = [K.psum("ps%d" % i, [128, 512], F32) for i in range(6)]
        pt = [K.psum("pt%d" % i, [128, 1024], BF16) for i in range(2)]

        class WSet:
            pass
        WS = []
        for p_ in range(2):
            W = WSet()
            sfx = "_%d" % p_
            W.p = p_
            W.xt = K.sbuf("xt" + sfx, [128, D], F32)
            W.junk = K.sbuf("junk" + sfx, [128, D], BF16)
            W.tmpf = K.sbuf("tmpf" + sfx, [128, D], F32)
            W.hnb = K.sbuf("hnb" + sfx, [128, D], BF16)
            W.hnT = K.sbuf("hnT" + sfx, [128, KC, 128], BF16)
            W.sm = K.sbuf("sm" + sfx, [128, 64], F32)
            W.hg = K.sbuf("hg" + sfx, [128, D], BF16)
            W.hgT = K.sbuf("hgT" + sfx, [128, KC, 128], BF16)
            W.xt2 = K.sbuf("xt2" + sfx, [128, D], F32)
            W.gates = K.sbuf("gates" + sfx, [128, 40], F32)
            W.bT = K.sbuf("bT" + sfx, [4, 128], F32)
            W.ssq = K.sbuf("ssq" + sfx, [128, 32], F32)
            W.lg = K.sbuf("lg" + sfx, [128, 72], F32)
            W.rsm = K.sbuf("rsm" + sfx, [128, 64], F32)
            W.t64 = K.sbuf("t64" + sfx, [128, 64], F32)
            W.ohs = K.sbuf("ohs" + sfx, [128, 64], BF16)
            W.rtot = K.sbuf("rtot" + sfx, [128, 64], F32)
            W.go = K.sbuf("go" + sfx, [128, D], BF16)
            W.res = K.sbuf("res" + sfx, [128, 257], F32)
            W.tmpA = K.sbuf("tmpA" + sfx, [128, 257], F32)
            W.dT = K.sbuf("dT" + sfx, [128, 128], F32)
            MXN = 4 * 128 * 3 + 4 * 257 + 2 * 128
            mx = st.enter_context(nc.sbuf_tensor("mx" + sfx, [128, max(MXN, 16 * 128 + 256 + 1024)], BF16))
            o_ = 0
            def cv(name, n, rs=None, **kw):
                nonlocal o_
                ap = mx[:, o_:o_ + n]
                o_ += n
                if rs:
                    ap = ap.rearrange(rs, **kw)
                return K.view(name + sfx, ap)
            W.qT = cv("qT", 512, "p (h n) -> p h n", h=4)
            W.kT = cv("kT", 512, "p (h n) -> p h n", h=4)
            W.ktok = cv("ktok", 512)
            W.vp = cv("vp", 4 * 257, "p (h n) -> p h n", h=4)
            W.pT_ = cv("pT_", 128)
            W.kw_ = cv("kw_", 128)
            o_ = 0
            W.qTs = K.view("qTs" + sfx, mx[0:64, 0:2048].rearrange("p (h n) -> p h n", h=16))
            o_ = 2048
            W.kn = cv("kn", 256)
            W.pprev = cv("pprev", 512)
            W.pcur = cv("pcur", 512)
            W.qn = W.junk
            WS.append(W)
        c32 = K.sbuf("c32", [128, 4, 257], F32)
        cb = K.sbuf("cb", [128, 4, 257], BF16)
        kTs = [K.sbuf("kTs%d" % i, [64, 4, 128], BF16) for i in range(3)]
        vps = [K.sbuf("vps%d" % i, [128, 4, 65], BF16) for i in range(3)]
        OH1 = K.sbuf("OH1", [128, NT, 64], BF16)
        OH2 = K.sbuf("OH2", [128, NT, 64], BF16)
        if NT * 64 >= KC * DE:
            w1b.append(K.view("w1b2", OH1.t[:].rearrange("p a b -> p (a b)")[:, 0:KC * DE].rearrange("p (k n) -> p k n", k=KC)))
            w3b.append(K.view("w3b2", OH2.t[:].rearrange("p a b -> p (a b)")[:, 0:KC * DE].rearrange("p (k n) -> p k n", k=KC)))
        else:
            w1b.append(K.sbuf("w1b2", [128, KC, DE], BF16))
            w3b.append(K.sbuf("w3b2", [128, KC, DE], BF16))
        R12 = K.sbuf("R12", [128, 2, NT], F32)
        GT = K.sbuf("GT", [128, 2, NT], F32)
        base = K.sbuf("base", [128, 64], F32)
        pp = K.sbuf("pp", [128, 4, 64], F32)
        ppi = K.sbuf("ppi", [128, 64], I32)
        DEST = K.sbuf("DEST", [128, 2, NT], F32)
        DESTi = K.sbuf("DESTi", [128, 2, NT], I32)
        SRC = K.sbuf("SRC", [128, 2, NT, 2], F32)
        BE = K.sbuf("BE", [128, NBLK], F32)
        BEi = K.sbuf("BEi", [128, NBLK], I32)
        stwc = [[K.sbuf("stw%d_%d" % (c_, i), [128, 2], F32) for i in range(2)] for c_ in range(2)]
        stokc = [[K.sbuf("stok%d_%d" % (c_, i), [128, 1], I32) for i in range(2)] for c_ in range(2)]
        sil = tmpc

        ya = WS[1].tmpf
        yb = WS[0].tmpf
        sm = WS[0].sm
        bigb = WS[1].xt
        big = bigb[:, 0:1024].rearrange("p (a b) -> p a b", b=64)
        xt = WS[0].xt
        xt2 = WS[0].xt2

        def rmsnorm_mod(W, src, a_idx, b_idx, dst_bf):
            op("act", lambda e: e.activation(out=W.junk[:], in_=src[:], func=AF.Square, accum_out=W.sm[:, 0:1]),
               reads=[src], writes=[W.junk, W.sm])
            op("dve", lambda e: e.tensor_scalar(out=W.sm[:, 1:2], in0=W.sm[:, 0:1], scalar1=1.0 / D, scalar2=EPS, op0=ALU.mult, op1=ALU.add),
               reads=[W.sm], writes=[W.sm])
            op("act", lambda e: e.activation(out=W.sm[:, 2:3], in_=W.sm[:, 1:2], func=AF.Sqrt), reads=[W.sm], writes=[W.sm])
            op("dve", lambda e: e.reciprocal(out=W.sm[:, 3:4], in_=W.sm[:, 2:3]), reads=[W.sm], writes=[W.sm])
            op("dve", lambda e: e.scalar_tensor_tensor(out=W.tmpf[:], in0=src[:], scalar=W.sm[:, 3:4], in1=mod6[:, a_idx, :],
                                                       op0=ALU.mult, op1=ALU.mult), reads=[src, W.sm, mod6], writes=[W.tmpf])
            op("pool", lambda e: e.tensor_tensor(out=dst_bf[:], in0=W.tmpf[:], in1=mod6[:, b_idx, :], op=ALU.add),
               reads=[W.tmpf, mod6], writes=[dst_bf])

        def transpose8(src_bf, dstT, pbank):
            K.hold += 1
            for kc in range(KC):
                op("pe", lambda e, kc=kc: e.transpose(out=pbank[:, kc * 128:(kc + 1) * 128], in_=src_bf[:, kc * 128:(kc + 1) * 128],
                                                      identity=identb[:]), reads=[src_bf, identb], writes=[pbank], inc=(kc == KC - 1))
            K.hold -= 1
            op("act", lambda e: e.copy(out=dstT[:].rearrange("p k n -> p (k n)"), in_=pbank[:]), reads=[pbank], writes=[dstT])

        def mm_group(pbuf, out_ap, pairs, reads):
            n = len(pairs)
            K.hold += 1
            for i, (l, r) in enumerate(pairs):
                op("pe", lambda e, l=l, r=r, i=i: e.matmul(out_ap, lhsT=l, rhs=r, start=(i == 0), stop=(i == n - 1)),
                   reads=reads, writes=[pbuf], inc=(i == n - 1))
            K.hold -= 1

        def bcast_row(dst_ap, dst_buf, src_dram_ap, n):
            dma("sp", lambda e: e.dma_start(out=dst_ap, in_=src_dram_ap.partition_broadcast(128)), reads=[wsrc], writes=[dst_buf])

        dma("sp", lambda e: e.dma_start(out=cond[:], in_=c_in.ap()), reads=[wsrc], writes=[cond])
        op("act", lambda e: e.activation(out=cond[:], in_=cond[:], func=AF.Silu), reads=[cond], writes=[cond])

        x_src = x_in_b
        x_src_t = x_in
        for layer in range(depth):
            j = layer // 2
            is_ml = (layer % 2 == 0)
            last = (layer == depth - 1)
            for kc in range(KC):
                op("dve", lambda e, kc=kc: e.tensor_scalar(out=condb[:, kc, :], in0=onesf[:], scalar1=cond[:, kc:kc + 1], scalar2=None,
                                                           op0=ALU.mult), reads=[onesf, cond], writes=[condb])
            for ncol in range(12):
                dma("sp", lambda e, ncol=ncol: e.dma_start(out=rowv[0:1, :], in_=b_ada[layer:layer + 1, ncol * 512:(ncol + 1) * 512]), reads=[wsrc], writes=[rowv])
                dma("sp", lambda e, ncol=ncol: e.dma_start(
                    out=stage[:], in_=w_ada[layer].rearrange("(kc p) n -> p kc n", p=128)[:, :, ncol * 512:(ncol + 1) * 512]),
                    reads=[wsrc], writes=[stage])
                pb = ps[ncol % 2]
                pairs = [(condb[:, kc, :], stage[:, kc, :]) for kc in range(KC)]
                pairs.append((onesf[0:1, :], rowv[0:1, :]))
                mm_group(pb, pb[:], pairs, [condb, stage, onesf, rowv])
                op("dve", lambda e, ncol=ncol, pb=pb: e.tensor_copy(out=mod6[:, ncol // 2, (ncol % 2) * 512:(ncol % 2 + 1) * 512], in_=pb[:]),
                   reads=[pb], writes=[mod6])
            for (gsrc, idx) in ((n1g, 1), (n2g, 4)):
                bcast_row(WS[0].tmpf[:], WS[0].tmpf, gsrc[layer, :], D)
                op("dve", lambda e, idx=idx: e.scalar_tensor_tensor(out=mod6[:, idx, :], in0=mod6[:, idx, :], scalar=1.0, in1=WS[0].tmpf[:],
                                                                    op0=ALU.add, op1=ALU.mult), reads=[mod6, WS[0].tmpf], writes=[mod6])
            dma("pool", lambda e: e.dma_start(out=wrt[:], in_=w_rt[layer].rearrange("(kc p) n -> p kc n", p=128)), reads=[wsrc], writes=[wrt])
            bcast_row(brb[:], brb, b_rt[layer, :], 72)
            K.barrier()
            if is_ml:
                for kc in range(KC):
                    dma("pool", lambda e, kc=kc: e.dma_start(out=wmix_in_ml[:, kc, :], in_=ml_w_in[j, kc * 128:(kc + 1) * 128, :]),
                        reads=[wsrc], writes=[wmix_in_ml])
                    dma("pool", lambda e, kc=kc: e.dma_start(out=wmix_out[:, kc, :], in_=ml_w_out[j, kc * 128:(kc + 1) * 128, :]),
                        reads=[wsrc], writes=[wmix_out])
                bcast_row(bgb[:], bgb, ml_b_gate[j, :], 8)
                bcast_row(goutb[:], goutb, ml_g_out[j, :], D)
                op("dve", lambda e: e.memset(c32[:], 0.0), writes=[c32])
                op("dve", lambda e: e.memset(cb[:], 0.0), writes=[cb])
                for W_ in WS:
                    op("dve", lambda e, W_=W_: e.memset(W_.vp[:], 1.0), writes=[W_.vp])
                win = wmix_in_ml
            else:
                for kc in range(KC):
                    dma("pool", lambda e, kc=kc: e.dma_start(out=wmix_in_sw[:, kc, :], in_=sw_w_in[j, kc * 128:(kc + 1) * 128, :]),
                        reads=[wsrc], writes=[wmix_in_sw])
                    dma("pool", lambda e, kc=kc: e.dma_start(out=wmix_out[:, kc, :], in_=sw_w_out[j, kc * 128:(kc + 1) * 128, :]),
                        reads=[wsrc], writes=[wmix_out])
                bcast_row(gqb[:], gqb, sw_g_q[j, :], 64)
                bcast_row(gkb[:], gkb, sw_g_k[j, :], 64)
                bcast_row(esink[:], esink, sw_sinks[j, :], 16)
                op("act", lambda e: e.activation(out=esink[:], in_=esink[:], func=AF.Exp), reads=[esink], writes=[esink])
                for i in range(3):
                    op("dve", lambda e, i=i: e.memset(kTs[i][:], 0.0), writes=[kTs[i]])
                    op("dve", lambda e, i=i: e.memset(vps[i][:], 0.0), writes=[vps[i]])
                win = wmix_in_sw
            op("dve", lambda e: e.memset(base[:], 0.0), writes=[base])

            real_ps, real_pt = ps, pt

            class _PS:
                def __init__(self, p_):
                    self.p_ = p_
                def __getitem__(self, i):
                    return real_ps[3 * self.p_ + (i % 3)]

            class _PT:
                def __init__(self, p_):
                    self.p_ = p_
                def __getitem__(self, i):
                    return real_pt[self.p_]

            def tile_body(t, ps, pt, W):
                rows = slice(t * 128, (t + 1) * 128)
                dma("sp", lambda e: e.dma_start(out=W.xt[:], in_=x_src_t[rows, :]), reads=[x_src], writes=[W.xt])
                if layer > 0:
                    dma("pool", lambda e: e.indirect_dma_start(out=W.tmpf[:], out_offset=None, in_=y_slots[:, :],
                                                               in_offset=bass.IndirectOffsetOnAxis(ap=DESTi[:, 0, t:t + 1], axis=0)),
                        reads=[y_slots, DESTi], writes=[W.tmpf])
                    dma("pool", lambda e: e.indirect_dma_start(out=W.xt2[:], out_offset=None, in_=y_slots[:, :],
                                                               in_offset=bass.IndirectOffsetOnAxis(ap=DESTi[:, 1, t:t + 1], axis=0)),
                        reads=[y_slots, DESTi], writes=[W.xt2])
                    op("dve", lambda e: e.tensor_tensor(out=W.tmpf[:], in0=W.tmpf[:], in1=W.xt2[:], op=ALU.add), reads=[W.tmpf, W.xt2], writes=[W.tmpf])
                    op("pool", lambda e: e.tensor_tensor(out=W.xt[:], in0=W.xt[:], in1=W.tmpf[:], op=ALU.add), reads=[W.xt, W.tmpf], writes=[W.xt])
                rmsnorm_mod(W, W.xt, 1, 0, W.hnb)
                transpose8(W.hnb, W.hnT, pt[0])
                if is_ml:
                    for (dst, coff, scl) in ((W.qT, 0, 1.0), (W.kT, 512, 128 ** -0.5)):
                        pb = ps[0] if coff == 0 else ps[1]
                        for h in range(4):
                            mm_group(pb, pb[:, h * 128:(h + 1) * 128],
                                     [(win[:, kc, coff + h * 128:coff + (h + 1) * 128], W.hnT[:, kc, :]) for kc in range(KC)], [win, W.hnT])
                        op("act", lambda e, dst=dst, pb=pb, scl=scl: e.mul(out=dst[:].rearrange("p h n -> p (h n)"), in_=pb[:], mul=scl),
                           reads=[pb], writes=[dst])
                    mm_group(ps[2], ps[2][:], [(W.hnT[:, kc, :], win[:, kc, 512:1024]) for kc in range(KC)], [win, W.hnT])
                    op("act", lambda e: e.mul(out=W.ktok[:], in_=ps[2][:], mul=128 ** -0.5), reads=[ps[2]], writes=[W.ktok])
                    for half in range(2):
                        pb = ps[3 + half]
                        mm_group(pb, pb[:], [(W.hnT[:, kc, :], win[:, kc, 1024 + half * 512:1024 + (half + 1) * 512]) for kc in range(KC)], [win, W.hnT])
                        op("dve", lambda e, pb=pb, half=half: e.tensor_copy(out=W.vp[:, 2 * half:2 * half + 2, 0:256],
                                                                            in_=pb[:].rearrange("p (h n) -> p h n", h=2)),
                           reads=[pb], writes=[W.vp])
                    for half in range(2):
                        pb = ps[(5 + half) % 6]
                        mm_group(pb, pb[:], [(W.hnT[:, kc, :], win[:, kc, 2048 + half * 512:2048 + (half + 1) * 512]) for kc in range(KC)], [win, W.hnT])
                        op("act", lambda e, pb=pb, half=half: e.activation(out=W.go[:, half * 512:(half + 1) * 512], in_=pb[:], func=AF.Sigmoid),
                           reads=[pb], writes=[W.go])
                    op("pool", lambda e: e.tensor_tensor(out=W.go[:], in0=W.go[:], in1=goutb[:], op=ALU.mult), reads=[W.go, goutb], writes=[W.go])
                    mm_group(ps[1], ps[1][:, 0:8], [(W.hnT[:, kc, :], win[:, kc, 3072:3080]) for kc in range(KC)], [win, W.hnT])
                    G = W.gates
                    op("dve", lambda e: e.tensor_tensor(out=G[:, 0:8], in0=ps[1][:, 0:8], in1=bgb[:], op=ALU.add), reads=[ps[1], bgb], writes=[G])
                    op("act", lambda e: e.activation(out=G[:, 0:8], in_=G[:, 0:8], func=AF.Tanh, scale=1.0 / 15.0), reads=[G], writes=[G])
                    op("dve", lambda e: e.tensor_scalar(out=G[:, 0:8], in0=G[:, 0:8], scalar1=15.0, scalar2=None, op0=ALU.mult), reads=[G], writes=[G])
                    op("act", lambda e: e.activation(out=G[:, 8:12], in_=G[:, 4:8], func=AF.Exp, scale=-1.0), reads=[G], writes=[G])
                    op("act", lambda e: e.activation(out=G[:, 8:12], in_=G[:, 8:12], func=AF.Ln, bias=1.0), reads=[G], writes=[G])
                    op("dve", lambda e: e.tensor_scalar(out=G[:, 8:12], in0=G[:, 8:12], scalar1=-1.0, scalar2=None, op0=ALU.mult), reads=[G], writes=[G])
                    mm_group(ps[0], ps[0][:, 0:4], [(triinc[:], G[:, 8:12])], [triinc, G])
                    mm_group(ps[0], ps[0][:, 4:8], [(onesf[:], G[:, 8:12])], [onesf, G])
                    op("dve", lambda e: e.tensor_copy(out=G[:, 12:16], in_=ps[0][:, 0:4]), reads=[ps[0]], writes=[G])
                    op("dve", lambda e: e.tensor_copy(out=G[:, 20:24], in_=ps[0][:, 4:8]), reads=[ps[0]], writes=[G])
                    op("dve", lambda e: e.tensor_tensor(out=G[:, 16:20], in0=G[:, 0:4], in1=G[:, 12:16], op=ALU.subtract), reads=[G], writes=[G])
                    op("dve", lambda e: e.tensor_tensor(out=G[:, 24:28], in0=G[:, 16:20], in1=G[:, 20:24], op=ALU.add), reads=[G], writes=[G])
                    op("act", lambda e: e.activation(out=G[:, 24:28], in_=G[:, 24:28], func=AF.Exp), reads=[G], writes=[G])
                    op("act", lambda e: e.activation(out=G[:, 28:32], in_=G[:, 20:24], func=AF.Exp), reads=[G], writes=[G])
                    op("act", lambda e: e.activation(out=G[:, 32:36], in_=G[:, 12:16], func=AF.Exp), reads=[G], writes=[G])
                    op("pe", lambda e: e.transpose(out=ps[0][0:4, 128:256], in_=G[:, 12:16], identity=identf[:]), reads=[G, identf], writes=[ps[0]])
                    op("dve", lambda e: e.tensor_copy(out=W.bT[:], in_=ps[0][0:4, 128:256]), reads=[ps[0]], writes=[W.bT])
                    for h in range(4):
                        mm_group(ps[1], ps[1][:, 0:128], [(sel4[h][:], W.bT[:]), (identf[:], negm[:])], [sel4[h], W.bT, identf, negm])
                        op("act", lambda e, h=h: e.activation(out=W.dT[:], in_=ps[1][:, 0:128], func=AF.Exp, bias=G[:, 16 + h:17 + h]),
                           reads=[ps[1], G], writes=[W.dT])
                        mm_group(ps[2], ps[2][:, 0:128], [(W.kT[:, h, :], W.qT[:, h, :])], [W.kT, W.qT])
                        op("dve", lambda e: e.tensor_tensor(out=W.pT_[:], in0=ps[2][:, 0:128], in1=W.dT[:], op=ALU.mult), reads=[ps[2], W.dT], writes=[W.pT_])
                        mm_group(ps[3], ps[3][:, 0:257], [(W.pT_[:], W.vp[:, h, :])], [W.pT_, W.vp])
                        if W.p == 1:
                            K.wait_for(("st", h))
                        mm_group(ps[4], ps[4][:, 0:257], [(W.qT[:, h, :], cb[:, h, :])], [W.qT, cb])
                        op("act", lambda e, h=h: e.activation(out=W.tmpA[:], in_=ps[4][:, 0:257], func=AF.Copy, scale=G[:, 32 + h:33 + h]),
                           reads=[ps[4], G], writes=[W.tmpA])
                        op("dve", lambda e: e.tensor_tensor(out=W.res[:], in0=W.tmpA[:], in1=ps[3][:, 0:257], op=ALU.add), reads=[W.tmpA, ps[3]], writes=[W.res])
                        op("pool", lambda e, h=h: e.tensor_scalar(out=W.kw_[:], in0=W.ktok[:, h * 128:(h + 1) * 128], scalar1=G[:, 24 + h:25 + h], scalar2=None,
                                                                  op0=ALU.mult), reads=[W.ktok, G], writes=[W.kw_])
                        mm_group(ps[5], ps[5][:, 0:257], [(W.kw_[:], W.vp[:, h, :])], [W.kw_, W.vp])
                        op("dve", lambda e, h=h: e.scalar_tensor_tensor(out=c32[:, h, :], in0=c32[:, h, :], scalar=G[:, 28 + h:29 + h], in1=ps[5][:, 0:257],
                                                                        op0=ALU.mult, op1=ALU.add), reads=[c32, G, ps[5]], writes=[c32])
                        op("pool", lambda e, h=h: e.tensor_copy(out=cb[:, h, :], in_=c32[:, h, :]), reads=[c32], writes=[cb])
                        if W.p == 0:
                            K.signal(("st", h))
                        S_ = W.sm
                        op("dve", lambda e: e.tensor_scalar(out=S_[:, 7:8], in0=W.res[:, 256:257], scalar1=-1.0, scalar2=None, op0=ALU.mult),
                           reads=[W.res], writes=[S_])
                        op("dve", lambda e: e.tensor_tensor(out=S_[:, 8:9], in0=S_[:, 7:8], in1=W.res[:, 256:257], op=ALU.max), reads=[W.res, S_], writes=[S_])
                        op("dve", lambda e: e.tensor_scalar(out=S_[:, 8:9], in0=S_[:, 8:9], scalar1=1.0, scalar2=None, op0=ALU.max), reads=[S_], writes=[S_])
                        op("dve", lambda e: e.reciprocal(out=S_[:, 9:10], in_=S_[:, 8:9]), reads=[S_], writes=[S_])
                        op("act", lambda e: e.activation(out=W.junk[:, 0:256], in_=W.res[:, 0:256], func=AF.Square, accum_out=S_[:, 10:11]),
                           reads=[W.res], writes=[W.junk, S_])
                        op("dve", lambda e: e.tensor_tensor(out=S_[:, 11:12], in0=S_[:, 9:10], in1=S_[:, 9:10], op=ALU.mult), reads=[S_], writes=[S_])
                        op("dve", lambda e: e.scalar_tensor_tensor(out=S_[:, 12:13], in0=S_[:, 10:11], scalar=1.0 / 256.0, in1=S_[:, 11:12],
                                                                   op0=ALU.mult, op1=ALU.mult), reads=[S_], writes=[S_])
                        op("dve", lambda e: e.tensor_scalar(out=S_[:, 12:13], in0=S_[:, 12:13], scalar1=EPS, scalar2=None, op0=ALU.add), reads=[S_], writes=[S_])
                        op("act", lambda e: e.activation(out=S_[:, 13:14], in_=S_[:, 12:13], func=AF.Sqrt), reads=[S_], writes=[S_])
                        op("dve", lambda e: e.reciprocal(out=S_[:, 14:15], in_=S_[:, 13:14]), reads=[S_], writes=[S_])
                        op("dve", lambda e: e.tensor_tensor(out=S_[:, 15:16], in0=S_[:, 14:15], in1=S_[:, 9:10], op=ALU.mult), reads=[S_], writes=[S_])
                        op("dve", lambda e, h=h: e.scalar_tensor_tensor(out=W.hg[:, h * 256:(h + 1) * 256], in0=W.res[:, 0:256], scalar=S_[:, 15:16],
                                                                        in1=W.go[:, h * 256:(h + 1) * 256], op0=ALU.mult, op1=ALU.mult),
                           reads=[W.res, S_, W.go], writes=[W.hg])
                else:
                    cur, prv = t % 3, (t + 2) % 3
                    for half in range(2):
                        mm_group(ps[half], ps[half][:], [(W.hnT[:, kc, :], win[:, kc, half * 512:(half + 1) * 512]) for kc in range(KC)], [win, W.hnT])
                    mm_group(ps[2], ps[2][:], [(W.hnT[:, kc, :], win[:, kc, 1024:1536]) for kc in range(KC)], [win, W.hnT])
                    for half in range(2):
                        op("act", lambda e, half=half: e.activation(out=W.tmpf[:, half * 512:(half + 1) * 512], in_=ps[half][:], func=AF.Square),
                           reads=[ps[half]], writes=[W.tmpf])
                    op("dve", lambda e: e.tensor_reduce(out=W.ssq[:, 0:16], in_=W.tmpf[:].rearrange("p (h d) -> p h d", d=64), axis=AX.X, op=ALU.add),
                       reads=[W.tmpf], writes=[W.ssq])
                    op("act", lambda e: e.activation(out=W.xt2[:, 0:256], in_=ps[2][:, 0:256], func=AF.Square), reads=[ps[2]], writes=[W.xt2])
                    op("dve", lambda e: e.tensor_reduce(out=W.ssq[:, 16:20], in_=W.xt2[:, 0:256].rearrange("p (h d) -> p h d", d=64), axis=AX.X, op=ALU.add),
                       reads=[W.xt2], writes=[W.ssq])
                    op("dve", lambda e: e.tensor_scalar(out=W.ssq[:, 0:20], in0=W.ssq[:, 0:20], scalar1=1.0 / 64.0, scalar2=EPS, op0=ALU.mult, op1=ALU.add),
                       reads=[W.ssq], writes=[W.ssq])
                    op("act", lambda e: e.activation(out=W.ssq[:, 0:20], in_=W.ssq[:, 0:20], func=AF.Sqrt), reads=[W.ssq], writes=[W.ssq])
                    op("dve", lambda e: e.reciprocal(out=W.ssq[:, 0:20], in_=W.ssq[:, 0:20]), reads=[W.ssq], writes=[W.ssq])
                    for half in range(2):
                        op("dve", lambda e, half=half: e.tensor_tensor(
                            out=W.tmpf[:, half * 512:(half + 1) * 512].rearrange("p (h d) -> p h d", d=64),
                            in0=ps[half][:].rearrange("p (h d) -> p h d", d=64),
                            in1=W.ssq[:, half * 8:(half + 1) * 8].unsqueeze(2).broadcast_to([128, 8, 64]), op=ALU.mult),
                            reads=[ps[half], W.ssq], writes=[W.tmpf])
                    op("pool", lambda e: e.tensor_tensor(out=W.qn[:].rearrange("p (h d) -> p h d", d=64), in0=W.tmpf[:].rearrange("p (h d) -> p h d", d=64),
                                                         in1=gqb[:].unsqueeze(1).broadcast_to([128, 16, 64]), op=ALU.mult),
                       reads=[W.tmpf, gqb], writes=[W.qn])
                    op("dve", lambda e: e.tensor_tensor(out=W.xt2[:, 0:256].rearrange("p (h d) -> p h d", d=64),
                                                        in0=ps[2][:, 0:256].rearrange("p (h d) -> p h d", d=64),
                                                        in1=W.ssq[:, 16:20].unsqueeze(2).broadcast_to([128, 4, 64]), op=ALU.mult),
                       reads=[ps[2], W.ssq], writes=[W.xt2])
                    op("pool", lambda e: e.tensor_tensor(out=W.kn[:].rearrange("p (h d) -> p h d", d=64), in0=W.xt2[:, 0:256].rearrange("p (h d) -> p h d", d=64),
                                                         in1=gkb[:].unsqueeze(1).broadcast_to([128, 4, 64]), op=ALU.mult),
                       reads=[W.xt2, gkb], writes=[W.kn])
                    op("act", lambda e: e.copy(out=vps[cur][:, :, 0:64], in_=ps[2][:, 256:512].rearrange("p (h d) -> p h d", d=64)),
                       reads=[ps[2]], writes=[vps[cur]])
                    op("pool", lambda e: e.memset(vps[cur][:, :, 64:65], 1.0), reads=[vps[cur]], writes=[vps[cur]])
                    for half in range(2):
                        pb = pt[0]
                        K.hold += 1
                        for h8 in range(8):
                            hh = half * 8 + h8
                            op("pe", lambda e, hh=hh, h8=h8, pb=pb: e.transpose(out=pb[0:64, h8 * 128:(h8 + 1) * 128], in_=W.qn[:, hh * 64:(hh + 1) * 64],
                                                                                identity=identb[:]), reads=[W.qn, identb], writes=[pb], inc=(h8 == 7))
                        K.hold -= 1
                        op("act", lambda e, half=half, pb=pb: e.copy(out=W.qTs[:, half * 8:(half + 1) * 8, :].rearrange("p h n -> p (h n)"), in_=pb[0:64, :]),
                           reads=[pb], writes=[W.qTs])
                    K.hold += 1
                    for g in range(4):
                        op("pe", lambda e, g=g: e.transpose(out=pt[0][0:64, g * 128:(g + 1) * 128], in_=W.kn[:, g * 64:(g + 1) * 64], identity=identb[:]),
                           reads=[W.kn, identb], writes=[pt[0]], inc=(g == 3))
                    K.hold -= 1
                    op("act", lambda e: e.copy(out=kTs[cur][:].rearrange("p h n -> p (h n)"), in_=pt[0][0:64, 0:512]), reads=[pt[0]], writes=[kTs[cur]])
                    if W.p == 0:
                        K.signal("kv")
                    else:
                        K.wait_for("kv")
                    for g in range(4):
                        qv = W.qTs[:, 4 * g:4 * g + 4, :].rearrange("p h n -> p (h n)")
                        mm_group(ps[3], ps[3][:], [(kTs[prv][:, g, :], qv), (identb[:], negprev[:].rearrange("p h q -> p (h q)"))],
                                 [kTs[prv], W.qTs, identb, negprev])
                        mm_group(ps[4], ps[4][:], [(kTs[cur][:, g, :], qv), (identb[:], negcur[:].rearrange("p h q -> p (h q)"))],
                                 [kTs[cur], W.qTs, identb, negcur])
                        op("act", lambda e: e.activation(out=W.pprev[:], in_=ps[3][:], func=AF.Exp, scale=0.125), reads=[ps[3]], writes=[W.pprev])
                        op("act", lambda e: e.activation(out=W.pcur[:], in_=ps[4][:], func=AF.Exp, scale=0.125), reads=[ps[4]], writes=[W.pcur])
                        for hh in range(4):
                            mm_group(ps[5], ps[5][:, hh * 65:(hh + 1) * 65],
                                     [(W.pprev[:, hh * 128:(hh + 1) * 128], vps[prv][:, g, :]), (W.pcur[:, hh * 128:(hh + 1) * 128], vps[cur][:, g, :])],
                                     [W.pprev, W.pcur, vps[prv], vps[cur]])
                        pv = ps[5][:, 0:260].rearrange("p (h d) -> p h d", d=65)
                        op("dve", lambda e, g=g, pv=pv: e.tensor_tensor(out=W.ssq[:, 20:24], in0=pv[:, :, 64], in1=esink[:, 4 * g:4 * g + 4], op=ALU.add),
                           reads=[ps[5], esink], writes=[W.ssq])
                        op("dve", lambda e: e.reciprocal(out=W.ssq[:, 24:28], in_=W.ssq[:, 20:24]), reads=[W.ssq], writes=[W.ssq])
                        op("dve", lambda e, g=g, pv=pv: e.tensor_tensor(out=W.hg[:, g * 256:(g + 1) * 256].rearrange("p (h d) -> p h d", d=64), in0=pv[:, :, 0:64],
                                                                        in1=W.ssq[:, 24:28].unsqueeze(2).broadcast_to([128, 4, 64]), op=ALU.mult),
                           reads=[ps[5], W.ssq], writes=[W.hg])
                transpose8(W.hg, W.hgT, pt[1])
                for half in range(2):
                    pb = ps[half]
                    mm_group(pb, pb[:], [(W.hgT[:, kc, :], wmix_out[:, kc, half * 512:(half + 1) * 512]) for kc in range(KC)], [W.hgT, wmix_out])
                    op("dve", lambda e, pb=pb, half=half: e.tensor_tensor(out=W.tmpf[:, half * 512:(half + 1) * 512], in0=pb[:],
                                                                          in1=mod6[:, 2, half * 512:(half + 1) * 512], op=ALU.mult),
                       reads=[pb, mod6], writes=[W.tmpf])
                op("pool", lambda e: e.tensor_tensor(out=W.xt2[:], in0=W.tmpf[:], in1=W.xt[:], op=ALU.add), reads=[W.tmpf, W.xt], writes=[W.xt2])
                dma("pool", lambda e: e.dma_start(out=xs[rows, :], in_=W.xt2[:]), reads=[W.xt2], writes=[xs])
                rmsnorm_mod(W, W.xt2, 4, 3, W.hnb)
                dma("pool", lambda e: e.dma_start(out=hn2d[rows, :], in_=W.hnb[:]), reads=[W.hnb], writes=[hn2d], group=("hn2", layer))
                transpose8(W.hnb, W.hnT, pt[0])
                mm_group(ps[2], ps[2][:, 0:72], [(W.hnT[:, kc, :], wrt[:, kc, :]) for kc in range(KC)], [W.hnT, wrt])
                op("dve", lambda e: e.tensor_tensor(out=W.lg[:], in0=ps[2][:, 0:72], in1=brb[:], op=ALU.add), reads=[ps[2], brb], writes=[W.lg])
                R = W.rsm
                op("dve", lambda e: e.tensor_reduce(out=R[:, 0:1], in_=W.lg[:, 0:8], axis=AX.X, op=ALU.max), reads=[W.lg], writes=[R])
                op("dve", lambda e: e.tensor_scalar(out=R[:, 1:2], in0=R[:, 0:1], scalar1=-1.0, scalar2=None, op0=ALU.mult), reads=[R], writes=[R])
                op("dve", lambda e: e.tensor_scalar(out=R[:, 4:12], in0=W.lg[:, 0:8], scalar1=R[:, 0:1], scalar2=None, op0=ALU.is_equal), reads=[W.lg, R], writes=[R])
                op("act", lambda e: e.activation(out=R[:, 48:56], in_=W.lg[:, 0:8], func=AF.Exp, bias=R[:, 1:2], accum_out=R[:, 2:3]), reads=[W.lg, R], writes=[R])
                op("dve", lambda e: e.reciprocal(out=R[:, 3:4], in_=R[:, 2:3]), reads=[R], writes=[R])
                op("dve", lambda e: e.tensor_tensor(out=W.t64[:].rearrange("p (g x) -> p g x", g=8), in0=W.lg[:, 8:72].rearrange("p (g x) -> p g x", g=8),
                                                    in1=R[:, 4:12].unsqueeze(2).broadcast_to([128, 8, 8]), op=ALU.mult), reads=[W.lg, R], writes=[W.t64])
                op("dve", lambda e: e.tensor_reduce(out=R[:, 12:20], in_=W.t64[:].rearrange("p (g x) -> p x g", g=8), axis=AX.X, op=ALU.add),
                   reads=[W.t64], writes=[R])
                op("dve", lambda e: e.max(out=R[:, 20:28], in_=R[:, 12:20]), reads=[R], writes=[R])
                op("dve", lambda e: e.tensor_scalar(out=R[:, 28:36], in0=R[:, 12:20], scalar1=R[:, 20:21], scalar2=None, op0=ALU.is_equal), reads=[R], writes=[R])
                op("dve", lambda e: e.tensor_scalar(out=R[:, 36:44], in0=R[:, 12:20], scalar1=R[:, 21:22], scalar2=None, op0=ALU.is_equal), reads=[R], writes=[R])
                op("dve", lambda e: e.tensor_tensor(out=R[:, 44:45], in0=R[:, 20:21], in1=R[:, 21:22], op=ALU.subtract), reads=[R], writes=[R])
                op("act", lambda e: e.activation(out=R[:, 45:46], in_=R[:, 44:45], func=AF.Sigmoid), reads=[R], writes=[R])
                op("dve", lambda e: e.tensor_tensor(out=GT[:, 0, t:t + 1], in0=R[:, 45:46], in1=R[:, 3:4], op=ALU.mult), reads=[R], writes=[GT])
                op("dve", lambda e: e.tensor_tensor(out=GT[:, 1, t:t + 1], in0=R[:, 3:4], in1=GT[:, 0, t:t + 1], op=ALU.subtract), reads=[R, GT], writes=[GT])
                for (OH, c0) in ((OH1, 28), (OH2, 36)):
                    op("dve", lambda e, OH=OH, c0=c0: e.tensor_tensor(out=OH[:, t, :].rearrange("p (g x) -> p g x", g=8),
                                                                      in0=R[:, 4:12].unsqueeze(2).broadcast_to([128, 8, 8]),
                                                                      in1=R[:, c0:c0 + 8].unsqueeze(1).broadcast_to([128, 8, 8]), op=ALU.mult),
                       reads=[R], writes=[OH])
                op("dve", lambda e: e.tensor_tensor(out=W.ohs[:], in0=OH1[:, t, :], in1=OH2[:, t, :], op=ALU.add), reads=[OH1, OH2], writes=[W.ohs])
                mm_group(ps[3], ps[3][:, 0:64], [(tristrb[:], W.ohs[:])], [tristrb, W.ohs])
                mm_group(ps[3], ps[3][:, 64:128], [(onesb[:], W.ohs[:])], [onesb, W.ohs])
                if W.p == 1:
                    K.wait_for("base")
                op("dve", lambda e: e.tensor_tensor(out=W.rtot[:], in0=ps[3][:, 0:64], in1=base[:], op=ALU.add), reads=[ps[3], base], writes=[W.rtot])
                op("dve", lambda e: e.tensor_tensor(out=base[:], in0=ps[3][:, 64:128], in1=base[:], op=ALU.add), reads=[ps[3], base], writes=[base])
                if W.p == 0:
                    K.signal("base")
                for k_, OH in ((0, OH1), (1, OH2)):
                    op("dve", lambda e, OH=OH: e.tensor_tensor(out=W.t64[:], in0=OH[:, t, :], in1=W.rtot[:], op=ALU.mult), reads=[OH, W.rtot], writes=[W.t64])
                    op("dve", lambda e, k_=k_: e.tensor_reduce(out=R12[:, k_, t:t + 1], in_=W.t64[:], axis=AX.X, op=ALU.add), reads=[W.t64], writes=[R12])

            for t0_ in range(0, NT, 2):
                K.interleave([lambda t=t0_ + q_: tile_body(t, _PS(t % 2), _PT(t % 2), WS[t % 2]) for q_ in range(min(2, NT - t0_))])

            op("dve", lambda e: e.tensor_scalar(out=pp[:, 0, :], in0=base[:], scalar1=1.0 / BS, scalar2=(BS / 2 - 0.5) / BS, op0=ALU.mult, op1=ALU.add),
               reads=[base], writes=[pp])
            op("dve", lambda e: e.tensor_copy(out=ppi[:], in_=pp[:, 0, :]), reads=[pp], writes=[ppi])
            op("dve", lambda e: e.tensor_copy(out=pp[:, 0, :], in_=ppi[:]), reads=[ppi], writes=[pp])
            op("dve", lambda e: e.tensor_scalar(out=pp[:, 0, :], in0=pp[:, 0, :], scalar1=float(BS), scalar2=None, op0=ALU.mult), reads=[pp], writes=[pp])
            op("dve", lambda e: e.tensor_tensor_scan(out=pp[:, 1, :], data0=onesf[:, 0:64], data1=pp[:, 0, :], initial=0.0, op0=ALU.mult, op1=ALU.add),
               reads=[pp, onesf], writes=[pp])
            op("dve", lambda e: e.tensor_tensor(out=pp[:, 2, :], in0=pp[:, 1, :], in1=pp[:, 0, :], op=ALU.subtract), reads=[pp], writes=[pp])
            for k_, OH in ((0, OH1), (1, OH2)):
                for c0 in range(0, NT, 16):
                    n = min(16, NT - c0)
                    op("dve", lambda e, OH=OH, c0=c0, n=n: e.tensor_tensor(out=big[:, 0:n, :], in0=OH[:, c0:c0 + n, :],
                                                                           in1=pp[:, 2, :].unsqueeze(1).broadcast_to([128, n, 64]), op=ALU.mult),
                       reads=[OH, pp], writes=[bigb])
                    op("dve", lambda e, k_=k_, c0=c0, n=n: e.tensor_reduce(out=DEST[:, k_, c0:c0 + n], in_=big[:, 0:n, :], axis=AX.X, op=ALU.add),
                       reads=[bigb], writes=[DEST])
            op("dve", lambda e: e.tensor_tensor(out=DEST[:], in0=DEST[:], in1=R12[:], op=ALU.add), reads=[DEST, R12], writes=[DEST])
            op("dve", lambda e: e.tensor_copy(out=DESTi[:], in_=DEST[:]), reads=[DEST], writes=[DESTi])
            for k_ in range(2):
                op("dve", lambda e, k_=k_: e.tensor_copy(out=SRC[:, k_, :, 0], in_=tokid[:]), reads=[tokid], writes=[SRC])
                op("dve", lambda e, k_=k_: e.tensor_copy(out=SRC[:, k_, :, 1], in_=GT[:, k_, :]), reads=[GT], writes=[SRC])
            for c0 in range(0, NBLK, 16):
                n = min(16, NBLK - c0)
                op("dve", lambda e, c0=c0, n=n: e.tensor_scalar(out=sm[:, 16:16 + n], in0=jb[:, 0:n], scalar1=float(c0 * BS), scalar2=None, op0=ALU.add),
                   reads=[jb], writes=[sm])
                op("dve", lambda e, n=n: e.tensor_tensor(out=big[:, 0:n, :], in0=pp[:, 1, :].unsqueeze(1).broadcast_to([128, n, 64]),
                                                         in1=sm[:, 16:16 + n].unsqueeze(2).broadcast_to([128, n, 64]), op=ALU.is_le),
                   reads=[pp, sm], writes=[bigb])
                op("dve", lambda e, c0=c0, n=n: e.tensor_reduce(out=BE[:, c0:c0 + n], in_=big[:, 0:n, :], axis=AX.X, op=ALU.add), reads=[bigb], writes=[BE])
            op("dve", lambda e: e.tensor_scalar(out=BE[:], in0=BE[:], scalar1=float(NE - 1), scalar2=128.0, op0=ALU.min, op1=ALU.mult), reads=[BE], writes=[BE])
            op("dve", lambda e: e.tensor_scalar(out=BE[:], in0=BE[:], scalar1=iota_p[:, 0:1], scalar2=float(layer * NE * 128), op0=ALU.add, op1=ALU.add),
               reads=[BE, iota_p], writes=[BE])
            op("dve", lambda e: e.tensor_copy(out=BEi[:], in_=BE[:]), reads=[BE], writes=[BEi])
            NR_ = NSLOT // 128
            for r0_ in range(0, NR_, 16):
                dma("sp", lambda e, r0_=r0_: e.dma_start(out=slot_tw.ap().rearrange("(p r) c -> p r c", p=128)[:, r0_:r0_ + 16, :], in_=padinit[:]),
                    reads=[padinit], writes=[slot_tw])
            for k_ in range(2):
                for t in range(NT):
                    dma("pool", lambda e, k_=k_, t=t: e.indirect_dma_start(
                        out=slot_tw[:, :], out_offset=bass.IndirectOffsetOnAxis(ap=DESTi[:, k_, t:t + 1], axis=0),
                        in_=SRC[:, k_, t, :], in_offset=None), reads=[SRC, DESTi], writes=[slot_tw], sem_buf=SRC)

            K.barrier()

            def expert_block(jblk, ps, pt, c_):
                pb_ = jblk % 3
                xg, xTm, hTm, stw, stok = xgc[c_], xTmc[c_], hTmc[c_], stwc[c_], stokc[c_]
                yo = [WS[c_].xt, WS[c_].xt2]
                sil = WS[c_].tmpA
                for (wb, wr_) in ((w1b[pb_], w1r), (w3b[pb_], w3r), (w2b[pb_], w2r)):
                    dma("pool", lambda e, wb=wb, wr_=wr_: e.indirect_dma_start(
                        out=wb[:].rearrange("p k n -> p (k n)"), out_offset=None, in_=wr_[:, :],
                        in_offset=bass.IndirectOffsetOnAxis(ap=BEi[:, jblk:jblk + 1], axis=0)), reads=[wsrc, BEi], writes=[wb])
                for sub in range(BS // 128):
                    r0 = jblk * BS + sub * 128
                    dma("sp", lambda e, sub=sub, r0=r0: e.dma_start(out=stw[sub][:], in_=slot_tw[r0:r0 + 128, :]), reads=[slot_tw], writes=[stw[sub]])
                    op("dve", lambda e, sub=sub: e.tensor_copy(out=stok[sub][:], in_=stw[sub][:, 0:1]), reads=[stw[sub]], writes=[stok[sub]])
                    dma("pool", lambda e, sub=sub: e.indirect_dma_start(
                        out=xg[sub][:], out_offset=None, in_=hn2d[:, :],
                        in_offset=bass.IndirectOffsetOnAxis(ap=stok[sub][:, 0:1], axis=0)), reads=[hn2d, stok[sub]], writes=[xg[sub]])
                    K.hold += 1
                    for kc in range(KC):
                        op("pe", lambda e, kc=kc, sub=sub: e.transpose(out=pt[0][:, kc * 128:(kc + 1) * 128], in_=xg[sub][:, kc * 128:(kc + 1) * 128],
                                                                       identity=identb[:]), reads=[xg[sub], identb], writes=[pt[0]], inc=(kc == KC - 1))
                    K.hold -= 1
                    op("act", lambda e, sub=sub: e.copy(out=xTm[:, :, sub * 128:(sub + 1) * 128], in_=pt[0][:].rearrange("p (k n) -> p k n", k=KC)),
                       reads=[pt[0]], writes=[xTm])
                for m in range(3):
                    p1, p3 = ps[(2 * m) % 3], ps[(2 * m + 1) % 3]
                    mm_group(p1, p1[:, 0:BS], [(w1b[pb_][:, kc, m * 128:(m + 1) * 128], xTm[:, kc, :]) for kc in range(KC)], [w1b[pb_], xTm])
                    mm_group(p3, p3[:, 0:BS], [(w3b[pb_][:, kc, m * 128:(m + 1) * 128], xTm[:, kc, :]) for kc in range(KC)], [w3b[pb_], xTm])
                    op("act", lambda e, p1=p1: e.activation(out=sil[:, 0:BS], in_=p1[:, 0:BS], func=AF.Silu), reads=[p1], writes=[sil])
                    op("dve", lambda e, p3=p3, m=m: e.tensor_tensor(out=hTm[:, m, :], in0=p3[:, 0:BS], in1=sil[:, 0:BS], op=ALU.mult), reads=[p3, sil], writes=[hTm])
                for sub in range(BS // 128):
                    r0 = jblk * BS + sub * 128
                    for half in range(2):
                        pb = ps[(2 * sub + half) % 3]
                        mm_group(pb, pb[:], [(hTm[:, c, sub * 128:(sub + 1) * 128], w2b[pb_][:, c, half * 512:(half + 1) * 512]) for c in range(3)],
                                 [hTm, w2b[pb_]])
                        op("act", lambda e, pb=pb, sub=sub, half=half: e.activation(out=yo[sub][:, half * 512:(half + 1) * 512], in_=pb[:], func=AF.Copy,
                                                                                    scale=stw[sub][:, 1:2]), reads=[pb, stw[sub]], writes=[yo[sub]])
                        op("dve", lambda e, sub=sub, half=half: e.tensor_tensor(out=yo[sub][:, half * 512:(half + 1) * 512], in0=yo[sub][:, half * 512:(half + 1) * 512],
                                                                                in1=mod6[:, 5, half * 512:(half + 1) * 512], op=ALU.mult),
                           reads=[yo[sub], mod6], writes=[yo[sub]])
                    dma("act", lambda e, sub=sub, r0=r0: e.dma_start(out=y_slots[r0:r0 + 128, :], in_=yo[sub][:]), reads=[yo[sub]], writes=[y_slots],
                        group=("ys", layer))

            for j0_ in range(0, NBLK, 2):
                K.interleave([lambda j=j0_ + q_: expert_block(j, _PS(j % 2), _PT(j % 2), j % 2) for q_ in range(min(2, NBLK - j0_))])

            K.barrier()
            dst_t = out if last else xs.t
            dst_b = out_b if last else xs

            def combine_tile(t, c_):
                rows = slice(t * 128, (t + 1) * 128)
                xt, xt2, ya, yb = WS[c_].xt, WS[c_].xt2, WS[c_].tmpf, ybc[c_]
                dma("sp", lambda e: e.dma_start(out=xt[:], in_=xs[rows, :]), reads=[xs], writes=[xt])
                dma("pool", lambda e, t=t: e.indirect_dma_start(out=ya[:], out_offset=None, in_=y_slots[:, :],
                                                               in_offset=bass.IndirectOffsetOnAxis(ap=DESTi[:, 0, t:t + 1], axis=0)),
                    reads=[y_slots, DESTi], writes=[ya])
                dma("pool", lambda e, t=t: e.indirect_dma_start(out=yb[:], out_offset=None, in_=y_slots[:, :],
                                                               in_offset=bass.IndirectOffsetOnAxis(ap=DESTi[:, 1, t:t + 1], axis=0)),
                    reads=[y_slots, DESTi], writes=[yb])
                op("dve", lambda e: e.tensor_tensor(out=ya[:], in0=ya[:], in1=yb[:], op=ALU.add), reads=[ya, yb], writes=[ya])
                op("dve", lambda e: e.tensor_tensor(out=xt2[:], in0=ya[:], in1=xt[:], op=ALU.add), reads=[ya, xt], writes=[xt2])
                if last:
                    dma("act", lambda e: e.dma_start(out=dst_t[rows, :], in_=xt2[:]), reads=[xt2], writes=[dst_b], group=("out", layer))
                else:
                    dma("act", lambda e: e.dma_start(out=dst_t[rows, :], in_=xt2[:]), reads=[xt2], writes=[dst_b])
                if debug:
                    dma("sp", lambda e: e.dma_start(out=dbg[layer * S + t * 128:layer * S + (t + 1) * 128, :], in_=xt2[:]), reads=[xt2], writes=[dbg_b])

            if last:
                for t0_ in range(0, NT, 2):
                    K.interleave([lambda t=t0_ + q_: combine_tile(t, t % 2) for q_ in range(min(2, NT - t0_))])
            x_src = xs
            x_src_t = xs.t
            K.barrier()
        K.finish([out_b] + ([dbg_b] if debug else []))
        K.barrier()
        ninst = K.ninst
    return nc, ninst


def prep_weights(inp):
    f = lambda a: np.ascontiguousarray(np.asarray(a, dtype=np.float32))
    w = {}
    for k in ("w_ada", "b_ada", "norm1_g", "norm2_g", "ml_w_in", "ml_b_gate", "ml_g_out", "ml_w_out",
              "sw_w_in", "sw_g_q", "sw_g_k", "sw_sinks", "sw_w_out"):
        w[k] = f(inp[k])
    w["w_rt"] = f(np.concatenate([np.asarray(inp["moe_w_group"]), np.asarray(inp["moe_w_router"])], axis=-1))
    w["b_rt"] = f(np.concatenate([np.asarray(inp["moe_b_group"]), np.asarray(inp["moe_b_router"])], axis=-1))
    w1 = np.asarray(inp["moe_w1"]).reshape(DEPTH, NE, KC, 128, DE)
    w["w1r"] = f(w1.transpose(0, 1, 3, 2, 4).reshape(DEPTH * NE * 128, KC * DE))
    w3 = np.asarray(inp["moe_w3"]).reshape(DEPTH, NE, KC, 128, DE)
    w["w3r"] = f(w3.transpose(0, 1, 3, 2, 4).reshape(DEPTH * NE * 128, KC * DE))
    w2 = np.asarray(inp["moe_w2"]).reshape(DEPTH, NE, 3, 128, D)
    w["w2r"] = f(w2.transpose(0, 1, 3, 2, 4).reshape(DEPTH * NE * 128, 3 * D))
    return w


def run(inp, S, depth=DEPTH, trace=False, debug=False):
    x = np.asarray(inp["x"], dtype=np.float32)
    c = np.asarray(inp["c"], dtype=np.float32)
    B = x.shape[0]
    w = prep_weights(inp)
    nc, ninst = build_program(S, depth, debug)
    in_maps = []
    for core in range(8):
        b = (core // 2) % B
        m = dict(w)
        m["x"] = np.ascontiguousarray(x[b, :S])
        m["c"] = np.ascontiguousarray(c[b].reshape(KC, 128).T)
        m["coff"] = np.full((128, 1), 0.0 if core % 2 == 0 else 1.0e7, dtype=np.float32)
        in_maps.append(m)
    res = run_bass_kernel_spmd(nc, in_maps, core_ids=list(range(8)), **({"trace": True} if trace else {}))
    outs = np.stack([res.results[2 * b]["out"] for b in range(B)], axis=0)
    return outs, res


def kernel(**inputs):
    S = np.asarray(inputs["x"]).shape[1]
    outs, _ = run(inputs, S, DEPTH)
    return outs.astype(np.float32)
```

```python
import math
import threading
from contextlib import ExitStack

import numpy as np
import concourse.bass as bass
import concourse.mybir as mybir
from concourse.bass_utils import run_bass_kernel_spmd

F32 = mybir.dt.float32
BF16 = mybir.dt.bfloat16
I32 = mybir.dt.int32
AF = mybir.ActivationFunctionType
ALU = mybir.AluOpType
AX = mybir.AxisListType

D = 1024
KC = 8
DEPTH = 4
EPS = 1e-6
ML_IN = 3080
SW_IN = 1536
NE = 64
DE = 384
BS = 256
NEG = -30000.0


class Buf:
    def __init__(self, K, name, t, dram=False):
        self.K = K
        self.name = name
        self.t = t
        self.dram = dram
        self.w = {}
        self.r = {}
        self.dsem = None
        self.dcnt = 0
        self.wgroup = None
        K.all_bufs.append(self)

    def __getitem__(self, k):
        return self.t[k]

    def ap(self):
        return self.t.ap() if self.dram else self.t[:]


class Eng:
    def __init__(self, name, h, sem):
        self.name = name
        self.h = h
        self.sem = sem
        self.cnt = 0
        self.waited = {}


class Kern:
    def __init__(self, nc, stack):
        self.nc = nc
        self.stack = stack
        self.sems = {}
        self.engs = {}
        self.all_bufs = []
        for nm, h in (("pe", nc.tensor), ("act", nc.scalar), ("dve", nc.vector),
                      ("pool", nc.gpsimd), ("sp", nc.sync)):
            s = stack.enter_context(nc.semaphore("sem_" + nm))
            self.sems[id(s)] = s
            self.engs[nm] = Eng(nm, h, s)
        self.dsem_pool = []
        self.ninst = 0
        self.outst = {"sp": [], "act": [], "pool": []}
        self.hold = 0
        self.il = None
        self.maxq = {"sp": 12, "act": 12, "pool": 8}

    def sbuf(self, name, shape, dt):
        t = self.stack.enter_context(self.nc.sbuf_tensor(name, list(shape), dt))
        return Buf(self, name, t)

    def psum(self, name, shape, dt=F32):
        t = self.stack.enter_context(self.nc.psum_tensor(name, list(shape), dt))
        return Buf(self, name, t)

    def dram(self, name, shape, dt, kind="Internal"):
        t = self.nc.dram_tensor(name, list(shape), dt, kind=kind)
        return Buf(self, name, t, dram=True)

    def view(self, name, ap):
        return Buf(self, name, ap)

    def _dsem(self, b):
        if b.dsem is None:
            s = self.stack.enter_context(self.nc.semaphore("ds_" + b.name))
            self.sems[id(s)] = s
            b.dsem = s
        return b.dsem

    def _deps(self, reads, writes, group=None):
        deps = {}
        for b in reads:
            for k, v in b.w.items():
                if deps.get(k, 0) < v:
                    deps[k] = v
        for b in writes:
            same = (group is not None and b.wgroup == group)
            for d in ((b.r,) if same else (b.w, b.r)):
                for k, v in d.items():
                    if deps.get(k, 0) < v:
                        deps[k] = v
        return deps

    def _wait(self, e, deps, skip_self=False):
        for k, v in deps.items():
            if skip_self and k == id(e.sem):
                continue
            if e.waited.get(k, 0) < v:
                e.h.wait_ge(self.sems[k], v)
                e.waited[k] = v
                self.ninst += 1

    def _commit(self, reads, writes, k, v, group=None):
        for b in reads:
            if b.r.get(k, 0) < v:
                b.r[k] = v
        for b in writes:
            if group is not None and b.wgroup == group:
                if b.w.get(k, 0) < v:
                    b.w[k] = v
            else:
                b.w = {k: v}
            b.wgroup = group
            b.r = {}

    def interleave(self, fns, credit=3):
        if len(fns) == 1:
            fns[0]()
            return
        il = {"turn": 0, "alive": [True] * len(fns), "ids": {}, "err": None, "credit": credit, "left": credit,
              "cv": threading.Condition(), "sig": set()}
        self.il = il

        def nxt(i):
            n = len(fns)
            for d in range(1, n):
                j = (i + d) % n
                if il["alive"][j]:
                    il["turn"] = j
                    il["left"] = il["credit"]
                    return
            il["left"] = il["credit"]

        il["nxt"] = nxt

        def worker(i, fn):
            il["ids"][threading.get_ident()] = i
            with il["cv"]:
                while il["turn"] != i:
                    il["cv"].wait()
            try:
                fn()
            except BaseException as ex:
                il["err"] = ex
            finally:
                with il["cv"]:
                    il["alive"][i] = False
                    nxt(i)
                    il["cv"].notify_all()

        ths = [threading.Thread(target=worker, args=(i, f)) for i, f in enumerate(fns)]
        for th in ths:
            th.start()
        for th in ths:
            th.join()
        self.il = None
        if il["err"] is not None:
            raise il["err"]

    def signal(self, key):
        il = self.il
        if il is None:
            return
        with il["cv"]:
            il["sig"].add(key)

    def wait_for(self, key):
        il = self.il
        if il is None:
            return
        i = il["ids"].get(threading.get_ident())
        assert self.hold == 0
        with il["cv"]:
            while key not in il["sig"]:
                assert any(a for j, a in enumerate(il["alive"]) if j != i), ("deadlock waiting for", key)
                il["nxt"](i)
                il["cv"].notify_all()
                while il["turn"] != i:
                    il["cv"].wait()

    def point(self):
        il = self.il
        if il is None or self.hold > 0:
            return
        i = il["ids"].get(threading.get_ident())
        if i is None:
            return
        with il["cv"]:
            il["left"] -= 1
            if il["left"] <= 0:
                il["nxt"](i)
                il["cv"].notify_all()
                while il["turn"] != i:
                    il["cv"].wait()

    def op(self, eng, fn, reads=(), writes=(), inc=True):
        self.point()
        e = self.engs[eng]
        self._wait(e, self._deps(reads, writes), skip_self=(eng == "pe"))
        ins = fn(e.h)
        self.ninst += 1
        if inc:
            e.cnt += 1
            ins.then_inc(e.sem, 1)
            v = e.cnt
        else:
            v = e.cnt + 1
        self._commit(reads, writes, id(e.sem), v)
        return ins

    def dma(self, q, fn, reads=(), writes=(), sem_buf=None, group=None):
        self.point()
        e = self.engs[q]
        self._wait(e, self._deps(reads, writes, group))
        if sem_buf is None:
            cand = [b for b in list(writes) + list(reads) if not b.dram]
            sem_buf = cand[0] if cand else (list(writes) + list(reads))[0]
        s = self._dsem(sem_buf)
        ins = fn(e.h)
        self.ninst += 1
        sem_buf.dcnt += 16
        ins.then_inc(s, 16)
        self._commit(reads, writes, id(s), sem_buf.dcnt, group)
        q_ = self.outst[q]
        q_.append((id(s), sem_buf.dcnt))
        if len(q_) > self.maxq[q]:
            k, v = q_.pop(0)
            self._wait(e, {k: v})
        return ins

    def barrier(self):
        deps = {}
        for e in self.engs.values():
            if e.cnt:
                deps[id(e.sem)] = e.cnt
        for b in self.all_bufs:
            for d in (b.w, b.r):
                for k, v in d.items():
                    if deps.get(k, 0) < v:
                        deps[k] = v
        for e in self.engs.values():
            self._wait(e, deps)

    def finish(self, bufs):
        e = self.engs["sp"]
        deps = {}
        for b in bufs:
            for d in (b.w, b.r):
                for k, v in d.items():
                    if deps.get(k, 0) < v:
                        deps[k] = v
        self._wait(e, deps)


def build_program(S, depth=DEPTH, debug=False):
    NT = S // 128
    NBLK = (2 * S) // BS + NE
    NSLOT = NBLK * BS
    assert NSLOT % 128 == 0
    nc = bass.Bass("TRN2", target_bir_lowering=False)

    def din(name, shape, dt=F32):
        return nc.dram_tensor(name, list(shape), dt, kind="ExternalInput")

    x_in = din("x", [S, D])
    c_in = din("c", [128, KC])
    coff_in = din("coff", [128, 1])
    w_ada = din("w_ada", [DEPTH, D, 6 * D])
    b_ada = din("b_ada", [DEPTH, 6 * D])
    n1g = din("norm1_g", [DEPTH, D])
    n2g = din("norm2_g", [DEPTH, D])
    ml_w_in = din("ml_w_in", [2, D, ML_IN])
    ml_b_gate = din("ml_b_gate", [2, 8])
    ml_g_out = din("ml_g_out", [2, D])
    ml_w_out = din("ml_w_out", [2, D, D])
    sw_w_in = din("sw_w_in", [2, D, SW_IN])
    sw_g_q = din("sw_g_q", [2, 64])
    sw_g_k = din("sw_g_k", [2, 64])
    sw_sinks = din("sw_sinks", [2, 16])
    sw_w_out = din("sw_w_out", [2, D, D])
    w_rt = din("w_rt", [DEPTH, D, 72])
    b_rt = din("b_rt", [DEPTH, 72])
    w1r = din("w1r", [DEPTH * NE * 128, KC * DE])
    w3r = din("w3r", [DEPTH * NE * 128, KC * DE])
    w2r = din("w2r", [DEPTH * NE * 128, 3 * D])
    out = nc.dram_tensor("out", [S, D], F32, kind="ExternalOutput")
    dbg = nc.dram_tensor("dbg", [depth * S, D], F32, kind="ExternalOutput") if debug else None

    with ExitStack() as st:
        K = Kern(nc, st)
        op, dma = K.op, K.dma
        x_in_b = Buf(K, "x_in", x_in, dram=True)
        out_b = Buf(K, "out", out, dram=True)
        dbg_b = Buf(K, "dbg", dbg, dram=True) if debug else None
        wsrc = Buf(K, "wsrc", None, dram=True)
        xs = K.dram("xs", [S, D], F32)
        hn2d = K.dram("hn2d", [S + 128, D], BF16)
        slot_tw = K.dram("slot_tw", [NSLOT, 2], F32)
        y_slots = K.dram("y_slots", [NSLOT, D], F32)

        identf = K.sbuf("identf", [128, 128], F32)
        identb = K.sbuf("identb", [128, 128], BF16)
        onesf = K.sbuf("onesf", [128, 128], F32)
        onesb = K.sbuf("onesb", [128, 128], BF16)
        triinc = K.sbuf("triinc", [128, 128], F32)
        tristrb = K.sbuf("tristrb", [128, 128], BF16)
        negm = K.sbuf("negm", [128, 128], F32)
        negcur = K.sbuf("negcur", [128, 4, 128], BF16)
        negprev = K.sbuf("negprev", [128, 4, 128], BF16)
        tmpc = K.sbuf("tmpc", [128, 512], F32)
        sel4 = [K.sbuf("sel4_%d" % h, [4, 128], F32) for h in range(4)]
        iota_p = K.sbuf("iota_p", [128, 1], F32)
        tokid = K.sbuf("tokid", [128, NT], F32)
        jb = K.sbuf("jb", [128, 16], F32)
        padinit = K.sbuf("padinit", [128, 16, 2], F32)
        zrow = K.sbuf("zrow", [128, 256], BF16)

        op("pool", lambda e: e.memset(onesf[:], 1.0), writes=[onesf])
        op("pool", lambda e: e.memset(onesb[:], 1.0), writes=[onesb])
        op("pool", lambda e: e.affine_select(out=identf[:], in_=onesf[:], pattern=[[-1, 128]], compare_op=ALU.is_equal,
                                             fill=0.0, base=0, channel_multiplier=1), reads=[onesf], writes=[identf])
        op("pool", lambda e: e.tensor_copy(out=identb[:], in_=identf[:]), reads=[identf], writes=[identb])
        op("pool", lambda e: e.affine_select(out=triinc[:], in_=onesf[:], pattern=[[1, 128]], compare_op=ALU.is_ge,
                                             fill=0.0, base=0, channel_multiplier=-1), reads=[onesf], writes=[triinc])
        op("pool", lambda e: e.affine_select(out=tmpc[:, 0:128], in_=onesf[:], pattern=[[1, 128]], compare_op=ALU.is_gt,
                                             fill=0.0, base=0, channel_multiplier=-1), reads=[onesf], writes=[tmpc])
        op("pool", lambda e: e.tensor_copy(out=tristrb[:], in_=tmpc[:, 0:128]), reads=[tmpc], writes=[tristrb])
        op("pool", lambda e: e.memset(tmpc[:], 0.0), reads=[tmpc], writes=[tmpc])
        op("pool", lambda e: e.affine_select(out=negm[:], in_=tmpc[:, 0:128], pattern=[[1, 128]], compare_op=ALU.is_ge,
                                             fill=NEG, base=0, channel_multiplier=-1), reads=[tmpc], writes=[negm])
        op("pool", lambda e: e.affine_select(out=negcur[:].rearrange("p h q -> p (h q)"), in_=tmpc[:], pattern=[[0, 4], [1, 128]],
                                             compare_op=ALU.is_ge, fill=NEG, base=0, channel_multiplier=-1),
           reads=[tmpc], writes=[negcur])
        op("pool", lambda e: e.affine_select(out=negprev[:].rearrange("p h q -> p (h q)"), in_=tmpc[:], pattern=[[0, 4], [-1, 128]],
                                             compare_op=ALU.is_gt, fill=NEG, base=0, channel_multiplier=1),
           reads=[tmpc], writes=[negprev])
        for h in range(4):
            op("pool", lambda e, h=h: e.affine_select(out=sel4[h][:], in_=onesf[0:4, :], pattern=[[0, 128]], compare_op=ALU.is_equal,
                                                      fill=0.0, base=-h, channel_multiplier=1), reads=[onesf], writes=[sel4[h]])
        op("pool", lambda e: e.iota(iota_p[:], pattern=[[0, 1]], base=0, channel_multiplier=1, allow_small_or_imprecise_dtypes=True),
           writes=[iota_p])
        op("pool", lambda e: e.iota(tokid[:], pattern=[[128, NT]], base=0, channel_multiplier=1, allow_small_or_imprecise_dtypes=True),
           writes=[tokid])
        op("pool", lambda e: e.iota(jb[:], pattern=[[BS, 16]], base=0, channel_multiplier=0, allow_small_or_imprecise_dtypes=True),
           writes=[jb])
        op("pool", lambda e: e.memset(padinit[:, :, 0:1], float(S)), writes=[padinit])
        op("pool", lambda e: e.memset(padinit[:, :, 1:2], 0.0), reads=[padinit], writes=[padinit])
        op("pool", lambda e: e.memset(zrow[:], 0.0), writes=[zrow])
        for q4 in range(4):
            dma("sp", lambda e, q4=q4: e.dma_start(out=hn2d[S:S + 128, q4 * 256:(q4 + 1) * 256], in_=zrow[:]), reads=[zrow], writes=[hn2d])

        mod6 = K.sbuf("mod6", [128, 6, D], F32)
        cond = K.sbuf("cond", [128, KC], F32)
        rowv = tmpc
        wrt = K.sbuf("wrt", [128, KC, 72], BF16)
        brb = K.sbuf("brb", [128, 72], F32)
        bgb = K.sbuf("bgb", [128, 8], F32)
        goutb = K.sbuf("goutb", [128, D], F32)
        gqb = K.sbuf("gqb", [128, 64], F32)
        gkb = K.sbuf("gkb", [128, 64], F32)
        esink = K.sbuf("esink", [128, 16], F32)
        ARENA = KC * (ML_IN + D)
        arena = st.enter_context(nc.sbuf_tensor("arena", [128, ARENA], BF16))
        wmix_in_ml = K.view("wmix_in_ml", arena[:, 0:KC * ML_IN].rearrange("p (k n) -> p k n", k=KC))
        wmix_out = K.view("wmix_out", arena[:, KC * ML_IN:ARENA].rearrange("p (k n) -> p k n", k=KC))
        wmix_in_sw = K.view("wmix_in_sw", arena[:, 0:KC * SW_IN].rearrange("p (k n) -> p k n", k=KC))
        stage = K.view("stage", arena[:, 0:2 * KC * 512].bitcast(F32).rearrange("p (k n) -> p k n", k=KC))
        condb = K.view("condb", arena[:, 2 * KC * 512:2 * KC * 512 + 2 * KC * 128].bitcast(F32).rearrange("p (k n) -> p k n", k=KC))
        o = 0
        def carve(name, n, shape_str=None, **kw):
            nonlocal o
            ap = arena[:, o:o + n]
            o += n
            if shape_str:
                ap = ap.rearrange(shape_str, **kw)
            return K.view(name, ap)
        w1b = [carve("w1b%d" % i, KC * DE, "p (k n) -> p k n", k=KC) for i in range(2)]
        w3b = [carve("w3b%d" % i, KC * DE, "p (k n) -> p k n", k=KC) for i in range(2)]
        w2b = [carve("w2b%d" % i, 3 * D, "p (k n) -> p k n", k=3) for i in range(3)]
        xgc = [[carve("xg%d_%d" % (c_, i), D) for i in range(2)] for c_ in range(2)]
        xTmc = [carve("xTm%d" % c_, KC * BS, "p (k n) -> p k n", k=KC) for c_ in range(2)]
        hTmc = [carve("hTm%d" % c_, 3 * BS, "p (k n) -> p k n", k=3) for c_ in range(2)]
        ybc = [K.view("ybc%d" % i, arena[:, i * 2 * D:(i + 1) * 2 * D].bitcast(F32)) for i in range(2)]
        assert o <= ARENA

        ps = [K.psum("ps%d" % i, [128, 512], F32) for i in range(6)]
        pt = [K.psum("pt%d" % i, [128, 1024], BF16) for i in range(2)]

        class WSet:
            pass
        WS = []
        for p_ in range(2):
            W = WSet()
            sfx = "_%d" % p_
            W.p = p_
            W.xt = K.sbuf("xt" + sfx, [128, D], F32)
            W.junk = K.sbuf("junk" + sfx, [128, D], BF16)
            W.tmpf = K.sbuf("tmpf" + sfx, [128, D], F32)
            W.hnb = K.sbuf("hnb" + sfx, [128, D], BF16)
            W.hnT = K.sbuf("hnT" + sfx, [128, KC, 128], BF16)
            W.sm = K.sbuf("sm" + sfx, [128, 64], F32)
            W.hg = K.sbuf("hg" + sfx, [128, D], BF16)
            W.hgT = K.sbuf("hgT" + sfx, [128, KC, 128], BF16)
            W.xt2 = K.sbuf("xt2" + sfx, [128, D], F32)
            W.gates = K.sbuf("gates" + sfx, [128, 40], F32)
            W.bT = K.sbuf("bT" + sfx, [4, 128], F32)
            W.ssq = K.sbuf("ssq" + sfx, [128, 32], F32)
            W.lg = K.sbuf("lg" + sfx, [128, 72], F32)
            W.rsm = K.sbuf("rsm" + sfx, [128, 64], F32)
            W.t64 = K.sbuf("t64" + sfx, [128, 64], F32)
            W.ohs = K.sbuf("ohs" + sfx, [128, 64], BF16)
            W.rtot = K.sbuf("rtot" + sfx, [128, 64], F32)
            W.go = K.sbuf("go" + sfx, [128, D], BF16)
            W.res = K.sbuf("res" + sfx, [128, 257], F32)
            W.tmpA = K.sbuf("tmpA" + sfx, [128, 257], F32)
            W.dT = K.sbuf("dT" + sfx, [128, 128], F32)
            MXN = 4 * 128 * 3 + 4 * 257 + 2 * 128
            mx = st.enter_context(nc.sbuf_tensor("mx" + sfx, [128, max(MXN, 16 * 128 + 256 + 1024)], BF16))
            o_ = 0
            def cv(name, n, rs=None, **kw):
                nonlocal o_
                ap = mx[:, o_:o_ + n]
                o_ += n
                if rs:
                    ap = ap.rearrange(rs, **kw)
                return K.view(name + sfx, ap)
            W.qT = cv("qT", 512, "p (h n) -> p h n", h=4)
            W.kT = cv("kT", 512, "p (h n) -> p h n", h=4)
            W.ktok = cv("ktok", 512)
            W.vp = cv("vp", 4 * 257, "p (h n) -> p h n", h=4)
            W.pT_ = cv("pT_", 128)
            W.kw_ = cv("kw_", 128)
            o_ = 0
            W.qTs = K.view("qTs" + sfx, mx[0:64, 0:2048].rearrange("p (h n) -> p h n", h=16))
            o_ = 2048
            W.kn = cv("kn", 256)
            W.pprev = cv("pprev", 512)
            W.pcur = cv("pcur", 512)
            W.qn = W.junk
            WS.append(W)
        c32 = K.sbuf("c32", [128, 4, 257], F32)
        cb = K.sbuf("cb", [128, 4, 257], BF16)
        kTs = [K.sbuf("kTs%d" % i, [64, 4, 128], BF16) for i in range(3)]
        vps = [K.sbuf("vps%d" % i, [128, 4, 65], BF16) for i in range(3)]
        OH1 = K.sbuf("OH1", [128, NT, 64], BF16)
        OH2 = K.sbuf("OH2", [128, NT, 64], BF16)
        if NT * 64 >= KC * DE:
            w1b.append(K.view("w1b2", OH1.t[:].rearrange("p a b -> p (a b)")[:, 0:KC * DE].rearrange("p (k n) -> p k n", k=KC)))
            w3b.append(K.view("w3b2", OH2.t[:].rearrange("p a b -> p (a b)")[:, 0:KC * DE].rearrange("p (k n) -> p k n", k=KC)))
        else:
            w1b.append(K.sbuf("w1b2", [128, KC, DE], BF16))
            w3b.append(K.sbuf("w3b2", [128, KC, DE], BF16))
        R12 = K.sbuf("R12", [128, 2, NT], F32)
        GT = K.sbuf("GT", [128, 2, NT], F32)
        base = K.sbuf("base", [128, 64], F32)
        pp = K.sbuf("pp", [128, 4, 64], F32)
        ppi = K.sbuf("ppi", [128, 64], I32)
        DEST = K.sbuf("DEST", [128, 2, NT], F32)
        DESTi = K.sbuf("DESTi", [128, 2, NT], I32)
        SRC = K.sbuf("SRC", [128, 2, NT, 2], F32)
        BE = K.sbuf("BE", [128, NBLK], F32)
        BEi = K.sbuf("BEi", [128, NBLK], I32)
        stwc = [[K.sbuf("stw%d_%d" % (c_, i), [128, 2], F32) for i in range(2)] for c_ in range(2)]
        stokc = [[K.sbuf("stok%d_%d" % (c_, i), [128, 1], I32) for i in range(2)] for c_ in range(2)]
        sil = tmpc

        ya = WS[1].tmpf
        yb = WS[0].tmpf
        sm = WS[0].sm
        bigb = WS[1].xt
        big = bigb[:, 0:1024].rearrange("p (a b) -> p a b", b=64)
        xt = WS[0].xt
        xt2 = WS[0].xt2

        def rmsnorm_mod(W, src, a_idx, b_idx, dst_bf):
            op("act", lambda e: e.activation(out=W.junk[:], in_=src[:], func=AF.Square, accum_out=W.sm[:, 0:1]),
               reads=[src], writes=[W.junk, W.sm])
            op("dve", lambda e: e.tensor_scalar(out=W.sm[:, 1:2], in0=W.sm[:, 0:1], scalar1=1.0 / D, scalar2=EPS, op0=ALU.mult, op1=ALU.add),
               reads=[W.sm], writes=[W.sm])
            op("act", lambda e: e.activation(out=W.sm[:, 2:3], in_=W.sm[:, 1:2], func=AF.Sqrt), reads=[W.sm], writes=[W.sm])
            op("dve", lambda e: e.reciprocal(out=W.sm[:, 3:4], in_=W.sm[:, 2:3]), reads=[W.sm], writes=[W.sm])
            op("dve", lambda e: e.scalar_tensor_tensor(out=W.tmpf[:], in0=src[:], scalar=W.sm[:, 3:4], in1=mod6[:, a_idx, :],
                                                       op0=ALU.mult, op1=ALU.mult), reads=[src, W.sm, mod6], writes=[W.tmpf])
            op("dve", lambda e: e.tensor_tensor(out=dst_bf[:], in0=W.tmpf[:], in1=mod6[:, b_idx, :], op=ALU.add),
               reads=[W.tmpf, mod6], writes=[dst_bf])

        def transpose8(src_bf, dstT, pbank):
            K.hold += 1
            for kc in range(KC):
                op("pe", lambda e, kc=kc: e.transpose(out=pbank[:, kc * 128:(kc + 1) * 128], in_=src_bf[:, kc * 128:(kc + 1) * 128],
                                                      identity=identb[:]), reads=[src_bf, identb], writes=[pbank], inc=(kc == KC - 1))
            K.hold -= 1
            op("act", lambda e: e.copy(out=dstT[:].rearrange("p k n -> p (k n)"), in_=pbank[:]), reads=[pbank], writes=[dstT])

        def mm_group(pbuf, out_ap, pairs, reads):
            n = len(pairs)
            K.hold += 1
            for i, (l, r) in enumerate(pairs):
                op("pe", lambda e, l=l, r=r, i=i: e.matmul(out_ap, lhsT=l, rhs=r, start=(i == 0), stop=(i == n - 1)),
                   reads=reads, writes=[pbuf], inc=(i == n - 1))
            K.hold -= 1

        def bcast_row(dst_ap, dst_buf, src_dram_ap, n):
            dma("sp", lambda e: e.dma_start(out=dst_ap, in_=src_dram_ap.partition_broadcast(128)), reads=[wsrc], writes=[dst_buf])

        dma("sp", lambda e: e.dma_start(out=cond[:], in_=c_in.ap()), reads=[wsrc], writes=[cond])
        op("act", lambda e: e.activation(out=cond[:], in_=cond[:], func=AF.Silu), reads=[cond], writes=[cond])

        x_src = x_in_b
        x_src_t = x_in
        for layer in range(depth):
            j = layer // 2
            is_ml = (layer % 2 == 0)
            last = (layer == depth - 1)
            for kc in range(KC):
                op("dve", lambda e, kc=kc: e.tensor_scalar(out=condb[:, kc, :], in0=onesf[:], scalar1=cond[:, kc:kc + 1], scalar2=None,
                                                           op0=ALU.mult), reads=[onesf, cond], writes=[condb])
            for ncol in range(12):
                dma("sp", lambda e, ncol=ncol: e.dma_start(out=rowv[0:1, :], in_=b_ada[layer:layer + 1, ncol * 512:(ncol + 1) * 512]), reads=[wsrc], writes=[rowv])
                dma("sp", lambda e, ncol=ncol: e.dma_start(
                    out=stage[:], in_=w_ada[layer].rearrange("(kc p) n -> p kc n", p=128)[:, :, ncol * 512:(ncol + 1) * 512]),
                    reads=[wsrc], writes=[stage])
                pb = ps[ncol % 2]
                pairs = [(condb[:, kc, :], stage[:, kc, :]) for kc in range(KC)]
                pairs.append((onesf[0:1, :], rowv[0:1, :]))
                mm_group(pb, pb[:], pairs, [condb, stage, onesf, rowv])
                op("dve", lambda e, ncol=ncol, pb=pb: e.tensor_copy(out=mod6[:, ncol // 2, (ncol % 2) * 512:(ncol % 2 + 1) * 512], in_=pb[:]),
                   reads=[pb], writes=[mod6])
            for (gsrc, idx) in ((n1g, 1), (n2g, 4)):
                bcast_row(WS[0].tmpf[:], WS[0].tmpf, gsrc[layer, :], D)
                op("dve", lambda e, idx=idx: e.scalar_tensor_tensor(out=mod6[:, idx, :], in0=mod6[:, idx, :], scalar=1.0, in1=WS[0].tmpf[:],
                                                                    op0=ALU.add, op1=ALU.mult), reads=[mod6, WS[0].tmpf], writes=[mod6])
            dma("pool", lambda e: e.dma_start(out=wrt[:], in_=w_rt[layer].rearrange("(kc p) n -> p kc n", p=128)), reads=[wsrc], writes=[wrt])
            bcast_row(brb[:], brb, b_rt[layer, :], 72)
            K.barrier()
            if is_ml:
                for kc in range(KC):
                    dma("pool", lambda e, kc=kc: e.dma_start(out=wmix_in_ml[:, kc, :], in_=ml_w_in[j, kc * 128:(kc + 1) * 128, :]),
                        reads=[wsrc], writes=[wmix_in_ml])
                    dma("pool", lambda e, kc=kc: e.dma_start(out=wmix_out[:, kc, :], in_=ml_w_out[j, kc * 128:(kc + 1) * 128, :]),
                        reads=[wsrc], writes=[wmix_out])
                bcast_row(bgb[:], bgb, ml_b_gate[j, :], 8)
                bcast_row(goutb[:], goutb, ml_g_out[j, :], D)
                op("dve", lambda e: e.memset(c32[:], 0.0), writes=[c32])
                op("dve", lambda e: e.memset(cb[:], 0.0), writes=[cb])
                for W_ in WS:
                    op("dve", lambda e, W_=W_: e.memset(W_.vp[:], 1.0), writes=[W_.vp])
                win = wmix_in_ml
            else:
                for kc in range(KC):
                    dma("pool", lambda e, kc=kc: e.dma_start(out=wmix_in_sw[:, kc, :], in_=sw_w_in[j, kc * 128:(kc + 1) * 128, :]),
                        reads=[wsrc], writes=[wmix_in_sw])
                    dma("pool", lambda e, kc=kc: e.dma_start(out=wmix_out[:, kc, :], in_=sw_w_out[j, kc * 128:(kc + 1) * 128, :]),
                        reads=[wsrc], writes=[wmix_out])
                bcast_row(gqb[:], gqb, sw_g_q[j, :], 64)
                bcast_row(gkb[:], gkb, sw_g_k[j, :], 64)
                bcast_row(esink[:], esink, sw_sinks[j, :], 16)
                op("act", lambda e: e.activation(out=esink[:], in_=esink[:], func=AF.Exp), reads=[esink], writes=[esink])
                for i in range(3):
                    op("dve", lambda e, i=i: e.memset(kTs[i][:], 0.0), writes=[kTs[i]])
                    op("dve", lambda e, i=i: e.memset(vps[i][:], 0.0), writes=[vps[i]])
                win = wmix_in_sw
            op("dve", lambda e: e.memset(base[:], 0.0), writes=[base])

            real_ps, real_pt = ps, pt

            class _PS:
                def __init__(self, p_):
                    self.p_ = p_
                def __getitem__(self, i):
                    return real_ps[3 * self.p_ + (i % 3)]

            class _PT:
                def __init__(self, p_):
                    self.p_ = p_
                def __getitem__(self, i):
                    return real_pt[self.p_]

            def tile_body(t, ps, pt, W):
                rows = slice(t * 128, (t + 1) * 128)
                dma("sp", lambda e: e.dma_start(out=W.xt[:], in_=x_src_t[rows, :]), reads=[x_src], writes=[W.xt])
                if layer > 0:
                    dma("pool", lambda e: e.indirect_dma_start(out=W.tmpf[:], out_offset=None, in_=y_slots[:, :],
                                                               in_offset=bass.IndirectOffsetOnAxis(ap=DESTi[:, 0, t:t + 1], axis=0)),
                        reads=[y_slots, DESTi], writes=[W.tmpf])
                    dma("pool", lambda e: e.indirect_dma_start(out=W.xt2[:], out_offset=None, in_=y_slots[:, :],
                                                               in_offset=bass.IndirectOffsetOnAxis(ap=DESTi[:, 1, t:t + 1], axis=0)),
                        reads=[y_slots, DESTi], writes=[W.xt2])
                    op("dve", lambda e: e.tensor_tensor(out=W.tmpf[:], in0=W.tmpf[:], in1=W.xt2[:], op=ALU.add), reads=[W.tmpf, W.xt2], writes=[W.tmpf])
                    op("dve", lambda e: e.tensor_tensor(out=W.xt[:], in0=W.xt[:], in1=W.tmpf[:], op=ALU.add), reads=[W.xt, W.tmpf], writes=[W.xt])
                rmsnorm_mod(W, W.xt, 1, 0, W.hnb)
                transpose8(W.hnb, W.hnT, pt[0])
                if is_ml:
                    for (dst, coff, scl) in ((W.qT, 0, 1.0), (W.kT, 512, 128 ** -0.5)):
                        pb = ps[0] if coff == 0 else ps[1]
                        for h in range(4):
                            mm_group(pb, pb[:, h * 128:(h + 1) * 128],
                                     [(win[:, kc, coff + h * 128:coff + (h + 1) * 128], W.hnT[:, kc, :]) for kc in range(KC)], [win, W.hnT])
                        op("act", lambda e, dst=dst, pb=pb, scl=scl: e.mul(out=dst[:].rearrange("p h n -> p (h n)"), in_=pb[:], mul=scl),
                           reads=[pb], writes=[dst])
                    mm_group(ps[2], ps[2][:], [(W.hnT[:, kc, :], win[:, kc, 512:1024]) for kc in range(KC)], [win, W.hnT])
                    op("act", lambda e: e.mul(out=W.ktok[:], in_=ps[2][:], mul=128 ** -0.5), reads=[ps[2]], writes=[W.ktok])
                    for half in range(2):
                        pb = ps[3 + half]
                        mm_group(pb, pb[:], [(W.hnT[:, kc, :], win[:, kc, 1024 + half * 512:1024 + (half + 1) * 512]) for kc in range(KC)], [win, W.hnT])
                        op("dve", lambda e, pb=pb, half=half: e.tensor_copy(out=W.vp[:, 2 * half:2 * half + 2, 0:256],
                                                                            in_=pb[:].rearrange("p (h n) -> p h n", h=2)),
                           reads=[pb], writes=[W.vp])
                    for half in range(2):
                        pb = ps[(5 + half) % 6]
                        mm_group(pb, pb[:], [(W.hnT[:, kc, :], win[:, kc, 2048 + half * 512:2048 + (half + 1) * 512]) for kc in range(KC)], [win, W.hnT])
                        op("act", lambda e, pb=pb, half=half: e.activation(out=W.go[:, half * 512:(half + 1) * 512], in_=pb[:], func=AF.Sigmoid),
                           reads=[pb], writes=[W.go])
                    op("dve", lambda e: e.tensor_tensor(out=W.go[:], in0=W.go[:], in1=goutb[:], op=ALU.mult), reads=[W.go, goutb], writes=[W.go])
                    mm_group(ps[1], ps[1][:, 0:8], [(W.hnT[:, kc, :], win[:, kc, 3072:3080]) for kc in range(KC)], [win, W.hnT])
                    G = W.gates
                    op("dve", lambda e: e.tensor_tensor(out=G[:, 0:8], in0=ps[1][:, 0:8], in1=bgb[:], op=ALU.add), reads=[ps[1], bgb], writes=[G])
                    op("act", lambda e: e.activation(out=G[:, 0:8], in_=G[:, 0:8], func=AF.Tanh, scale=1.0 / 15.0), reads=[G], writes=[G])
                    op("dve", lambda e: e.tensor_scalar(out=G[:, 0:8], in0=G[:, 0:8], scalar1=15.0, scalar2=None, op0=ALU.mult), reads=[G], writes=[G])
                    op("act", lambda e: e.activation(out=G[:, 8:12], in_=G[:, 4:8], func=AF.Exp, scale=-1.0), reads=[G], writes=[G])
                    op("act", lambda e: e.activation(out=G[:, 8:12], in_=G[:, 8:12], func=AF.Ln, bias=1.0), reads=[G], writes=[G])
                    op("dve", lambda e: e.tensor_scalar(out=G[:, 8:12], in0=G[:, 8:12], scalar1=-1.0, scalar2=None, op0=ALU.mult), reads=[G], writes=[G])
                    mm_group(ps[0], ps[0][:, 0:4], [(triinc[:], G[:, 8:12])], [triinc, G])
                    mm_group(ps[0], ps[0][:, 4:8], [(onesf[:], G[:, 8:12])], [onesf, G])
                    op("dve", lambda e: e.tensor_copy(out=G[:, 12:16], in_=ps[0][:, 0:4]), reads=[ps[0]], writes=[G])
                    op("dve", lambda e: e.tensor_copy(out=G[:, 20:24], in_=ps[0][:, 4:8]), reads=[ps[0]], writes=[G])
                    op("dve", lambda e: e.tensor_tensor(out=G[:, 16:20], in0=G[:, 0:4], in1=G[:, 12:16], op=ALU.subtract), reads=[G], writes=[G])
                    op("dve", lambda e: e.tensor_tensor(out=G[:, 24:28], in0=G[:, 16:20], in1=G[:, 20:24], op=ALU.add), reads=[G], writes=[G])
                    op("act", lambda e: e.activation(out=G[:, 24:28], in_=G[:, 24:28], func=AF.Exp), reads=[G], writes=[G])
                    op("act", lambda e: e.activation(out=G[:, 28:32], in_=G[:, 20:24], func=AF.Exp), reads=[G], writes=[G])
                    op("act", lambda e: e.activation(out=G[:, 32:36], in_=G[:, 12:16], func=AF.Exp), reads=[G], writes=[G])
                    op("pe", lambda e: e.transpose(out=ps[0][0:4, 128:256], in_=G[:, 12:16], identity=identf[:]), reads=[G, identf], writes=[ps[0]])
                    op("dve", lambda e: e.tensor_copy(out=W.bT[:], in_=ps[0][0:4, 128:256]), reads=[ps[0]], writes=[W.bT])
                    for h in range(4):
                        mm_group(ps[1], ps[1][:, 0:128], [(sel4[h][:], W.bT[:]), (identf[:], negm[:])], [sel4[h], W.bT, identf, negm])
                        op("act", lambda e, h=h: e.activation(out=W.dT[:], in_=ps[1][:, 0:128], func=AF.Exp, bias=G[:, 16 + h:17 + h]),
                           reads=[ps[1], G], writes=[W.dT])
                        mm_group(ps[2], ps[2][:, 0:128], [(W.kT[:, h, :], W.qT[:, h, :])], [W.kT, W.qT])
                        op("dve", lambda e: e.tensor_tensor(out=W.pT_[:], in0=ps[2][:, 0:128], in1=W.dT[:], op=ALU.mult), reads=[ps[2], W.dT], writes=[W.pT_])
                        mm_group(ps[3], ps[3][:, 0:257], [(W.pT_[:], W.vp[:, h, :])], [W.pT_, W.vp])
                        if W.p == 1:
                            K.wait_for(("st", h))
                        mm_group(ps[4], ps[4][:, 0:257], [(W.qT[:, h, :], cb[:, h, :])], [W.qT, cb])
                        op("act", lambda e, h=h: e.activation(out=W.tmpA[:], in_=ps[4][:, 0:257], func=AF.Copy, scale=G[:, 32 + h:33 + h]),
                           reads=[ps[4], G], writes=[W.tmpA])
                        op("dve", lambda e: e.tensor_tensor(out=W.res[:], in0=W.tmpA[:], in1=ps[3][:, 0:257], op=ALU.add), reads=[W.tmpA, ps[3]], writes=[W.res])
                        op("pool", lambda e, h=h: e.tensor_scalar(out=W.kw_[:], in0=W.ktok[:, h * 128:(h + 1) * 128], scalar1=G[:, 24 + h:25 + h], scalar2=None,
                                                                  op0=ALU.mult), reads=[W.ktok, G], writes=[W.kw_])
                        mm_group(ps[5], ps[5][:, 0:257], [(W.kw_[:], W.vp[:, h, :])], [W.kw_, W.vp])
                        op("dve", lambda e, h=h: e.scalar_tensor_tensor(out=c32[:, h, :], in0=c32[:, h, :], scalar=G[:, 28 + h:29 + h], in1=ps[5][:, 0:257],
                                                                        op0=ALU.mult, op1=ALU.add), reads=[c32, G, ps[5]], writes=[c32])
                        op("pool", lambda e, h=h: e.tensor_copy(out=cb[:, h, :], in_=c32[:, h, :]), reads=[c32], writes=[cb])
                        if W.p == 0:
                            K.signal(("st", h))
                        S_ = W.sm
                        op("dve", lambda e: e.tensor_scalar(out=S_[:, 7:8], in0=W.res[:, 256:257], scalar1=-1.0, scalar2=None, op0=ALU.mult),
                           reads=[W.res], writes=[S_])
                        op("dve", lambda e: e.tensor_tensor(out=S_[:, 8:9], in0=S_[:, 7:8], in1=W.res[:, 256:257], op=ALU.max), reads=[W.res, S_], writes=[S_])
                        op("dve", lambda e: e.tensor_scalar(out=S_[:, 8:9], in0=S_[:, 8:9], scalar1=1.0, scalar2=None, op0=ALU.max), reads=[S_], writes=[S_])
                        op("dve", lambda e: e.reciprocal(out=S_[:, 9:10], in_=S_[:, 8:9]), reads=[S_], writes=[S_])
                        op("act", lambda e: e.activation(out=W.junk[:, 0:256], in_=W.res[:, 0:256], func=AF.Square, accum_out=S_[:, 10:11]),
                           reads=[W.res], writes=[W.junk, S_])
                        op("dve", lambda e: e.tensor_tensor(out=S_[:, 11:12], in0=S_[:, 9:10], in1=S_[:, 9:10], op=ALU.mult), reads=[S_], writes=[S_])
                        op("dve", lambda e: e.scalar_tensor_tensor(out=S_[:, 12:13], in0=S_[:, 10:11], scalar=1.0 / 256.0, in1=S_[:, 11:12],
                                                                   op0=ALU.mult, op1=ALU.mult), reads=[S_], writes=[S_])
                        op("dve", lambda e: e.tensor_scalar(out=S_[:, 12:13], in0=S_[:, 12:13], scalar1=EPS, scalar2=None, op0=ALU.add), reads=[S_], writes=[S_])
                        op("act", lambda e: e.activation(out=S_[:, 13:14], in_=S_[:, 12:13], func=AF.Sqrt), reads=[S_], writes=[S_])
                        op("dve", lambda e: e.reciprocal(out=S_[:, 14:15], in_=S_[:, 13:14]), reads=[S_], writes=[S_])
                        op("dve", lambda e: e.tensor_tensor(out=S_[:, 15:16], in0=S_[:, 14:15], in1=S_[:, 9:10], op=ALU.mult), reads=[S_], writes=[S_])
                        op("dve", lambda e, h=h: e.scalar_tensor_tensor(out=W.hg[:, h * 256:(h + 1) * 256], in0=W.res[:, 0:256], scalar=S_[:, 15:16],
                                                                        in1=W.go[:, h * 256:(h + 1) * 256], op0=ALU.mult, op1=ALU.mult),
                           reads=[W.res, S_, W.go], writes=[W.hg])
                else:
                    cur, prv = t % 3, (t + 2) % 3
                    for half in range(2):
                        mm_group(ps[half], ps[half][:], [(W.hnT[:, kc, :], win[:, kc, half * 512:(half + 1) * 512]) for kc in range(KC)], [win, W.hnT])
                    mm_group(ps[2], ps[2][:], [(W.hnT[:, kc, :], win[:, kc, 1024:1536]) for kc in range(KC)], [win, W.hnT])
                    for half in range(2):
                        op("act", lambda e, half=half: e.activation(out=W.tmpf[:, half * 512:(half + 1) * 512], in_=ps[half][:], func=AF.Square),
                           reads=[ps[half]], writes=[W.tmpf])
                    op("dve", lambda e: e.tensor_reduce(out=W.ssq[:, 0:16], in_=W.tmpf[:].rearrange("p (h d) -> p h d", d=64), axis=AX.X, op=ALU.add),
                       reads=[W.tmpf], writes=[W.ssq])
                    op("act", lambda e: e.activation(out=W.xt2[:, 0:256], in_=ps[2][:, 0:256], func=AF.Square), reads=[ps[2]], writes=[W.xt2])
                    op("dve", lambda e: e.tensor_reduce(out=W.ssq[:, 16:20], in_=W.xt2[:, 0:256].rearrange("p (h d) -> p h d", d=64), axis=AX.X, op=ALU.add),
                       reads=[W.xt2], writes=[W.ssq])
                    op("dve", lambda e: e.tensor_scalar(out=W.ssq[:, 0:20], in0=W.ssq[:, 0:20], scalar1=1.0 / 64.0, scalar2=EPS, op0=ALU.mult, op1=ALU.add),
                       reads=[W.ssq], writes=[W.ssq])
                    op("act", lambda e: e.activation(out=W.ssq[:, 0:20], in_=W.ssq[:, 0:20], func=AF.Sqrt), reads=[W.ssq], writes=[W.ssq])
                    op("dve", lambda e: e.reciprocal(out=W.ssq[:, 0:20], in_=W.ssq[:, 0:20]), reads=[W.ssq], writes=[W.ssq])
                    for half in range(2):
                        op("dve", lambda e, half=half: e.tensor_tensor(
                            out=W.tmpf[:, half * 512:(half + 1) * 512].rearrange("p (h d) -> p h d", d=64),
                            in0=ps[half][:].rearrange("p (h d) -> p h d", d=64),
                            in1=W.ssq[:, half * 8:(half + 1) * 8].unsqueeze(2).broadcast_to([128, 8, 64]), op=ALU.mult),
                            reads=[ps[half], W.ssq], writes=[W.tmpf])
                    op("dve", lambda e: e.tensor_tensor(out=W.qn[:].rearrange("p (h d) -> p h d", d=64), in0=W.tmpf[:].rearrange("p (h d) -> p h d", d=64),
                                                         in1=gqb[:].unsqueeze(1).broadcast_to([128, 16, 64]), op=ALU.mult),
                       reads=[W.tmpf, gqb], writes=[W.qn])
                    op("dve", lambda e: e.tensor_tensor(out=W.xt2[:, 0:256].rearrange("p (h d) -> p h d", d=64),
                                                        in0=ps[2][:, 0:256].rearrange("p (h d) -> p h d", d=64),
                                                        in1=W.ssq[:, 16:20].unsqueeze(2).broadcast_to([128, 4, 64]), op=ALU.mult),
                       reads=[ps[2], W.ssq], writes=[W.xt2])
                    op("pool", lambda e: e.tensor_tensor(out=W.kn[:].rearrange("p (h d) -> p h d", d=64), in0=W.xt2[:, 0:256].rearrange("p (h d) -> p h d", d=64),
                                                         in1=gkb[:].unsqueeze(1).broadcast_to([128, 4, 64]), op=ALU.mult),
                       reads=[W.xt2, gkb], writes=[W.kn])
                    op("act", lambda e: e.copy(out=vps[cur][:, :, 0:64], in_=ps[2][:, 256:512].rearrange("p (h d) -> p h d", d=64)),
                       reads=[ps[2]], writes=[vps[cur]])
                    op("pool", lambda e: e.memset(vps[cur][:, :, 64:65], 1.0), reads=[vps[cur]], writes=[vps[cur]])
                    for half in range(2):
                        pb = pt[0]
                        K.hold += 1
                        for h8 in range(8):
                            hh = half * 8 + h8
                            op("pe", lambda e, hh=hh, h8=h8, pb=pb: e.transpose(out=pb[0:64, h8 * 128:(h8 + 1) * 128], in_=W.qn[:, hh * 64:(hh + 1) * 64],
                                                                                identity=identb[:]), reads=[W.qn, identb], writes=[pb], inc=(h8 == 7))
                        K.hold -= 1
                        op("act", lambda e, half=half, pb=pb: e.copy(out=W.qTs[:, half * 8:(half + 1) * 8, :].rearrange("p h n -> p (h n)"), in_=pb[0:64, :]),
                           reads=[pb], writes=[W.qTs])
                    K.hold += 1
                    for g in range(4):
                        op("pe", lambda e, g=g: e.transpose(out=pt[0][0:64, g * 128:(g + 1) * 128], in_=W.kn[:, g * 64:(g + 1) * 64], identity=identb[:]),
                           reads=[W.kn, identb], writes=[pt[0]], inc=(g == 3))
                    K.hold -= 1
                    op("act", lambda e: e.copy(out=kTs[cur][:].rearrange("p h n -> p (h n)"), in_=pt[0][0:64, 0:512]), reads=[pt[0]], writes=[kTs[cur]])
                    if W.p == 0:
                        K.signal("kv")
                    else:
                        K.wait_for("kv")
                    for g in range(4):
                        qv = W.qTs[:, 4 * g:4 * g + 4, :].rearrange("p h n -> p (h n)")
                        mm_group(ps[3], ps[3][:], [(kTs[prv][:, g, :], qv), (identb[:], negprev[:].rearrange("p h q -> p (h q)"))],
                                 [kTs[prv], W.qTs, identb, negprev])
                        mm_group(ps[4], ps[4][:], [(kTs[cur][:, g, :], qv), (identb[:], negcur[:].rearrange("p h q -> p (h q)"))],
                                 [kTs[cur], W.qTs, identb, negcur])
                        op("act", lambda e: e.activation(out=W.pprev[:], in_=ps[3][:], func=AF.Exp, scale=0.125), reads=[ps[3]], writes=[W.pprev])
                        op("act", lambda e: e.activation(out=W.pcur[:], in_=ps[4][:], func=AF.Exp, scale=0.125), reads=[ps[4]], writes=[W.pcur])
                        for hh in range(4):
                            mm_group(ps[5], ps[5][:, hh * 65:(hh + 1) * 65],
                                     [(W.pprev[:, hh * 128:(hh + 1) * 128], vps[prv][:, g, :]), (W.pcur[:, hh * 128:(hh + 1) * 128], vps[cur][:, g, :])],
                                     [W.pprev, W.pcur, vps[prv], vps[cur]])
                        pv = ps[5][:, 0:260].rearrange("p (h d) -> p h d", d=65)
                        op("dve", lambda e, g=g, pv=pv: e.tensor_tensor(out=W.ssq[:, 20:24], in0=pv[:, :, 64], in1=esink[:, 4 * g:4 * g + 4], op=ALU.add),
                           reads=[ps[5], esink], writes=[W.ssq])
                        op("dve", lambda e: e.reciprocal(out=W.ssq[:, 24:28], in_=W.ssq[:, 20:24]), reads=[W.ssq], writes=[W.ssq])
                        op("dve", lambda e, g=g, pv=pv: e.tensor_tensor(out=W.hg[:, g * 256:(g + 1) * 256].rearrange("p (h d) -> p h d", d=64), in0=pv[:, :, 0:64],
                                                                        in1=W.ssq[:, 24:28].unsqueeze(2).broadcast_to([128, 4, 64]), op=ALU.mult),
                           reads=[ps[5], W.ssq], writes=[W.hg])
                transpose8(W.hg, W.hgT, pt[1])
                for half in range(2):
                    pb = ps[half]
                    mm_group(pb, pb[:], [(W.hgT[:, kc, :], wmix_out[:, kc, half * 512:(half + 1) * 512]) for kc in range(KC)], [W.hgT, wmix_out])
                    op("dve", lambda e, pb=pb, half=half: e.tensor_tensor(out=W.tmpf[:, half * 512:(half + 1) * 512], in0=pb[:],
                                                                          in1=mod6[:, 2, half * 512:(half + 1) * 512], op=ALU.mult),
                       reads=[pb, mod6], writes=[W.tmpf])
                op("dve", lambda e: e.tensor_tensor(out=W.xt2[:], in0=W.tmpf[:], in1=W.xt[:], op=ALU.add), reads=[W.tmpf, W.xt], writes=[W.xt2])
                dma("pool", lambda e: e.dma_start(out=xs[rows, :], in_=W.xt2[:]), reads=[W.xt2], writes=[xs])
                rmsnorm_mod(W, W.xt2, 4, 3, W.hnb)
                dma("pool", lambda e: e.dma_start(out=hn2d[rows, :], in_=W.hnb[:]), reads=[W.hnb], writes=[hn2d], group=("hn2", layer))
                transpose8(W.hnb, W.hnT, pt[0])
                mm_group(ps[2], ps[2][:, 0:72], [(W.hnT[:, kc, :], wrt[:, kc, :]) for kc in range(KC)], [W.hnT, wrt])
                op("dve", lambda e: e.tensor_tensor(out=W.lg[:], in0=ps[2][:, 0:72], in1=brb[:], op=ALU.add), reads=[ps[2], brb], writes=[W.lg])
                R = W.rsm
                op("dve", lambda e: e.tensor_reduce(out=R[:, 0:1], in_=W.lg[:, 0:8], axis=AX.X, op=ALU.max), reads=[W.lg], writes=[R])
                op("dve", lambda e: e.tensor_scalar(out=R[:, 1:2], in0=R[:, 0:1], scalar1=-1.0, scalar2=None, op0=ALU.mult), reads=[R], writes=[R])
                op("dve", lambda e: e.tensor_scalar(out=R[:, 4:12], in0=W.lg[:, 0:8], scalar1=R[:, 0:1], scalar2=None, op0=ALU.is_equal), reads=[W.lg, R], writes=[R])
                op("act", lambda e: e.activation(out=R[:, 48:56], in_=W.lg[:, 0:8], func=AF.Exp, bias=R[:, 1:2], accum_out=R[:, 2:3]), reads=[W.lg, R], writes=[R])
                op("dve", lambda e: e.reciprocal(out=R[:, 3:4], in_=R[:, 2:3]), reads=[R], writes=[R])
                op("dve", lambda e: e.tensor_tensor(out=W.t64[:].rearrange("p (g x) -> p g x", g=8), in0=W.lg[:, 8:72].rearrange("p (g x) -> p g x", g=8),
                                                    in1=R[:, 4:12].unsqueeze(2).broadcast_to([128, 8, 8]), op=ALU.mult), reads=[W.lg, R], writes=[W.t64])
                op("dve", lambda e: e.tensor_reduce(out=R[:, 12:20], in_=W.t64[:].rearrange("p (g x) -> p x g", g=8), axis=AX.X, op=ALU.add),
                   reads=[W.t64], writes=[R])
                op("dve", lambda e: e.max(out=R[:, 20:28], in_=R[:, 12:20]), reads=[R], writes=[R])
                op("dve", lambda e: e.tensor_scalar(out=R[:, 28:36], in0=R[:, 12:20], scalar1=R[:, 20:21], scalar2=None, op0=ALU.is_equal), reads=[R], writes=[R])
                op("dve", lambda e: e.tensor_scalar(out=R[:, 36:44], in0=R[:, 12:20], scalar1=R[:, 21:22], scalar2=None, op0=ALU.is_equal), reads=[R], writes=[R])
                op("dve", lambda e: e.tensor_tensor(out=R[:, 44:45], in0=R[:, 20:21], in1=R[:, 21:22], op=ALU.subtract), reads=[R], writes=[R])
                op("act", lambda e: e.activation(out=R[:, 45:46], in_=R[:, 44:45], func=AF.Sigmoid), reads=[R], writes=[R])
                op("dve", lambda e: e.tensor_tensor(out=GT[:, 0, t:t + 1], in0=R[:, 45:46], in1=R[:, 3:4], op=ALU.mult), reads=[R], writes=[GT])
                op("dve", lambda e: e.tensor_tensor(out=GT[:, 1, t:t + 1], in0=R[:, 3:4], in1=GT[:, 0, t:t + 1], op=ALU.subtract), reads=[R, GT], writes=[GT])
                for (OH, c0) in ((OH1, 28), (OH2, 36)):
                    op("dve", lambda e, OH=OH, c0=c0: e.tensor_tensor(out=OH[:, t, :].rearrange("p (g x) -> p g x", g=8),
                                                                      in0=R[:, 4:12].unsqueeze(2).broadcast_to([128, 8, 8]),
                                                                      in1=R[:, c0:c0 + 8].unsqueeze(1).broadcast_to([128, 8, 8]), op=ALU.mult),
                       reads=[R], writes=[OH])
                op("dve", lambda e: e.tensor_tensor(out=W.ohs[:], in0=OH1[:, t, :], in1=OH2[:, t, :], op=ALU.add), reads=[OH1, OH2], writes=[W.ohs])
                mm_group(ps[3], ps[3][:, 0:64], [(tristrb[:], W.ohs[:])], [tristrb, W.ohs])
                mm_group(ps[3], ps[3][:, 64:128], [(onesb[:], W.ohs[:])], [onesb, W.ohs])
                if W.p == 1:
                    K.wait_for("base")
                op("dve", lambda e: e.tensor_tensor(out=W.rtot[:], in0=ps[3][:, 0:64], in1=base[:], op=ALU.add), reads=[ps[3], base], writes=[W.rtot])
                op("dve", lambda e: e.tensor_tensor(out=base[:], in0=ps[3][:, 64:128], in1=base[:], op=ALU.add), reads=[ps[3], base], writes=[base])
                if W.p == 0:
                    K.signal("base")
                for k_, OH in ((0, OH1), (1, OH2)):
                    op("dve", lambda e, OH=OH: e.tensor_tensor(out=W.t64[:], in0=OH[:, t, :], in1=W.rtot[:], op=ALU.mult), reads=[OH, W.rtot], writes=[W.t64])
                    op("dve", lambda e, k_=k_: e.tensor_reduce(out=R12[:, k_, t:t + 1], in_=W.t64[:], axis=AX.X, op=ALU.add), reads=[W.t64], writes=[R12])

            for t0_ in range(0, NT, 2):
                K.interleave([lambda t=t0_ + q_: tile_body(t, _PS(t % 2), _PT(t % 2), WS[t % 2]) for q_ in range(min(2, NT - t0_))])

            op("dve", lambda e: e.tensor_scalar(out=pp[:, 0, :], in0=base[:], scalar1=1.0 / BS, scalar2=(BS / 2 - 0.5) / BS, op0=ALU.mult, op1=ALU.add),
               reads=[base], writes=[pp])
            op("dve", lambda e: e.tensor_copy(out=ppi[:], in_=pp[:, 0, :]), reads=[pp], writes=[ppi])
            op("dve", lambda e: e.tensor_copy(out=pp[:, 0, :], in_=ppi[:]), reads=[ppi], writes=[pp])
            op("dve", lambda e: e.tensor_scalar(out=pp[:, 0, :], in0=pp[:, 0, :], scalar1=float(BS), scalar2=None, op0=ALU.mult), reads=[pp], writes=[pp])
            op("dve", lambda e: e.tensor_tensor_scan(out=pp[:, 1, :], data0=onesf[:, 0:64], data1=pp[:, 0, :], initial=0.0, op0=ALU.mult, op1=ALU.add),
               reads=[pp, onesf], writes=[pp])
            op("dve", lambda e: e.tensor_tensor(out=pp[:, 2, :], in0=pp[:, 1, :], in1=pp[:, 0, :], op=ALU.subtract), reads=[pp], writes=[pp])
            for k_, OH in ((0, OH1), (1, OH2)):
                for c0 in range(0, NT, 16):
                    n = min(16, NT - c0)
                    op("dve", lambda e, OH=OH, c0=c0, n=n: e.tensor_tensor(out=big[:, 0:n, :], in0=OH[:, c0:c0 + n, :],
                                                                           in1=pp[:, 2, :].unsqueeze(1).broadcast_to([128, n, 64]), op=ALU.mult),
                       reads=[OH, pp], writes=[bigb])
                    op("dve", lambda e, k_=k_, c0=c0, n=n: e.tensor_reduce(out=DEST[:, k_, c0:c0 + n], in_=big[:, 0:n, :], axis=AX.X, op=ALU.add),
                       reads=[bigb], writes=[DEST])
            op("dve", lambda e: e.tensor_tensor(out=DEST[:], in0=DEST[:], in1=R12[:], op=ALU.add), reads=[DEST, R12], writes=[DEST])
            op("dve", lambda e: e.tensor_copy(out=DESTi[:], in_=DEST[:]), reads=[DEST], writes=[DESTi])
            for k_ in range(2):
                op("dve", lambda e, k_=k_: e.tensor_copy(out=SRC[:, k_, :, 0], in_=tokid[:]), reads=[tokid], writes=[SRC])
                op("dve", lambda e, k_=k_: e.tensor_copy(out=SRC[:, k_, :, 1], in_=GT[:, k_, :]), reads=[GT], writes=[SRC])
            for c0 in range(0, NBLK, 16):
                n = min(16, NBLK - c0)
                op("dve", lambda e, c0=c0, n=n: e.tensor_scalar(out=sm[:, 16:16 + n], in0=jb[:, 0:n], scalar1=float(c0 * BS), scalar2=None, op0=ALU.add),
                   reads=[jb], writes=[sm])
                op("dve", lambda e, n=n: e.tensor_tensor(out=big[:, 0:n, :], in0=pp[:, 1, :].unsqueeze(1).broadcast_to([128, n, 64]),
                                                         in1=sm[:, 16:16 + n].unsqueeze(2).broadcast_to([128, n, 64]), op=ALU.is_le),
                   reads=[pp, sm], writes=[bigb])
                op("dve", lambda e, c0=c0, n=n: e.tensor_reduce(out=BE[:, c0:c0 + n], in_=big[:, 0:n, :], axis=AX.X, op=ALU.add), reads=[bigb], writes=[BE])
            op("dve", lambda e: e.tensor_scalar(out=BE[:], in0=BE[:], scalar1=float(NE - 1), scalar2=128.0, op0=ALU.min, op1=ALU.mult), reads=[BE], writes=[BE])
            op("dve", lambda e: e.tensor_scalar(out=BE[:], in0=BE[:], scalar1=iota_p[:, 0:1], scalar2=float(layer * NE * 128), op0=ALU.add, op1=ALU.add),
               reads=[BE, iota_p], writes=[BE])
            op("dve", lambda e: e.tensor_copy(out=BEi[:], in_=BE[:]), reads=[BE], writes=[BEi])
            NR_ = NSLOT // 128
            for r0_ in range(0, NR_, 16):
                dma("sp", lambda e, r0_=r0_: e.dma_start(out=slot_tw.ap().rearrange("(p r) c -> p r c", p=128)[:, r0_:r0_ + 16, :], in_=padinit[:]),
                    reads=[padinit], writes=[slot_tw])
            for k_ in range(2):
                for t in range(NT):
                    dma("pool", lambda e, k_=k_, t=t: e.indirect_dma_start(
                        out=slot_tw[:, :], out_offset=bass.IndirectOffsetOnAxis(ap=DESTi[:, k_, t:t + 1], axis=0),
                        in_=SRC[:, k_, t, :], in_offset=None), reads=[SRC, DESTi], writes=[slot_tw], sem_buf=SRC)

            K.barrier()

            def expert_block(jblk, ps, pt, c_):
                pb_ = jblk % 3
                xg, xTm, hTm, stw, stok = xgc[c_], xTmc[c_], hTmc[c_], stwc[c_], stokc[c_]
                yo = [WS[c_].xt, WS[c_].xt2]
                sil = WS[c_].tmpA
                for (wb, wr_) in ((w1b[pb_], w1r), (w3b[pb_], w3r), (w2b[pb_], w2r)):
                    dma("pool", lambda e, wb=wb, wr_=wr_: e.indirect_dma_start(
                        out=wb[:].rearrange("p k n -> p (k n)"), out_offset=None, in_=wr_[:, :],
                        in_offset=bass.IndirectOffsetOnAxis(ap=BEi[:, jblk:jblk + 1], axis=0)), reads=[wsrc, BEi], writes=[wb])
                for sub in range(BS // 128):
                    r0 = jblk * BS + sub * 128
                    dma("sp", lambda e, sub=sub, r0=r0: e.dma_start(out=stw[sub][:], in_=slot_tw[r0:r0 + 128, :]), reads=[slot_tw], writes=[stw[sub]])
                    op("dve", lambda e, sub=sub: e.tensor_copy(out=stok[sub][:], in_=stw[sub][:, 0:1]), reads=[stw[sub]], writes=[stok[sub]])
                    dma("pool", lambda e, sub=sub: e.indirect_dma_start(
                        out=xg[sub][:], out_offset=None, in_=hn2d[:, :],
                        in_offset=bass.IndirectOffsetOnAxis(ap=stok[sub][:, 0:1], axis=0)), reads=[hn2d, stok[sub]], writes=[xg[sub]])
                    K.hold += 1
                    for kc in range(KC):
                        op("pe", lambda e, kc=kc, sub=sub: e.transpose(out=pt[0][:, kc * 128:(kc + 1) * 128], in_=xg[sub][:, kc * 128:(kc + 1) * 128],
                                                                       identity=identb[:]), reads=[xg[sub], identb], writes=[pt[0]], inc=(kc == KC - 1))
                    K.hold -= 1
                    op("act", lambda e, sub=sub: e.copy(out=xTm[:, :, sub * 128:(sub + 1) * 128], in_=pt[0][:].rearrange("p (k n) -> p k n", k=KC)),
                       reads=[pt[0]], writes=[xTm])
                for m in range(3):
                    p1, p3 = ps[(2 * m) % 3], ps[(2 * m + 1) % 3]
                    mm_group(p1, p1[:, 0:BS], [(w1b[pb_][:, kc, m * 128:(m + 1) * 128], xTm[:, kc, :]) for kc in range(KC)], [w1b[pb_], xTm])
                    mm_group(p3, p3[:, 0:BS], [(w3b[pb_][:, kc, m * 128:(m + 1) * 128], xTm[:, kc, :]) for kc in range(KC)], [w3b[pb_], xTm])
                    op("act", lambda e, p1=p1: e.activation(out=sil[:, 0:BS], in_=p1[:, 0:BS], func=AF.Silu), reads=[p1], writes=[sil])
                    op("dve", lambda e, p3=p3, m=m: e.tensor_tensor(out=hTm[:, m, :], in0=p3[:, 0:BS], in1=sil[:, 0:BS], op=ALU.mult), reads=[p3, sil], writes=[hTm])
                for sub in range(BS // 128):
                    r0 = jblk * BS + sub * 128
                    for half in range(2):
                        pb = ps[(2 * sub + half) % 3]
                        mm_group(pb, pb[:], [(hTm[:, c, sub * 128:(sub + 1) * 128], w2b[pb_][:, c, half * 512:(half + 1) * 512]) for c in range(3)],
                                 [hTm, w2b[pb_]])
                        op("act", lambda e, pb=pb, sub=sub, half=half: e.activation(out=yo[sub][:, half * 512:(half + 1) * 512], in_=pb[:], func=AF.Copy,
                                                                                    scale=stw[sub][:, 1:2]), reads=[pb, stw[sub]], writes=[yo[sub]])
                        op("dve", lambda e, sub=sub, half=half: e.tensor_tensor(out=yo[sub][:, half * 512:(half + 1) * 512], in0=yo[sub][:, half * 512:(half + 1) * 512],
                                                                                in1=mod6[:, 5, half * 512:(half + 1) * 512], op=ALU.mult),
                           reads=[yo[sub], mod6], writes=[yo[sub]])
                    dma("act", lambda e, sub=sub, r0=r0: e.dma_start(out=y_slots[r0:r0 + 128, :], in_=yo[sub][:]), reads=[yo[sub]], writes=[y_slots],
                        group=("ys", layer))

            for j0_ in range(0, NBLK, 2):
                K.interleave([lambda j=j0_ + q_: expert_block(j, _PS(j % 2), _PT(j % 2), j % 2) for q_ in range(min(2, NBLK - j0_))])

            K.barrier()
            dst_t = out if last else xs.t
            dst_b = out_b if last else xs

            def combine_tile(t, c_):
                rows = slice(t * 128, (t + 1) * 128)
                xt, xt2, ya, yb = WS[c_].xt, WS[c_].xt2, WS[c_].tmpf, ybc[c_]
                dma("sp", lambda e: e.dma_start(out=xt[:], in_=xs[rows, :]), reads=[xs], writes=[xt])
                dma("pool", lambda e, t=t: e.indirect_dma_start(out=ya[:], out_offset=None, in_=y_slots[:, :],
                                                               in_offset=bass.IndirectOffsetOnAxis(ap=DESTi[:, 0, t:t + 1], axis=0)),
                    reads=[y_slots, DESTi], writes=[ya])
                dma("pool", lambda e, t=t: e.indirect_dma_start(out=yb[:], out_offset=None, in_=y_slots[:, :],
                                                               in_offset=bass.IndirectOffsetOnAxis(ap=DESTi[:, 1, t:t + 1], axis=0)),
                    reads=[y_slots, DESTi], writes=[yb])
                op("dve", lambda e: e.tensor_tensor(out=ya[:], in0=ya[:], in1=yb[:], op=ALU.add), reads=[ya, yb], writes=[ya])
                op("dve", lambda e: e.tensor_tensor(out=xt2[:], in0=ya[:], in1=xt[:], op=ALU.add), reads=[ya, xt], writes=[xt2])
                if last:
                    dma("act", lambda e: e.dma_start(out=dst_t[rows, :], in_=xt2[:]), reads=[xt2], writes=[dst_b], group=("out", layer))
                else:
                    dma("act", lambda e: e.dma_start(out=dst_t[rows, :], in_=xt2[:]), reads=[xt2], writes=[dst_b])
                if debug:
                    dma("sp", lambda e: e.dma_start(out=dbg[layer * S + t * 128:layer * S + (t + 1) * 128, :], in_=xt2[:]), reads=[xt2], writes=[dbg_b])

            if last:
                for t0_ in range(0, NT, 2):
                    K.interleave([lambda t=t0_ + q_: combine_tile(t, t % 2) for q_ in range(min(2, NT - t0_))])
            x_src = xs
            x_src_t = xs.t
            K.barrier()
        K.finish([out_b] + ([dbg_b] if debug else []))
        K.barrier()
        ninst = K.ninst
    return nc, ninst


def prep_weights(inp):
    f = lambda a: np.ascontiguousarray(np.asarray(a, dtype=np.float32))
    w = {}
    for k in ("w_ada", "b_ada", "norm1_g", "norm2_g", "ml_w_in", "ml_b_gate", "ml_g_out", "ml_w_out",
              "sw_w_in", "sw_g_q", "sw_g_k", "sw_sinks", "sw_w_out"):
        w[k] = f(inp[k])
    w["w_rt"] = f(np.concatenate([np.asarray(inp["moe_w_group"]), np.asarray(inp["moe_w_router"])], axis=-1))
    w["b_rt"] = f(np.concatenate([np.asarray(inp["moe_b_group"]), np.asarray(inp["moe_b_router"])], axis=-1))
    w1 = np.asarray(inp["moe_w1"]).reshape(DEPTH, NE, KC, 128, DE)
    w["w1r"] = f(w1.transpose(0, 1, 3, 2, 4).reshape(DEPTH * NE * 128, KC * DE))
    w3 = np.asarray(inp["moe_w3"]).reshape(DEPTH, NE, KC, 128, DE)
    w["w3r"] = f(w3.transpose(0, 1, 3, 2, 4).reshape(DEPTH * NE * 128, KC * DE))
    w2 = np.asarray(inp["moe_w2"]).reshape(DEPTH, NE, 3, 128, D)
    w["w2r"] = f(w2.transpose(0, 1, 3, 2, 4).reshape(DEPTH * NE * 128, 3 * D))
    return w


def run(inp, S, depth=DEPTH, trace=False, debug=False):
    x = np.asarray(inp["x"], dtype=np.float32)
    c = np.asarray(inp["c"], dtype=np.float32)
    B = x.shape[0]
    w = prep_weights(inp)
    nc, ninst = build_program(S, depth, debug)
    in_maps = []
    for core in range(8):
        b = (core // 2) % B
        m = dict(w)
        m["x"] = np.ascontiguousarray(x[b, :S])
        m["c"] = np.ascontiguousarray(c[b].reshape(KC, 128).T)
        m["coff"] = np.full((128, 1), 0.0 if core % 2 == 0 else 1.0e7, dtype=np.float32)
        in_maps.append(m)
    res = run_bass_kernel_spmd(nc, in_maps, core_ids=list(range(8)), **({"trace": True} if trace else {}))
    outs = np.stack([res.results[2 * b]["out"] for b in range(B)], axis=0)
    return outs, res


def kernel(**inputs):
    S = np.asarray(inputs["x"]).shape[1]
    outs, _ = run(inputs, S, DEPTH)
    return outs.astype(np.float32)
```

```python
import math
import threading
from contextlib import ExitStack

import numpy as np
import concourse.bass as bass
import concourse.mybir as mybir
from concourse.bass_utils import run_bass_kernel_spmd

F32 = mybir.dt.float32
BF16 = mybir.dt.bfloat16
I32 = mybir.dt.int32
AF = mybir.ActivationFunctionType
ALU = mybir.AluOpType
AX = mybir.AxisListType

D = 1024
KC = 8
DEPTH = 4
EPS = 1e-6
ML_IN = 3080
SW_IN = 1536
NE = 64
DE = 384
BS = 256
NEG = -30000.0


class Buf:
    def __init__(self, K, name, t, dram=False):
        self.K = K
        self.name = name
        self.t = t
        self.dram = dram
        self.w = {}
        self.r = {}
        self.dsem = None
        self.dcnt = 0
        self.wgroup = None
        K.all_bufs.append(self)

    def __getitem__(self, k):
        return self.t[k]

    def ap(self):
        return self.t.ap() if self.dram else self.t[:]


class Eng:
    def __init__(self, name, h, sem):
        self.name = name
        self.h = h
        self.sem = sem
        self.cnt = 0
        self.waited = {}


class Kern:
    def __init__(self, nc, stack):
        self.nc = nc
        self.stack = stack
        self.sems = {}
        self.engs = {}
        self.all_bufs = []
        for nm, h in (("pe", nc.tensor), ("act", nc.scalar), ("dve", nc.vector),
                      ("pool", nc.gpsimd), ("sp", nc.sync)):
            s = stack.enter_context(nc.semaphore("sem_" + nm))
            self.sems[id(s)] = s
            self.engs[nm] = Eng(nm, h, s)
        self.dsem_pool = []
        self.ninst = 0
        self.outst = {"sp": [], "act": [], "pool": []}
        self.hold = 0
        self.il = None
        self.maxq = {"sp": 12, "act": 12, "pool": 8}

    def sbuf(self, name, shape, dt):
        t = self.stack.enter_context(self.nc.sbuf_tensor(name, list(shape), dt))
        return Buf(self, name, t)

    def psum(self, name, shape, dt=F32):
        t = self.stack.enter_context(self.nc.psum_tensor(name, list(shape), dt))
        return Buf(self, name, t)

    def dram(self, name, shape, dt, kind="Internal"):
        t = self.nc.dram_tensor(name, list(shape), dt, kind=kind)
        return Buf(self, name, t, dram=True)

    def view(self, name, ap):
        return Buf(self, name, ap)

    def _dsem(self, b):
        if b.dsem is None:
            s = self.stack.enter_context(self.nc.semaphore("ds_" + b.name))
            self.sems[id(s)] = s
            b.dsem = s
        return b.dsem

    def _deps(self, reads, writes, group=None):
        deps = {}
        for b in reads:
            for k, v in b.w.items():
                if deps.get(k, 0) < v:
                    deps[k] = v
        for b in writes:
            same = (group is not None and b.wgroup == group)
            for d in ((b.r,) if same else (b.w, b.r)):
                for k, v in d.items():
                    if deps.get(k, 0) < v:
                        deps[k] = v
        return deps

    def _wait(self, e, deps, skip_self=False):
        for k, v in deps.items():
            if skip_self and k == id(e.sem):
                continue
            if e.waited.get(k, 0) < v:
                e.h.wait_ge(self.sems[k], v)
                e.waited[k] = v
                self.ninst += 1

    def _commit(self, reads, writes, k, v, group=None):
        for b in reads:
            if b.r.get(k, 0) < v:
                b.r[k] = v
        for b in writes:
            if group is not None and b.wgroup == group:
                if b.w.get(k, 0) < v:
                    b.w[k] = v
            else:
                b.w = {k: v}
            b.wgroup = group
            b.r = {}

    def interleave(self, fns, credit=1):
        if len(fns) == 1:
            fns[0]()
            return
        il = {"turn": 0, "alive": [True] * len(fns), "ids": {}, "err": None, "credit": credit, "left": credit,
              "cv": threading.Condition(), "sig": set()}
        self.il = il

        def nxt(i):
            n = len(fns)
            for d in range(1, n):
                j = (i + d) % n
                if il["alive"][j]:
                    il["turn"] = j
                    il["left"] = il["credit"]
                    return
            il["left"] = il["credit"]

        il["nxt"] = nxt

        def worker(i, fn):
            il["ids"][threading.get_ident()] = i
            with il["cv"]:
                while il["turn"] != i:
                    il["cv"].wait()
            try:
                fn()
            except BaseException as ex:
                il["err"] = ex
            finally:
                with il["cv"]:
                    il["alive"][i] = False
                    nxt(i)
                    il["cv"].notify_all()

        ths = [threading.Thread(target=worker, args=(i, f)) for i, f in enumerate(fns)]
        for th in ths:
            th.start()
        for th in ths:
            th.join()
        self.il = None
        if il["err"] is not None:
            raise il["err"]

    def signal(self, key):
        il = self.il
        if il is None:
            return
        with il["cv"]:
            il["sig"].add(key)

    def wait_for(self, key):
        il = self.il
        if il is None:
            return
        i = il["ids"].get(threading.get_ident())
        assert self.hold == 0
        with il["cv"]:
            while key not in il["sig"]:
                assert any(a for j, a in enumerate(il["alive"]) if j != i), ("deadlock waiting for", key)
                il["nxt"](i)
                il["cv"].notify_all()
                while il["turn"] != i:
                    il["cv"].wait()

    def point(self):
        il = self.il
        if il is None or self.hold > 0:
            return
        i = il["ids"].get(threading.get_ident())
        if i is None:
            return
        with il["cv"]:
            il["left"] -= 1
            if il["left"] <= 0:
                il["nxt"](i)
                il["cv"].notify_all()
                while il["turn"] != i:
                    il["cv"].wait()

    def op(self, eng, fn, reads=(), writes=(), inc=True):
        self.point()
        e = self.engs[eng]
        self._wait(e, self._deps(reads, writes), skip_self=(eng == "pe"))
        ins = fn(e.h)
        self.ninst += 1
        if inc:
            e.cnt += 1
            ins.then_inc(e.sem, 1)
            v = e.cnt
        else:
            v = e.cnt + 1
        self._commit(reads, writes, id(e.sem), v)
        return ins

    def dma(self, q, fn, reads=(), writes=(), sem_buf=None, group=None):
        self.point()
        e = self.engs[q]
        self._wait(e, self._deps(reads, writes, group))
        if sem_buf is None:
            cand = [b for b in list(writes) + list(reads) if not b.dram]
            sem_buf = cand[0] if cand else (list(writes) + list(reads))[0]
        s = self._dsem(sem_buf)
        ins = fn(e.h)
        self.ninst += 1
        sem_buf.dcnt += 16
        ins.then_inc(s, 16)
        self._commit(reads, writes, id(s), sem_buf.dcnt, group)
        q_ = self.outst[q]
        q_.append((id(s), sem_buf.dcnt))
        if len(q_) > self.maxq[q]:
            k, v = q_.pop(0)
            self._wait(e, {k: v})
        return ins

    def barrier(self):
        deps = {}
        for e in self.engs.values():
            if e.cnt:
                deps[id(e.sem)] = e.cnt
        for b in self.all_bufs:
            for d in (b.w, b.r):
                for k, v in d.items():
                    if deps.get(k, 0) < v:
                        deps[k] = v
        for e in self.engs.values():
            self._wait(e, deps)

    def finish(self, bufs):
        e = self.engs["sp"]
        deps = {}
        for b in bufs:
            for d in (b.w, b.r):
                for k, v in d.items():
                    if deps.get(k, 0) < v:
                        deps[k] = v
        self._wait(e, deps)


def build_program(S, depth=DEPTH, debug=False):
    NT = S // 128
    NBLK = (2 * S) // BS + NE
    NSLOT = NBLK * BS
    assert NSLOT % 128 == 0
    nc = bass.Bass("TRN2", target_bir_lowering=False)

    def din(name, shape, dt=F32):
        return nc.dram_tensor(name, list(shape), dt, kind="ExternalInput")

    x_in = din("x", [S, D])
    c_in = din("c", [128, KC])
    coff_in = din("coff", [128, 1])
    w_ada = din("w_ada", [DEPTH, D, 6 * D])
    b_ada = din("b_ada", [DEPTH, 6 * D])
    n1g = din("norm1_g", [DEPTH, D])
    n2g = din("norm2_g", [DEPTH, D])
    ml_w_in = din("ml_w_in", [2, D, ML_IN])
    ml_b_gate = din("ml_b_gate", [2, 8])
    ml_g_out = din("ml_g_out", [2, D])
    ml_w_out = din("ml_w_out", [2, D, D])
    sw_w_in = din("sw_w_in", [2, D, SW_IN])
    sw_g_q = din("sw_g_q", [2, 64])
    sw_g_k = din("sw_g_k", [2, 64])
    sw_sinks = din("sw_sinks", [2, 16])
    sw_w_out = din("sw_w_out", [2, D, D])
    w_rt = din("w_rt", [DEPTH, D, 72])
    b_rt = din("b_rt", [DEPTH, 72])
    w1r = din("w1r", [DEPTH * NE * 128, KC * DE])
    w3r = din("w3r", [DEPTH * NE * 128, KC * DE])
    w2r = din("w2r", [DEPTH * NE * 128, 3 * D])
    out = nc.dram_tensor("out", [S, D], F32, kind="ExternalOutput")
    dbg = nc.dram_tensor("dbg", [depth * S, D], F32, kind="ExternalOutput") if debug else None

    with ExitStack() as st:
        K = Kern(nc, st)
        op, dma = K.op, K.dma
        x_in_b = Buf(K, "x_in", x_in, dram=True)
        out_b = Buf(K, "out", out, dram=True)
        dbg_b = Buf(K, "dbg", dbg, dram=True) if debug else None
        wsrc = Buf(K, "wsrc", None, dram=True)
        xs = K.dram("xs", [S, D], F32)
        hn2d = K.dram("hn2d", [S + 128, D], BF16)
        slot_tw = K.dram("slot_tw", [NSLOT, 2], F32)
        y_slots = K.dram("y_slots", [NSLOT, D], F32)

        identf = K.sbuf("identf", [128, 128], F32)
        identb = K.sbuf("identb", [128, 128], BF16)
        onesf = K.sbuf("onesf", [128, 128], F32)
        onesb = K.sbuf("onesb", [128, 128], BF16)
        triinc = K.sbuf("triinc", [128, 128], F32)
        tristrb = K.sbuf("tristrb", [128, 128], BF16)
        negm = K.sbuf("negm", [128, 128], F32)
        negcur = K.sbuf("negcur", [128, 4, 128], BF16)
        negprev = K.sbuf("negprev", [128, 4, 128], BF16)
        tmpc = K.sbuf("tmpc", [128, 512], F32)
        sel4 = [K.sbuf("sel4_%d" % h, [4, 128], F32) for h in range(4)]
        iota_p = K.sbuf("iota_p", [128, 1], F32)
        tokid = K.sbuf("tokid", [128, NT], F32)
        jb = K.sbuf("jb", [128, 16], F32)
        padinit = K.sbuf("padinit", [128, 16, 2], F32)
        zrow = K.sbuf("zrow", [128, 256], BF16)

        op("pool", lambda e: e.memset(onesf[:], 1.0), writes=[onesf])
        op("pool", lambda e: e.memset(onesb[:], 1.0), writes=[onesb])
        op("pool", lambda e: e.affine_select(out=identf[:], in_=onesf[:], pattern=[[-1, 128]], compare_op=ALU.is_equal,
                                             fill=0.0, base=0, channel_multiplier=1), reads=[onesf], writes=[identf])
        op("pool", lambda e: e.tensor_copy(out=identb[:], in_=identf[:]), reads=[identf], writes=[identb])
        op("pool", lambda e: e.affine_select(out=triinc[:], in_=onesf[:], pattern=[[1, 128]], compare_op=ALU.is_ge,
                                             fill=0.0, base=0, channel_multiplier=-1), reads=[onesf], writes=[triinc])
        op("pool", lambda e: e.affine_select(out=tmpc[:, 0:128], in_=onesf[:], pattern=[[1, 128]], compare_op=ALU.is_gt,
                                             fill=0.0, base=0, channel_multiplier=-1), reads=[onesf], writes=[tmpc])
        op("pool", lambda e: e.tensor_copy(out=tristrb[:], in_=tmpc[:, 0:128]), reads=[tmpc], writes=[tristrb])
        op("pool", lambda e: e.memset(tmpc[:], 0.0), reads=[tmpc], writes=[tmpc])
        op("pool", lambda e: e.affine_select(out=negm[:], in_=tmpc[:, 0:128], pattern=[[1, 128]], compare_op=ALU.is_ge,
                                             fill=NEG, base=0, channel_multiplier=-1), reads=[tmpc], writes=[negm])
        op("pool", lambda e: e.affine_select(out=negcur[:].rearrange("p h q -> p (h q)"), in_=tmpc[:], pattern=[[0, 4], [1, 128]],
                                             compare_op=ALU.is_ge, fill=NEG, base=0, channel_multiplier=-1),
           reads=[tmpc], writes=[negcur])
        op("pool", lambda e: e.affine_select(out=negprev[:].rearrange("p h q -> p (h q)"), in_=tmpc[:], pattern=[[0, 4], [-1, 128]],
                                             compare_op=ALU.is_gt, fill=NEG, base=0, channel_multiplier=1),
           reads=[tmpc], writes=[negprev])
        for h in range(4):
            op("pool", lambda e, h=h: e.affine_select(out=sel4[h][:], in_=onesf[0:4, :], pattern=[[0, 128]], compare_op=ALU.is_equal,
                                                      fill=0.0, base=-h, channel_multiplier=1), reads=[onesf], writes=[sel4[h]])
        op("pool", lambda e: e.iota(iota_p[:], pattern=[[0, 1]], base=0, channel_multiplier=1, allow_small_or_imprecise_dtypes=True),
           writes=[iota_p])
        op("pool", lambda e: e.iota(tokid[:], pattern=[[128, NT]], base=0, channel_multiplier=1, allow_small_or_imprecise_dtypes=True),
           writes=[tokid])
        op("pool", lambda e: e.iota(jb[:], pattern=[[BS, 16]], base=0, channel_multiplier=0, allow_small_or_imprecise_dtypes=True),
           writes=[jb])
        op("pool", lambda e: e.memset(padinit[:, :, 0:1], float(S)), writes=[padinit])
        op("pool", lambda e: e.memset(padinit[:, :, 1:2], 0.0), reads=[padinit], writes=[padinit])
        op("pool", lambda e: e.memset(zrow[:], 0.0), writes=[zrow])
        for q4 in range(4):
            dma("sp", lambda e, q4=q4: e.dma_start(out=hn2d[S:S + 128, q4 * 256:(q4 + 1) * 256], in_=zrow[:]), reads=[zrow], writes=[hn2d])

        mod6 = K.sbuf("mod6", [128, 6, D], F32)
        cond = K.sbuf("cond", [128, KC], F32)
        rowv = tmpc
        wrt = K.sbuf("wrt", [128, KC, 72], BF16)
        brb = K.sbuf("brb", [128, 72], F32)
        bgb = K.sbuf("bgb", [128, 8], F32)
        goutb = K.sbuf("goutb", [128, D], F32)
        gqb = K.sbuf("gqb", [128, 64], F32)
        gkb = K.sbuf("gkb", [128, 64], F32)
        esink = K.sbuf("esink", [128, 16], F32)
        ARENA = KC * (ML_IN + D)
        arena = st.enter_context(nc.sbuf_tensor("arena", [128, ARENA], BF16))
        wmix_in_ml = K.view("wmix_in_ml", arena[:, 0:KC * ML_IN].rearrange("p (k n) -> p k n", k=KC))
        wmix_out = K.view("wmix_out", arena[:, KC * ML_IN:ARENA].rearrange("p (k n) -> p k n", k=KC))
        wmix_in_sw = K.view("wmix_in_sw", arena[:, 0:KC * SW_IN].rearrange("p (k n) -> p k n", k=KC))
        stages = [K.view("stage0", arena[:, 0:8192].bitcast(F32).rearrange("p (k n) -> p k n", k=KC)),
                  K.view("stage1", arena[:, 10240:18432].bitcast(F32).rearrange("p (k n) -> p k n", k=KC))]
        rowall = K.view("rowall", arena[0:1, 18432:18432 + 12288].bitcast(F32))
        condb = K.view("condb", arena[:, 2 * KC * 512:2 * KC * 512 + 2 * KC * 128].bitcast(F32).rearrange("p (k n) -> p k n", k=KC))
        o = 0
        def carve(name, n, shape_str=None, **kw):
            nonlocal o
            ap = arena[:, o:o + n]
            o += n
            if shape_str:
                ap = ap.rearrange(shape_str, **kw)
            return K.view(name, ap)
        w1b = [carve("w1b%d" % i, KC * DE, "p (k n) -> p k n", k=KC) for i in range(2)]
        w3b = [carve("w3b%d" % i, KC * DE, "p (k n) -> p k n", k=KC) for i in range(2)]
        w2b = [carve("w2b%d" % i, 3 * D, "p (k n) -> p k n", k=3) for i in range(3)]
        xgc = [[carve("xg%d_%d" % (c_, i), D) for i in range(2)] for c_ in range(2)]
        xTmc = [carve("xTm%d" % c_, KC * BS, "p (k n) -> p k n", k=KC) for c_ in range(2)]
        hTmc = [carve("hTm%d" % c_, 3 * BS, "p (k n) -> p k n", k=3) for c_ in range(2)]
        ybc = [K.view("ybc%d" % i, arena[:, i * 2 * D:(i + 1) * 2 * D].bitcast(F32)) for i in range(2)]
        assert o <= ARENA

        ps = [K.psum("ps%d" % i, [128, 512], F32) for i in range(6)]
        pt = [K.psum("pt%d" % i, [128, 1024], BF16) for i in range(2)]

        class WSet:
            pass
        WS = []
        for p_ in range(2):
            W = WSet()
            sfx = "_%d" % p_
            W.p = p_
            W.xt = K.sbuf("xt" + sfx, [128, D], F32)
            W.junk = K.sbuf("junk" + sfx, [128, D], BF16)
            W.tmpf = K.sbuf("tmpf" + sfx, [128, D], F32)
            W.hnb = K.sbuf("hnb" + sfx, [128, D], BF16)
            W.hnT = K.sbuf("hnT" + sfx, [128, KC, 128], BF16)
            W.sm = K.sbuf("sm" + sfx, [128, 64], F32)
            W.hg = K.sbuf("hg" + sfx, [128, D], BF16)
            W.hgT = K.sbuf("hgT" + sfx, [128, KC, 128], BF16)
            W.xt2 = K.sbuf("xt2" + sfx, [128, D], F32)
            W.gates = K.sbuf("gates" + sfx, [128, 40], F32)
            W.bT = K.sbuf("bT" + sfx, [4, 128], F32)
            W.ssq = K.sbuf("ssq" + sfx, [128, 32], F32)
            W.lg = K.sbuf("lg" + sfx, [128, 72], F32)
            W.rsm = K.sbuf("rsm" + sfx, [128, 64], F32)
            W.t64 = K.sbuf("t64" + sfx, [128, 64], F32)
            W.ohs = K.sbuf("ohs" + sfx, [128, 64], BF16)
            W.rtot = K.sbuf("rtot" + sfx, [128, 64], F32)
            W.go = K.sbuf("go" + sfx, [128, D], BF16)
            W.res = K.sbuf("res" + sfx, [128, 257], F32)
            W.tmpA = K.sbuf("tmpA" + sfx, [128, 257], F32)
            W.dT = K.sbuf("dT" + sfx, [128, 128], F32)
            MXN = 4 * 128 * 3 + 4 * 257 + 2 * 128
            mx = st.enter_context(nc.sbuf_tensor("mx" + sfx, [128, max(MXN, 16 * 128 + 256 + 1024)], BF16))
            o_ = 0
            def cv(name, n, rs=None, **kw):
                nonlocal o_
                ap = mx[:, o_:o_ + n]
                o_ += n
                if rs:
                    ap = ap.rearrange(rs, **kw)
                return K.view(name + sfx, ap)
            W.qT = cv("qT", 512, "p (h n) -> p h n", h=4)
            W.kT = cv("kT", 512, "p (h n) -> p h n", h=4)
            W.ktok = cv("ktok", 512)
            W.vp = cv("vp", 4 * 257, "p (h n) -> p h n", h=4)
            W.pT_ = cv("pT_", 128)
            W.kw_ = cv("kw_", 128)
            o_ = 0
            W.qTs = K.view("qTs" + sfx, mx[0:64, 0:2048].rearrange("p (h n) -> p h n", h=16))
            o_ = 2048
            W.kn = cv("kn", 256)
            W.pprev = cv("pprev", 512)
            W.pcur = cv("pcur", 512)
            W.qn = W.junk
            WS.append(W)
        c32 = K.sbuf("c32", [128, 4, 257], F32)
        cb = K.sbuf("cb", [128, 4, 257], BF16)
        kTs = [K.sbuf("kTs%d" % i, [64, 4, 128], BF16) for i in range(3)]
        vps = [K.sbuf("vps%d" % i, [128, 4, 65], BF16) for i in range(3)]
        OH1 = K.sbuf("OH1", [128, NT, 64], BF16)
        OH2 = K.sbuf("OH2", [128, NT, 64], BF16)
        if NT * 64 >= KC * DE:
            w1b.append(K.view("w1b2", OH1.t[:].rearrange("p a b -> p (a b)")[:, 0:KC * DE].rearrange("p (k n) -> p k n", k=KC)))
            w3b.append(K.view("w3b2", OH2.t[:].rearrange("p a b -> p (a b)")[:, 0:KC * DE].rearrange("p (k n) -> p k n", k=KC)))
        else:
            w1b.append(K.sbuf("w1b2", [128, KC, DE], BF16))
            w3b.append(K.sbuf("w3b2", [128, KC, DE], BF16))
        R12 = K.sbuf("R12", [128, 2, NT], F32)
        GT = K.sbuf("GT", [128, 2, NT], F32)
        base = K.sbuf("base", [128, 64], F32)
        pp = K.sbuf("pp", [128, 4, 64], F32)
        ppi = K.sbuf("ppi", [128, 64], I32)
        DEST = K.sbuf("DEST", [128, 2, NT], F32)
        DESTi = K.sbuf("DESTi", [128, 2, NT], I32)
        SRC = K.sbuf("SRC", [128, 2, NT, 2], F32)
        BE = K.sbuf("BE", [128, NBLK], F32)
        BEi = K.sbuf("BEi", [128, NBLK], I32)
        stwc = [[K.sbuf("stw%d_%d" % (c_, i), [128, 2], F32) for i in range(2)] for c_ in range(2)]
        stokc = [[K.sbuf("stok%d_%d" % (c_, i), [128, 1], I32) for i in range(2)] for c_ in range(2)]
        sil = tmpc

        ya = WS[1].tmpf
        yb = WS[0].tmpf
        sm = WS[0].sm
        bigb = WS[1].xt
        big = bigb[:, 0:1024].rearrange("p (a b) -> p a b", b=64)
        xt = WS[0].xt
        xt2 = WS[0].xt2

        def rmsnorm_mod(W, src, a_idx, b_idx, dst_bf):
            op("act", lambda e: e.activation(out=W.junk[:], in_=src[:], func=AF.Square, accum_out=W.sm[:, 0:1]),
               reads=[src], writes=[W.junk, W.sm])
            op("dve", lambda e: e.tensor_scalar(out=W.sm[:, 1:2], in0=W.sm[:, 0:1], scalar1=1.0 / D, scalar2=EPS, op0=ALU.mult, op1=ALU.add),
               reads=[W.sm], writes=[W.sm])
            op("act", lambda e: e.activation(out=W.sm[:, 2:3], in_=W.sm[:, 1:2], func=AF.Sqrt), reads=[W.sm], writes=[W.sm])
            op("dve", lambda e: e.reciprocal(out=W.sm[:, 3:4], in_=W.sm[:, 2:3]), reads=[W.sm], writes=[W.sm])
            op("dve", lambda e: e.scalar_tensor_tensor(out=W.tmpf[:], in0=src[:], scalar=W.sm[:, 3:4], in1=mod6[:, a_idx, :],
                                                       op0=ALU.mult, op1=ALU.mult), reads=[src, W.sm, mod6], writes=[W.tmpf])
            op("dve", lambda e: e.tensor_tensor(out=dst_bf[:], in0=W.tmpf[:], in1=mod6[:, b_idx, :], op=ALU.add),
               reads=[W.tmpf, mod6], writes=[dst_bf])

        def transpose8(src_bf, dstT, pbank):
            K.hold += 1
            for kc in range(KC):
                op("pe", lambda e, kc=kc: e.transpose(out=pbank[:, kc * 128:(kc + 1) * 128], in_=src_bf[:, kc * 128:(kc + 1) * 128],
                                                      identity=identb[:]), reads=[src_bf, identb], writes=[pbank], inc=(kc == KC - 1))
            K.hold -= 1
            op("act", lambda e: e.copy(out=dstT[:].rearrange("p k n -> p (k n)"), in_=pbank[:]), reads=[pbank], writes=[dstT])

        def mm_group(pbuf, out_ap, pairs, reads):
            n = len(pairs)
            K.hold += 1
            for i, (l, r) in enumerate(pairs):
                op("pe", lambda e, l=l, r=r, i=i: e.matmul(out_ap, lhsT=l, rhs=r, start=(i == 0), stop=(i == n - 1)),
                   reads=reads, writes=[pbuf], inc=(i == n - 1))
            K.hold -= 1

        def bcast_row(dst_ap, dst_buf, src_dram_ap, n):
            dma("sp", lambda e: e.dma_start(out=dst_ap, in_=src_dram_ap.partition_broadcast(128)), reads=[wsrc], writes=[dst_buf])

        dma("sp", lambda e: e.dma_start(out=cond[:], in_=c_in.ap()), reads=[wsrc], writes=[cond])
        op("act", lambda e: e.activation(out=cond[:], in_=cond[:], func=AF.Silu), reads=[cond], writes=[cond])

        x_src = x_in_b
        x_src_t = x_in
        for layer in range(depth):
            j = layer // 2
            is_ml = (layer % 2 == 0)
            last = (layer == depth - 1)
            for kc in range(KC):
                op("dve", lambda e, kc=kc: e.tensor_scalar(out=condb[:, kc, :], in0=onesf[:], scalar1=cond[:, kc:kc + 1], scalar2=None,
                                                           op0=ALU.mult), reads=[onesf, cond], writes=[condb])
            for ncol in range(12):
                stage = stages[ncol % 2]
                if ncol == 0:
                    dma("act", lambda e: e.dma_start(out=rowall[:], in_=b_ada[layer:layer + 1, :]), reads=[wsrc], writes=[rowall])
                dma("sp", lambda e, ncol=ncol: e.dma_start(
                    out=stage[:], in_=w_ada[layer].rearrange("(kc p) n -> p kc n", p=128)[:, :, ncol * 512:(ncol + 1) * 512]),
                    reads=[wsrc], writes=[stage])
                pb = ps[ncol % 2]
                pairs = [(condb[:, kc, :], stage[:, kc, :]) for kc in range(KC)]
                pairs.append((onesf[0:1, :], rowall[0:1, ncol * 512:(ncol + 1) * 512]))
                mm_group(pb, pb[:], pairs, [condb, stage, onesf, rowall])
                op("dve", lambda e, ncol=ncol, pb=pb: e.tensor_copy(out=mod6[:, ncol // 2, (ncol % 2) * 512:(ncol % 2 + 1) * 512], in_=pb[:]),
                   reads=[pb], writes=[mod6])
            for (gsrc, idx) in ((n1g, 1), (n2g, 4)):
                bcast_row(WS[0].tmpf[:], WS[0].tmpf, gsrc[layer, :], D)
                op("dve", lambda e, idx=idx: e.scalar_tensor_tensor(out=mod6[:, idx, :], in0=mod6[:, idx, :], scalar=1.0, in1=WS[0].tmpf[:],
                                                                    op0=ALU.add, op1=ALU.mult), reads=[mod6, WS[0].tmpf], writes=[mod6])
            dma("pool", lambda e: e.dma_start(out=wrt[:], in_=w_rt[layer].rearrange("(kc p) n -> p kc n", p=128)), reads=[wsrc], writes=[wrt])
            bcast_row(brb[:], brb, b_rt[layer, :], 72)
            K.barrier()
            if is_ml:
                for kc in range(KC):
                    dma("pool", lambda e, kc=kc: e.dma_start(out=wmix_in_ml[:, kc, :], in_=ml_w_in[j, kc * 128:(kc + 1) * 128, :]),
                        reads=[wsrc], writes=[wmix_in_ml])
                    dma("pool", lambda e, kc=kc: e.dma_start(out=wmix_out[:, kc, :], in_=ml_w_out[j, kc * 128:(kc + 1) * 128, :]),
                        reads=[wsrc], writes=[wmix_out])
                bcast_row(bgb[:], bgb, ml_b_gate[j, :], 8)
                bcast_row(goutb[:], goutb, ml_g_out[j, :], D)
                op("dve", lambda e: e.memset(c32[:], 0.0), writes=[c32])
                op("dve", lambda e: e.memset(cb[:], 0.0), writes=[cb])
                for W_ in WS:
                    op("dve", lambda e, W_=W_: e.memset(W_.vp[:], 1.0), writes=[W_.vp])
                win = wmix_in_ml
            else:
                for kc in range(KC):
                    dma("pool", lambda e, kc=kc: e.dma_start(out=wmix_in_sw[:, kc, :], in_=sw_w_in[j, kc * 128:(kc + 1) * 128, :]),
                        reads=[wsrc], writes=[wmix_in_sw])
                    dma("pool", lambda e, kc=kc: e.dma_start(out=wmix_out[:, kc, :], in_=sw_w_out[j, kc * 128:(kc + 1) * 128, :]),
                        reads=[wsrc], writes=[wmix_out])
                bcast_row(gqb[:], gqb, sw_g_q[j, :], 64)
                bcast_row(gkb[:], gkb, sw_g_k[j, :], 64)
                bcast_row(esink[:], esink, sw_sinks[j, :], 16)
                op("act", lambda e: e.activation(out=esink[:], in_=esink[:], func=AF.Exp), reads=[esink], writes=[esink])
                for i in range(3):
                    op("dve", lambda e, i=i: e.memset(kTs[i][:], 0.0), writes=[kTs[i]])
                    op("dve", lambda e, i=i: e.memset(vps[i][:], 0.0), writes=[vps[i]])
                win = wmix_in_sw
            op("dve", lambda e: e.memset(base[:], 0.0), writes=[base])

            real_ps, real_pt = ps, pt

            class _PS:
                def __init__(self, p_):
                    self.p_ = p_
                def __getitem__(self, i):
                    return real_ps[3 * self.p_ + (i % 3)]

            class _PT:
                def __init__(self, p_):
                    self.p_ = p_
                def __getitem__(self, i):
                    return real_pt[self.p_]

            def tile_body(t, ps, pt, W):
                rows = slice(t * 128, (t + 1) * 128)
                dma("sp", lambda e: e.dma_start(out=W.xt[:], in_=x_src_t[rows, :]), reads=[x_src], writes=[W.xt])
                if layer > 0:
                    dma("pool", lambda e: e.indirect_dma_start(out=W.tmpf[:], out_offset=None, in_=y_slots[:, :],
                                                               in_offset=bass.IndirectOffsetOnAxis(ap=DESTi[:, 0, t:t + 1], axis=0)),
                        reads=[y_slots, DESTi], writes=[W.tmpf])
                    dma("pool", lambda e: e.indirect_dma_start(out=W.xt2[:], out_offset=None, in_=y_slots[:, :],
                                                               in_offset=bass.IndirectOffsetOnAxis(ap=DESTi[:, 1, t:t + 1], axis=0)),
                        reads=[y_slots, DESTi], writes=[W.xt2])
                    op("dve", lambda e: e.tensor_tensor(out=W.tmpf[:], in0=W.tmpf[:], in1=W.xt2[:], op=ALU.add), reads=[W.tmpf, W.xt2], writes=[W.tmpf])
                    op("dve", lambda e: e.tensor_tensor(out=W.xt[:], in0=W.xt[:], in1=W.tmpf[:], op=ALU.add), reads=[W.xt, W.tmpf], writes=[W.xt])
                rmsnorm_mod(W, W.xt, 1, 0, W.hnb)
                transpose8(W.hnb, W.hnT, pt[0])
                if is_ml:
                    for (dst, coff, scl) in ((W.qT, 0, 1.0), (W.kT, 512, 128 ** -0.5)):
                        pb = ps[0] if coff == 0 else ps[1]
                        for h in range(4):
                            mm_group(pb, pb[:, h * 128:(h + 1) * 128],
                                     [(win[:, kc, coff + h * 128:coff + (h + 1) * 128], W.hnT[:, kc, :]) for kc in range(KC)], [win, W.hnT])
                        op("act", lambda e, dst=dst, pb=pb, scl=scl: e.mul(out=dst[:].rearrange("p h n -> p (h n)"), in_=pb[:], mul=scl),
                           reads=[pb], writes=[dst])
                    mm_group(ps[2], ps[2][:], [(W.hnT[:, kc, :], win[:, kc, 512:1024]) for kc in range(KC)], [win, W.hnT])
                    op("act", lambda e: e.mul(out=W.ktok[:], in_=ps[2][:], mul=128 ** -0.5), reads=[ps[2]], writes=[W.ktok])
                    for half in range(2):
                        pb = ps[3 + half]
                        mm_group(pb, pb[:], [(W.hnT[:, kc, :], win[:, kc, 1024 + half * 512:1024 + (half + 1) * 512]) for kc in range(KC)], [win, W.hnT])
                        op("dve", lambda e, pb=pb, half=half: e.tensor_copy(out=W.vp[:, 2 * half:2 * half + 2, 0:256],
                                                                            in_=pb[:].rearrange("p (h n) -> p h n", h=2)),
                           reads=[pb], writes=[W.vp])
                    for half in range(2):
                        pb = ps[(5 + half) % 6]
                        mm_group(pb, pb[:], [(W.hnT[:, kc, :], win[:, kc, 2048 + half * 512:2048 + (half + 1) * 512]) for kc in range(KC)], [win, W.hnT])
                        op("act", lambda e, pb=pb, half=half: e.activation(out=W.go[:, half * 512:(half + 1) * 512], in_=pb[:], func=AF.Sigmoid),
                           reads=[pb], writes=[W.go])
                    op("dve", lambda e: e.tensor_tensor(out=W.go[:], in0=W.go[:], in1=goutb[:], op=ALU.mult), reads=[W.go, goutb], writes=[W.go])
                    mm_group(ps[1], ps[1][:, 0:8], [(W.hnT[:, kc, :], win[:, kc, 3072:3080]) for kc in range(KC)], [win, W.hnT])
                    G = W.gates
                    op("dve", lambda e: e.tensor_tensor(out=G[:, 0:8], in0=ps[1][:, 0:8], in1=bgb[:], op=ALU.add), reads=[ps[1], bgb], writes=[G])
                    op("act", lambda e: e.activation(out=G[:, 0:8], in_=G[:, 0:8], func=AF.Tanh, scale=1.0 / 15.0), reads=[G], writes=[G])
                    op("dve", lambda e: e.tensor_scalar(out=G[:, 0:8], in0=G[:, 0:8], scalar1=15.0, scalar2=None, op0=ALU.mult), reads=[G], writes=[G])
                    op("act", lambda e: e.activation(out=G[:, 8:12], in_=G[:, 4:8], func=AF.Exp, scale=-1.0), reads=[G], writes=[G])
                    op("act", lambda e: e.activation(out=G[:, 8:12], in_=G[:, 8:12], func=AF.Ln, bias=1.0), reads=[G], writes=[G])
                    op("dve", lambda e: e.tensor_scalar(out=G[:, 8:12], in0=G[:, 8:12], scalar1=-1.0, scalar2=None, op0=ALU.mult), reads=[G], writes=[G])
                    mm_group(ps[0], ps[0][:, 0:4], [(triinc[:], G[:, 8:12])], [triinc, G])
                    mm_group(ps[0], ps[0][:, 4:8], [(onesf[:], G[:, 8:12])], [onesf, G])
                    op("dve", lambda e: e.tensor_copy(out=G[:, 12:16], in_=ps[0][:, 0:4]), reads=[ps[0]], writes=[G])
                    op("dve", lambda e: e.tensor_copy(out=G[:, 20:24], in_=ps[0][:, 4:8]), reads=[ps[0]], writes=[G])
                    op("dve", lambda e: e.tensor_tensor(out=G[:, 16:20], in0=G[:, 0:4], in1=G[:, 12:16], op=ALU.subtract), reads=[G], writes=[G])
                    op("dve", lambda e: e.tensor_tensor(out=G[:, 24:28], in0=G[:, 16:20], in1=G[:, 20:24], op=ALU.add), reads=[G], writes=[G])
                    op("act", lambda e: e.activation(out=G[:, 24:28], in_=G[:, 24:28], func=AF.Exp), reads=[G], writes=[G])
                    op("act", lambda e: e.activation(out=G[:, 28:32], in_=G[:, 20:24], func=AF.Exp), reads=[G], writes=[G])
                    op("act", lambda e: e.activation(out=G[:, 32:36], in_=G[:, 12:16], func=AF.Exp), reads=[G], writes=[G])
                    op("pe", lambda e: e.transpose(out=ps[0][0:4, 128:256], in_=G[:, 12:16], identity=identf[:]), reads=[G, identf], writes=[ps[0]])
                    op("dve", lambda e: e.tensor_copy(out=W.bT[:], in_=ps[0][0:4, 128:256]), reads=[ps[0]], writes=[W.bT])
                    for h in range(4):
                        mm_group(ps[1], ps[1][:, 0:128], [(sel4[h][:], W.bT[:]), (identf[:], negm[:])], [sel4[h], W.bT, identf, negm])
                        op("act", lambda e, h=h: e.activation(out=W.dT[:], in_=ps[1][:, 0:128], func=AF.Exp, bias=G[:, 16 + h:17 + h]),
                           reads=[ps[1], G], writes=[W.dT])
                        mm_group(ps[2], ps[2][:, 0:128], [(W.kT[:, h, :], W.qT[:, h, :])], [W.kT, W.qT])
                        op("dve", lambda e: e.tensor_tensor(out=W.pT_[:], in0=ps[2][:, 0:128], in1=W.dT[:], op=ALU.mult), reads=[ps[2], W.dT], writes=[W.pT_])
                        mm_group(ps[3], ps[3][:, 0:257], [(W.pT_[:], W.vp[:, h, :])], [W.pT_, W.vp])
                        if W.p == 1:
                            K.wait_for(("st", h))
                        mm_group(ps[4], ps[4][:, 0:257], [(W.qT[:, h, :], cb[:, h, :])], [W.qT, cb])
                        op("act", lambda e, h=h: e.activation(out=W.tmpA[:], in_=ps[4][:, 0:257], func=AF.Copy, scale=G[:, 32 + h:33 + h]),
                           reads=[ps[4], G], writes=[W.tmpA])
                        op("dve", lambda e: e.tensor_tensor(out=W.res[:], in0=W.tmpA[:], in1=ps[3][:, 0:257], op=ALU.add), reads=[W.tmpA, ps[3]], writes=[W.res])
                        op("pool", lambda e, h=h: e.tensor_scalar(out=W.kw_[:], in0=W.ktok[:, h * 128:(h + 1) * 128], scalar1=G[:, 24 + h:25 + h], scalar2=None,
                                                                  op0=ALU.mult), reads=[W.ktok, G], writes=[W.kw_])
                        mm_group(ps[5], ps[5][:, 0:257], [(W.kw_[:], W.vp[:, h, :])], [W.kw_, W.vp])
                        op("dve", lambda e, h=h: e.scalar_tensor_tensor(out=c32[:, h, :], in0=c32[:, h, :], scalar=G[:, 28 + h:29 + h], in1=ps[5][:, 0:257],
                                                                        op0=ALU.mult, op1=ALU.add), reads=[c32, G, ps[5]], writes=[c32])
                        op("pool", lambda e, h=h: e.tensor_copy(out=cb[:, h, :], in_=c32[:, h, :]), reads=[c32], writes=[cb])
                        if W.p == 0:
                            K.signal(("st", h))
                        S_ = W.sm
                        op("dve", lambda e: e.tensor_scalar(out=S_[:, 7:8], in0=W.res[:, 256:257], scalar1=-1.0, scalar2=None, op0=ALU.mult),
                           reads=[W.res], writes=[S_])
                        op("dve", lambda e: e.tensor_tensor(out=S_[:, 8:9], in0=S_[:, 7:8], in1=W.res[:, 256:257], op=ALU.max), reads=[W.res, S_], writes=[S_])
                        op("dve", lambda e: e.tensor_scalar(out=S_[:, 8:9], in0=S_[:, 8:9], scalar1=1.0, scalar2=None, op0=ALU.max), reads=[S_], writes=[S_])
                        op("dve", lambda e: e.reciprocal(out=S_[:, 9:10], in_=S_[:, 8:9]), reads=[S_], writes=[S_])
                        op("act", lambda e: e.activation(out=W.junk[:, 0:256], in_=W.res[:, 0:256], func=AF.Square, accum_out=S_[:, 10:11]),
                           reads=[W.res], writes=[W.junk, S_])
                        op("dve", lambda e: e.tensor_tensor(out=S_[:, 11:12], in0=S_[:, 9:10], in1=S_[:, 9:10], op=ALU.mult), reads=[S_], writes=[S_])
                        op("dve", lambda e: e.scalar_tensor_tensor(out=S_[:, 12:13], in0=S_[:, 10:11], scalar=1.0 / 256.0, in1=S_[:, 11:12],
                                                                   op0=ALU.mult, op1=ALU.mult), reads=[S_], writes=[S_])
                        op("dve", lambda e: e.tensor_scalar(out=S_[:, 12:13], in0=S_[:, 12:13], scalar1=EPS, scalar2=None, op0=ALU.add), reads=[S_], writes=[S_])
                        op("act", lambda e: e.activation(out=S_[:, 13:14], in_=S_[:, 12:13], func=AF.Sqrt), reads=[S_], writes=[S_])
                        op("dve", lambda e: e.reciprocal(out=S_[:, 14:15], in_=S_[:, 13:14]), reads=[S_], writes=[S_])
                        op("dve", lambda e: e.tensor_tensor(out=S_[:, 15:16], in0=S_[:, 14:15], in1=S_[:, 9:10], op=ALU.mult), reads=[S_], writes=[S_])
                        op("dve", lambda e, h=h: e.scalar_tensor_tensor(out=W.hg[:, h * 256:(h + 1) * 256], in0=W.res[:, 0:256], scalar=S_[:, 15:16],
                                                                        in1=W.go[:, h * 256:(h + 1) * 256], op0=ALU.mult, op1=ALU.mult),
                           reads=[W.res, S_, W.go], writes=[W.hg])
                else:
                    cur, prv = t % 3, (t + 2) % 3
                    for half in range(2):
                        mm_group(ps[half], ps[half][:], [(W.hnT[:, kc, :], win[:, kc, half * 512:(half + 1) * 512]) for kc in range(KC)], [win, W.hnT])
                    mm_group(ps[2], ps[2][:], [(W.hnT[:, kc, :], win[:, kc, 1024:1536]) for kc in range(KC)], [win, W.hnT])
                    for half in range(2):
                        op("act", lambda e, half=half: e.activation(out=W.tmpf[:, half * 512:(half + 1) * 512], in_=ps[half][:], func=AF.Square),
                           reads=[ps[half]], writes=[W.tmpf])
                    op("dve", lambda e: e.tensor_reduce(out=W.ssq[:, 0:16], in_=W.tmpf[:].rearrange("p (h d) -> p h d", d=64), axis=AX.X, op=ALU.add),
                       reads=[W.tmpf], writes=[W.ssq])
                    op("act", lambda e: e.activation(out=W.xt2[:, 0:256], in_=ps[2][:, 0:256], func=AF.Square), reads=[ps[2]], writes=[W.xt2])
                    op("dve", lambda e: e.tensor_reduce(out=W.ssq[:, 16:20], in_=W.xt2[:, 0:256].rearrange("p (h d) -> p h d", d=64), axis=AX.X, op=ALU.add),
                       reads=[W.xt2], writes=[W.ssq])
                    op("dve", lambda e: e.tensor_scalar(out=W.ssq[:, 0:20], in0=W.ssq[:, 0:20], scalar1=1.0 / 64.0, scalar2=EPS, op0=ALU.mult, op1=ALU.add),
                       reads=[W.ssq], writes=[W.ssq])
                    op("act", lambda e: e.activation(out=W.ssq[:, 0:20], in_=W.ssq[:, 0:20], func=AF.Sqrt), reads=[W.ssq], writes=[W.ssq])
                    op("dve", lambda e: e.reciprocal(out=W.ssq[:, 0:20], in_=W.ssq[:, 0:20]), reads=[W.ssq], writes=[W.ssq])
                    for half in range(2):
                        op("dve", lambda e, half=half: e.tensor_tensor(
                            out=W.tmpf[:, half * 512:(half + 1) * 512].rearrange("p (h d) -> p h d", d=64),
                            in0=ps[half][:].rearrange("p (h d) -> p h d", d=64),
                            in1=W.ssq[:, half * 8:(half + 1) * 8].unsqueeze(2).broadcast_to([128, 8, 64]), op=ALU.mult),
                            reads=[ps[half], W.ssq], writes=[W.tmpf])
                    op("dve", lambda e: e.tensor_tensor(out=W.qn[:].rearrange("p (h d) -> p h d", d=64), in0=W.tmpf[:].rearrange("p (h d) -> p h d", d=64),
                                                         in1=gqb[:].unsqueeze(1).broadcast_to([128, 16, 64]), op=ALU.mult),
                       reads=[W.tmpf, gqb], writes=[W.qn])
                    op("dve", lambda e: e.tensor_tensor(out=W.xt2[:, 0:256].rearrange("p (h d) -> p h d", d=64),
                                                        in0=ps[2][:, 0:256].rearrange("p (h d) -> p h d", d=64),
                                                        in1=W.ssq[:, 16:20].unsqueeze(2).broadcast_to([128, 4, 64]), op=ALU.mult),
                       reads=[ps[2], W.ssq], writes=[W.xt2])
                    op("dve", lambda e: e.tensor_tensor(out=W.kn[:].rearrange("p (h d) -> p h d", d=64), in0=W.xt2[:, 0:256].rearrange("p (h d) -> p h d", d=64),
                                                         in1=gkb[:].unsqueeze(1).broadcast_to([128, 4, 64]), op=ALU.mult),
                       reads=[W.xt2, gkb], writes=[W.kn])
                    op("act", lambda e: e.copy(out=vps[cur][:, :, 0:64], in_=ps[2][:, 256:512].rearrange("p (h d) -> p h d", d=64)),
                       reads=[ps[2]], writes=[vps[cur]])
                    op("pool", lambda e: e.memset(vps[cur][:, :, 64:65], 1.0), reads=[vps[cur]], writes=[vps[cur]])
                    for half in range(2):
                        pb = pt[0]
                        K.hold += 1
                        for h8 in range(8):
                            hh = half * 8 + h8
                            op("pe", lambda e, hh=hh, h8=h8, pb=pb: e.transpose(out=pb[0:64, h8 * 128:(h8 + 1) * 128], in_=W.qn[:, hh * 64:(hh + 1) * 64],
                                                                                identity=identb[:]), reads=[W.qn, identb], writes=[pb], inc=(h8 == 7))
                        K.hold -= 1
                        op("act", lambda e, half=half, pb=pb: e.copy(out=W.qTs[:, half * 8:(half + 1) * 8, :].rearrange("p h n -> p (h n)"), in_=pb[0:64, :]),
                           reads=[pb], writes=[W.qTs])
                    K.hold += 1
                    for g in range(4):
                        op("pe", lambda e, g=g: e.transpose(out=pt[0][0:64, g * 128:(g + 1) * 128], in_=W.kn[:, g * 64:(g + 1) * 64], identity=identb[:]),
                           reads=[W.kn, identb], writes=[pt[0]], inc=(g == 3))
                    K.hold -= 1
                    op("act", lambda e: e.copy(out=kTs[cur][:].rearrange("p h n -> p (h n)"), in_=pt[0][0:64, 0:512]), reads=[pt[0]], writes=[kTs[cur]])
                    if W.p == 0:
                        K.signal("kv")
                    else:
                        K.wait_for("kv")
                    for g in range(4):
                        qv = W.qTs[:, 4 * g:4 * g + 4, :].rearrange("p h n -> p (h n)")
                        mm_group(ps[3], ps[3][:], [(kTs[prv][:, g, :], qv), (identb[:], negprev[:].rearrange("p h q -> p (h q)"))],
                                 [kTs[prv], W.qTs, identb, negprev])
                        mm_group(ps[4], ps[4][:], [(kTs[cur][:, g, :], qv), (identb[:], negcur[:].rearrange("p h q -> p (h q)"))],
                                 [kTs[cur], W.qTs, identb, negcur])
                        op("act", lambda e: e.activation(out=W.pprev[:], in_=ps[3][:], func=AF.Exp, scale=0.125), reads=[ps[3]], writes=[W.pprev])
                        op("act", lambda e: e.activation(out=W.pcur[:], in_=ps[4][:], func=AF.Exp, scale=0.125), reads=[ps[4]], writes=[W.pcur])
                        for hh in range(4):
                            mm_group(ps[5], ps[5][:, hh * 65:(hh + 1) * 65],
                                     [(W.pprev[:, hh * 128:(hh + 1) * 128], vps[prv][:, g, :]), (W.pcur[:, hh * 128:(hh + 1) * 128], vps[cur][:, g, :])],
                                     [W.pprev, W.pcur, vps[prv], vps[cur]])
                        pv = ps[5][:, 0:260].rearrange("p (h d) -> p h d", d=65)
                        op("dve", lambda e, g=g, pv=pv: e.tensor_tensor(out=W.ssq[:, 20:24], in0=pv[:, :, 64], in1=esink[:, 4 * g:4 * g + 4], op=ALU.add),
                           reads=[ps[5], esink], writes=[W.ssq])
                        op("dve", lambda e: e.reciprocal(out=W.ssq[:, 24:28], in_=W.ssq[:, 20:24]), reads=[W.ssq], writes=[W.ssq])
                        op("dve", lambda e, g=g, pv=pv: e.tensor_tensor(out=W.hg[:, g * 256:(g + 1) * 256].rearrange("p (h d) -> p h d", d=64), in0=pv[:, :, 0:64],
                                                                        in1=W.ssq[:, 24:28].unsqueeze(2).broadcast_to([128, 4, 64]), op=ALU.mult),
                           reads=[ps[5], W.ssq], writes=[W.hg])
                transpose8(W.hg, W.hgT, pt[1])
                for half in range(2):
                    pb = ps[half]
                    mm_group(pb, pb[:], [(W.hgT[:, kc, :], wmix_out[:, kc, half * 512:(half + 1) * 512]) for kc in range(KC)], [W.hgT, wmix_out])
                    op("dve", lambda e, pb=pb, half=half: e.tensor_tensor(out=W.tmpf[:, half * 512:(half + 1) * 512], in0=pb[:],
                                                                          in1=mod6[:, 2, half * 512:(half + 1) * 512], op=ALU.mult),
                       reads=[pb, mod6], writes=[W.tmpf])
                op("dve", lambda e: e.tensor_tensor(out=W.xt2[:], in0=W.tmpf[:], in1=W.xt[:], op=ALU.add), reads=[W.tmpf, W.xt], writes=[W.xt2])
                dma("pool", lambda e: e.dma_start(out=xs[rows, :], in_=W.xt2[:]), reads=[W.xt2], writes=[xs])
                rmsnorm_mod(W, W.xt2, 4, 3, W.hnb)
                dma("pool", lambda e: e.dma_start(out=hn2d[rows, :], in_=W.hnb[:]), reads=[W.hnb], writes=[hn2d], group=("hn2", layer))
                transpose8(W.hnb, W.hnT, pt[0])
                mm_group(ps[2], ps[2][:, 0:72], [(W.hnT[:, kc, :], wrt[:, kc, :]) for kc in range(KC)], [W.hnT, wrt])
                op("dve", lambda e: e.tensor_tensor(out=W.lg[:], in0=ps[2][:, 0:72], in1=brb[:], op=ALU.add), reads=[ps[2], brb], writes=[W.lg])
                R = W.rsm
                op("dve", lambda e: e.tensor_reduce(out=R[:, 0:1], in_=W.lg[:, 0:8], axis=AX.X, op=ALU.max), reads=[W.lg], writes=[R])
                op("dve", lambda e: e.tensor_scalar(out=R[:, 1:2], in0=R[:, 0:1], scalar1=-1.0, scalar2=None, op0=ALU.mult), reads=[R], writes=[R])
                op("dve", lambda e: e.tensor_scalar(out=R[:, 4:12], in0=W.lg[:, 0:8], scalar1=R[:, 0:1], scalar2=None, op0=ALU.is_equal), reads=[W.lg, R], writes=[R])
                op("act", lambda e: e.activation(out=R[:, 48:56], in_=W.lg[:, 0:8], func=AF.Exp, bias=R[:, 1:2], accum_out=R[:, 2:3]), reads=[W.lg, R], writes=[R])
                op("dve", lambda e: e.reciprocal(out=R[:, 3:4], in_=R[:, 2:3]), reads=[R], writes=[R])
                op("dve", lambda e: e.tensor_tensor(out=W.t64[:].rearrange("p (g x) -> p g x", g=8), in0=W.lg[:, 8:72].rearrange("p (g x) -> p g x", g=8),
                                                    in1=R[:, 4:12].unsqueeze(2).broadcast_to([128, 8, 8]), op=ALU.mult), reads=[W.lg, R], writes=[W.t64])
                op("dve", lambda e: e.tensor_reduce(out=R[:, 12:20], in_=W.t64[:].rearrange("p (g x) -> p x g", g=8), axis=AX.X, op=ALU.add),
                   reads=[W.t64], writes=[R])
                op("dve", lambda e: e.max(out=R[:, 20:28], in_=R[:, 12:20]), reads=[R], writes=[R])
                op("dve", lambda e: e.tensor_scalar(out=R[:, 28:36], in0=R[:, 12:20], scalar1=R[:, 20:21], scalar2=None, op0=ALU.is_equal), reads=[R], writes=[R])
                op("dve", lambda e: e.tensor_scalar(out=R[:, 36:44], in0=R[:, 12:20], scalar1=R[:, 21:22], scalar2=None, op0=ALU.is_equal), reads=[R], writes=[R])
                op("dve", lambda e: e.tensor_tensor(out=R[:, 44:45], in0=R[:, 20:21], in1=R[:, 21:22], op=ALU.subtract), reads=[R], writes=[R])
                op("act", lambda e: e.activation(out=R[:, 45:46], in_=R[:, 44:45], func=AF.Sigmoid), reads=[R], writes=[R])
                op("dve", lambda e: e.tensor_tensor(out=GT[:, 0, t:t + 1], in0=R[:, 45:46], in1=R[:, 3:4], op=ALU.mult), reads=[R], writes=[GT])
                op("dve", lambda e: e.tensor_tensor(out=GT[:, 1, t:t + 1], in0=R[:, 3:4], in1=GT[:, 0, t:t + 1], op=ALU.subtract), reads=[R, GT], writes=[GT])
                for (OH, c0) in ((OH1, 28), (OH2, 36)):
                    op("dve", lambda e, OH=OH, c0=c0: e.tensor_tensor(out=OH[:, t, :].rearrange("p (g x) -> p g x", g=8),
                                                                      in0=R[:, 4:12].unsqueeze(2).broadcast_to([128, 8, 8]),
                                                                      in1=R[:, c0:c0 + 8].unsqueeze(1).broadcast_to([128, 8, 8]), op=ALU.mult),
                       reads=[R], writes=[OH])
                op("dve", lambda e: e.tensor_tensor(out=W.ohs[:], in0=OH1[:, t, :], in1=OH2[:, t, :], op=ALU.add), reads=[OH1, OH2], writes=[W.ohs])
                mm_group(ps[3], ps[3][:, 0:64], [(tristrb[:], W.ohs[:])], [tristrb, W.ohs])
                mm_group(ps[3], ps[3][:, 64:128], [(onesb[:], W.ohs[:])], [onesb, W.ohs])
                if W.p == 1:
                    K.wait_for("base")
                op("dve", lambda e: e.tensor_tensor(out=W.rtot[:], in0=ps[3][:, 0:64], in1=base[:], op=ALU.add), reads=[ps[3], base], writes=[W.rtot])
                op("dve", lambda e: e.tensor_tensor(out=base[:], in0=ps[3][:, 64:128], in1=base[:], op=ALU.add), reads=[ps[3], base], writes=[base])
                if W.p == 0:
                    K.signal("base")
                for k_, OH in ((0, OH1), (1, OH2)):
                    op("dve", lambda e, OH=OH: e.tensor_tensor(out=W.t64[:], in0=OH[:, t, :], in1=W.rtot[:], op=ALU.mult), reads=[OH, W.rtot], writes=[W.t64])
                    op("dve", lambda e, k_=k_: e.tensor_reduce(out=R12[:, k_, t:t + 1], in_=W.t64[:], axis=AX.X, op=ALU.add), reads=[W.t64], writes=[R12])

            for t0_ in range(0, NT, 2):
                K.interleave([lambda t=t0_ + q_: tile_body(t, _PS(t % 2), _PT(t % 2), WS[t % 2]) for q_ in range(min(2, NT - t0_))])

            op("dve", lambda e: e.tensor_scalar(out=pp[:, 0, :], in0=base[:], scalar1=1.0 / BS, scalar2=(BS / 2 - 0.5) / BS, op0=ALU.mult, op1=ALU.add),
               reads=[base], writes=[pp])
            op("dve", lambda e: e.tensor_copy(out=ppi[:], in_=pp[:, 0, :]), reads=[pp], writes=[ppi])
            op("dve", lambda e: e.tensor_copy(out=pp[:, 0, :], in_=ppi[:]), reads=[ppi], writes=[pp])
            op("dve", lambda e: e.tensor_scalar(out=pp[:, 0, :], in0=pp[:, 0, :], scalar1=float(BS), scalar2=None, op0=ALU.mult), reads=[pp], writes=[pp])
            op("dve", lambda e: e.tensor_tensor_scan(out=pp[:, 1, :], data0=onesf[:, 0:64], data1=pp[:, 0, :], initial=0.0, op0=ALU.mult, op1=ALU.add),
               reads=[pp, onesf], writes=[pp])
            op("dve", lambda e: e.tensor_tensor(out=pp[:, 2, :], in0=pp[:, 1, :], in1=pp[:, 0, :], op=ALU.subtract), reads=[pp], writes=[pp])
            for k_, OH in ((0, OH1), (1, OH2)):
                for c0 in range(0, NT, 16):
                    n = min(16, NT - c0)
                    op("dve", lambda e, OH=OH, c0=c0, n=n: e.tensor_tensor(out=big[:, 0:n, :], in0=OH[:, c0:c0 + n, :],
                                                                           in1=pp[:, 2, :].unsqueeze(1).broadcast_to([128, n, 64]), op=ALU.mult),
                       reads=[OH, pp], writes=[bigb])
                    op("dve", lambda e, k_=k_, c0=c0, n=n: e.tensor_reduce(out=DEST[:, k_, c0:c0 + n], in_=big[:, 0:n, :], axis=AX.X, op=ALU.add),
                       reads=[bigb], writes=[DEST])
            op("dve", lambda e: e.tensor_tensor(out=DEST[:], in0=DEST[:], in1=R12[:], op=ALU.add), reads=[DEST, R12], writes=[DEST])
            op("dve", lambda e: e.tensor_copy(out=DESTi[:], in_=DEST[:]), reads=[DEST], writes=[DESTi])
            for k_ in range(2):
                op("dve", lambda e, k_=k_: e.tensor_copy(out=SRC[:, k_, :, 0], in_=tokid[:]), reads=[tokid], writes=[SRC])
                op("dve", lambda e, k_=k_: e.tensor_copy(out=SRC[:, k_, :, 1], in_=GT[:, k_, :]), reads=[GT], writes=[SRC])
            for c0 in range(0, NBLK, 16):
                n = min(16, NBLK - c0)
                op("dve", lambda e, c0=c0, n=n: e.tensor_scalar(out=sm[:, 16:16 + n], in0=jb[:, 0:n], scalar1=float(c0 * BS), scalar2=None, op0=ALU.add),
                   reads=[jb], writes=[sm])
                op("dve", lambda e, n=n: e.tensor_tensor(out=big[:, 0:n, :], in0=pp[:, 1, :].unsqueeze(1).broadcast_to([128, n, 64]),
                                                         in1=sm[:, 16:16 + n].unsqueeze(2).broadcast_to([128, n, 64]), op=ALU.is_le),
                   reads=[pp, sm], writes=[bigb])
                op("dve", lambda e, c0=c0, n=n: e.tensor_reduce(out=BE[:, c0:c0 + n], in_=big[:, 0:n, :], axis=AX.X, op=ALU.add), reads=[bigb], writes=[BE])
            op("dve", lambda e: e.tensor_scalar(out=BE[:], in0=BE[:], scalar1=float(NE - 1), scalar2=128.0, op0=ALU.min, op1=ALU.mult), reads=[BE], writes=[BE])
            op("dve", lambda e: e.tensor_scalar(out=BE[:], in0=BE[:], scalar1=iota_p[:, 0:1], scalar2=float(layer * NE * 128), op0=ALU.add, op1=ALU.add),
               reads=[BE, iota_p], writes=[BE])
            op("dve", lambda e: e.tensor_copy(out=BEi[:], in_=BE[:]), reads=[BE], writes=[BEi])
            NR_ = NSLOT // 128
            for r0_ in range(0, NR_, 16):
                dma("sp", lambda e, r0_=r0_: e.dma_start(out=slot_tw.ap().rearrange("(p r) c -> p r c", p=128)[:, r0_:r0_ + 16, :], in_=padinit[:]),
                    reads=[padinit], writes=[slot_tw], group=("pi", layer))
            for k_ in range(2):
                for t in range(NT):
                    dma("pool", lambda e, k_=k_, t=t: e.indirect_dma_start(
                        out=slot_tw[:, :], out_offset=bass.IndirectOffsetOnAxis(ap=DESTi[:, k_, t:t + 1], axis=0),
                        in_=SRC[:, k_, t, :], in_offset=None), reads=[SRC, DESTi], writes=[slot_tw], sem_buf=SRC, group=("sc", layer))

            K.barrier()

            def expert_block(jblk, ps, pt, c_):
                pb_ = jblk % 3
                xg, xTm, hTm, stw, stok = xgc[c_], xTmc[c_], hTmc[c_], stwc[c_], stokc[c_]
                yo = [WS[c_].xt, WS[c_].xt2]
                sil = WS[c_].tmpA
                for (wb, wr_) in ((w1b[pb_], w1r), (w3b[pb_], w3r), (w2b[pb_], w2r)):
                    dma("pool", lambda e, wb=wb, wr_=wr_: e.indirect_dma_start(
                        out=wb[:].rearrange("p k n -> p (k n)"), out_offset=None, in_=wr_[:, :],
                        in_offset=bass.IndirectOffsetOnAxis(ap=BEi[:, jblk:jblk + 1], axis=0)), reads=[wsrc, BEi], writes=[wb])
                for sub in range(BS // 128):
                    r0 = jblk * BS + sub * 128
                    dma("sp", lambda e, sub=sub, r0=r0: e.dma_start(out=stw[sub][:], in_=slot_tw[r0:r0 + 128, :]), reads=[slot_tw], writes=[stw[sub]])
                    op("dve", lambda e, sub=sub: e.tensor_copy(out=stok[sub][:], in_=stw[sub][:, 0:1]), reads=[stw[sub]], writes=[stok[sub]])
                    dma("pool", lambda e, sub=sub: e.indirect_dma_start(
                        out=xg[sub][:], out_offset=None, in_=hn2d[:, :],
                        in_offset=bass.IndirectOffsetOnAxis(ap=stok[sub][:, 0:1], axis=0)), reads=[hn2d, stok[sub]], writes=[xg[sub]])
                    K.hold += 1
                    for kc in range(KC):
                        op("pe", lambda e, kc=kc, sub=sub: e.transpose(out=pt[0][:, kc * 128:(kc + 1) * 128], in_=xg[sub][:, kc * 128:(kc + 1) * 128],
                                                                       identity=identb[:]), reads=[xg[sub], identb], writes=[pt[0]], inc=(kc == KC - 1))
                    K.hold -= 1
                    op("act", lambda e, sub=sub: e.copy(out=xTm[:, :, sub * 128:(sub + 1) * 128], in_=pt[0][:].rearrange("p (k n) -> p k n", k=KC)),
                       reads=[pt[0]], writes=[xTm])
                for m in range(3):
                    p1, p3 = ps[(2 * m) % 3], ps[(2 * m + 1) % 3]
                    mm_group(p1, p1[:, 0:BS], [(w1b[pb_][:, kc, m * 128:(m + 1) * 128], xTm[:, kc, :]) for kc in range(KC)], [w1b[pb_], xTm])
                    mm_group(p3, p3[:, 0:BS], [(w3b[pb_][:, kc, m * 128:(m + 1) * 128], xTm[:, kc, :]) for kc in range(KC)], [w3b[pb_], xTm])
                    op("act", lambda e, p1=p1: e.activation(out=sil[:, 0:BS], in_=p1[:, 0:BS], func=AF.Silu), reads=[p1], writes=[sil])
                    op("dve", lambda e, p3=p3, m=m: e.tensor_tensor(out=hTm[:, m, :], in0=p3[:, 0:BS], in1=sil[:, 0:BS], op=ALU.mult), reads=[p3, sil], writes=[hTm])
                for sub in range(BS // 128):
                    r0 = jblk * BS + sub * 128
                    for half in range(2):
                        pb = ps[(2 * sub + half) % 3]
                        mm_group(pb, pb[:], [(hTm[:, c, sub * 128:(sub + 1) * 128], w2b[pb_][:, c, half * 512:(half + 1) * 512]) for c in range(3)],
                                 [hTm, w2b[pb_]])
                        op("act", lambda e, pb=pb, sub=sub, half=half: e.activation(out=yo[sub][:, half * 512:(half + 1) * 512], in_=pb[:], func=AF.Copy,
                                                                                    scale=stw[sub][:, 1:2]), reads=[pb, stw[sub]], writes=[yo[sub]])
                        op("dve", lambda e, sub=sub, half=half: e.tensor_tensor(out=yo[sub][:, half * 512:(half + 1) * 512], in0=yo[sub][:, half * 512:(half + 1) * 512],
                                                                                in1=mod6[:, 5, half * 512:(half + 1) * 512], op=ALU.mult),
                           reads=[yo[sub], mod6], writes=[yo[sub]])
                    dma("act", lambda e, sub=sub, r0=r0: e.dma_start(out=y_slots[r0:r0 + 128, :], in_=yo[sub][:]), reads=[yo[sub]], writes=[y_slots],
                        group=("ys", layer))

            for j0_ in range(0, NBLK, 2):
                K.interleave([lambda j=j0_ + q_: expert_block(j, _PS(j % 2), _PT(j % 2), j % 2) for q_ in range(min(2, NBLK - j0_))])

            K.barrier()
            dst_t = out if last else xs.t
            dst_b = out_b if last else xs

            def combine_tile(t, c_):
                rows = slice(t * 128, (t + 1) * 128)
                xt, xt2, ya, yb = WS[c_].xt, WS[c_].xt2, WS[c_].tmpf, ybc[c_]
                dma("sp", lambda e: e.dma_start(out=xt[:], in_=xs[rows, :]), reads=[xs], writes=[xt])
                dma("pool", lambda e, t=t: e.indirect_dma_start(out=ya[:], out_offset=None, in_=y_slots[:, :],
                                                               in_offset=bass.IndirectOffsetOnAxis(ap=DESTi[:, 0, t:t + 1], axis=0)),
                    reads=[y_slots, DESTi], writes=[ya])
                dma("pool", lambda e, t=t: e.indirect_dma_start(out=yb[:], out_offset=None, in_=y_slots[:, :],
                                                               in_offset=bass.IndirectOffsetOnAxis(ap=DESTi[:, 1, t:t + 1], axis=0)),
                    reads=[y_slots, DESTi], writes=[yb])
                op("dve", lambda e: e.tensor_tensor(out=ya[:], in0=ya[:], in1=yb[:], op=ALU.add), reads=[ya, yb], writes=[ya])
                op("dve", lambda e: e.tensor_tensor(out=xt2[:], in0=ya[:], in1=xt[:], op=ALU.add), reads=[ya, xt], writes=[xt2])
                if last:
                    dma("act", lambda e: e.dma_start(out=dst_t[rows, :], in_=xt2[:]), reads=[xt2], writes=[dst_b], group=("out", layer))
                else:
                    dma("act", lambda e: e.dma_start(out=dst_t[rows, :], in_=xt2[:]), reads=[xt2], writes=[dst_b])
                if debug:
                    dma("sp", lambda e: e.dma_start(out=dbg[layer * S + t * 128:layer * S + (t + 1) * 128, :], in_=xt2[:]), reads=[xt2], writes=[dbg_b])

            if last:
                for t0_ in range(0, NT, 2):
                    K.interleave([lambda t=t0_ + q_: combine_tile(t, t % 2) for q_ in range(min(2, NT - t0_))])
            x_src = xs
            x_src_t = xs.t
            K.barrier()
        K.finish([out_b] + ([dbg_b] if debug else []))
        K.barrier()
        ninst = K.ninst
    return nc, ninst


def prep_weights(inp):
    f = lambda a: np.ascontiguousarray(np.asarray(a, dtype=np.float32))
    w = {}
    for k in ("w_ada", "b_ada", "norm1_g", "norm2_g", "ml_w_in", "ml_b_gate", "ml_g_out", "ml_w_out",
              "sw_w_in", "sw_g_q", "sw_g_k", "sw_sinks", "sw_w_out"):
        w[k] = f(inp[k])
    w["w_rt"] = f(np.concatenate([np.asarray(inp["moe_w_group"]), np.asarray(inp["moe_w_router"])], axis=-1))
    w["b_rt"] = f(np.concatenate([np.asarray(inp["moe_b_group"]), np.asarray(inp["moe_b_router"])], axis=-1))
    w1 = np.asarray(inp["moe_w1"]).reshape(DEPTH, NE, KC, 128, DE)
    w["w1r"] = f(w1.transpose(0, 1, 3, 2, 4).reshape(DEPTH * NE * 128, KC * DE))
    w3 = np.asarray(inp["moe_w3"]).reshape(DEPTH, NE, KC, 128, DE)
    w["w3r"] = f(w3.transpose(0, 1, 3, 2, 4).reshape(DEPTH * NE * 128, KC * DE))
    w2 = np.asarray(inp["moe_w2"]).reshape(DEPTH, NE, 3, 128, D)
    w["w2r"] = f(w2.transpose(0, 1, 3, 2, 4).reshape(DEPTH * NE * 128, 3 * D))
    return w


def run(inp, S, depth=DEPTH, trace=False, debug=False):
    x = np.asarray(inp["x"], dtype=np.float32)
    c = np.asarray(inp["c"], dtype=np.float32)
    B = x.shape[0]
    w = prep_weights(inp)
    nc, ninst = build_program(S, depth, debug)
    in_maps = []
    for core in range(8):
        b = (core // 2) % B
        m = dict(w)
        m["x"] = np.ascontiguousarray(x[b, :S])
        m["c"] = np.ascontiguousarray(c[b].reshape(KC, 128).T)
        m["coff"] = np.full((128, 1), 0.0 if core % 2 == 0 else 1.0e7, dtype=np.float32)
        in_maps.append(m)
    res = run_bass_kernel_spmd(nc, in_maps, core_ids=list(range(8)), **({"trace": True} if trace else {}))
    outs = np.stack([res.results[2 * b]["out"] for b in range(B)], axis=0)
    return outs, res


def kernel(**inputs):
    S = np.asarray(inputs["x"]).shape[1]
    outs, _ = run(inputs, S, DEPTH)
    return outs.astype(np.float32)
```
